# Optimizing a Trainium2 kernel written in Bass

```python
import math
import jax, jax.numpy as jnp
from jax import lax
import numpy as np

D_MODEL = 1024
BATCH = 4
SEQ = 8192
DEPTH = 1

ATT_GROUPS = ((128, 1), (512, 4), (2048, 16))
N_GROUPS = len(ATT_GROUPS)
HEADS_PER_GROUP = 8
HEAD_DIM = 64
N_ATT_HEADS = N_GROUPS * HEADS_PER_GROUP
QKV_WIDTH = N_ATT_HEADS * HEAD_DIM
ATT_OUT_WIDTH = HEADS_PER_GROUP * HEAD_DIM
ATT_BLOCK = 128
N_BUCKETS = 32
MAX_DISTANCE = 2048
CONV_CH = 768
CONV_WIDTH = 31
N_EXPERTS = 32
TOP_K = 4
D_FF = 1024
SWIGLU_LIMIT = 7.0
SWIGLU_ALPHA = 1.702
MOE_BLOCK = 512
IN_WIDTH = 3 * QKV_WIDTH + 2 * CONV_CH + 2 * D_MODEL
LN_EPS = 1e-5
NEG_INF = -1e30
DEEPNORM_ALPHA = (2 * DEPTH) ** 0.25
DEEPNORM_BETA = (8 * DEPTH) ** -0.25

kernel_name = 'hybrid_dilated_attn_conformer_conv_moe_deepnorm'


def layer_norm(x, g, b):
    xf = x.astype(jnp.float32)
    mu = jnp.mean(xf, axis=-1, keepdims=True)
    var = jnp.mean(jnp.square(xf - mu), axis=-1, keepdims=True)
    y = (xf - mu) * lax.rsqrt(var + LN_EPS) * g.astype(jnp.float32) + b.astype(jnp.float32)
    return y.astype(x.dtype)


def t5_bucket(dist):
    max_exact = N_BUCKETS // 2
    log_ratio = jnp.log(jnp.maximum(dist, max_exact).astype(jnp.float32) / max_exact) / math.log(MAX_DISTANCE / max_exact)
    large = jnp.minimum(max_exact + (log_ratio * (N_BUCKETS - max_exact)).astype(jnp.int32), N_BUCKETS - 1)
    return jnp.where(dist < max_exact, dist, large)


def dilated_window_attention(q, k, v, bias_table, window, dil):
    B, S, H, E = q.shape
    L = S // dil
    nb = -(-L // ATT_BLOCK)
    Lp = nb * ATT_BLOCK
    sub_win = window // dil

    def to_sub(t):
        return jnp.swapaxes(t.reshape(B, L, dil, H, E), 1, 2)

    def key_blocks(t):
        tp = jnp.pad(t, ((0, 0), (0, 0), (ATT_BLOCK, Lp - L), (0, 0), (0, 0)))
        prev = tp[:, :, :Lp].reshape(B, dil, nb, ATT_BLOCK, H, E)
        cur = tp[:, :, ATT_BLOCK:].reshape(B, dil, nb, ATT_BLOCK, H, E)
        return jnp.concatenate([prev, cur], axis=3)

    qs = jnp.pad(to_sub(q), ((0, 0), (0, 0), (0, Lp - L), (0, 0), (0, 0))).reshape(B, dil, nb, ATT_BLOCK, H, E)
    kb = key_blocks(to_sub(k))
    vb = key_blocks(to_sub(v))

    qi = jnp.arange(ATT_BLOCK)[:, None]
    kj = jnp.arange(2 * ATT_BLOCK)[None, :]
    dist = qi - kj + ATT_BLOCK
    in_band = (dist >= 0) & (dist <= sub_win)
    key_pos = jnp.arange(nb)[:, None, None] * ATT_BLOCK + kj[None] - ATT_BLOCK
    mask = in_band[None] & (key_pos >= 0)
    bias = bias_table[t5_bucket(jnp.maximum(dist, 0) * dil)]
    bias = jnp.transpose(bias, (2, 0, 1)).astype(jnp.float32)

    s = jnp.einsum('brnqhe,brnkhe->brnhqk', qs, kb, preferred_element_type=jnp.float32) * (HEAD_DIM ** -0.5) + bias
    s = jnp.where(mask[:, None], s, NEG_INF)
    m = jnp.max(s, axis=-1, keepdims=True)
    p = jnp.exp(s - m)
    den = jnp.sum(p, axis=-1)
    o = jnp.einsum('brnhqk,brnkhe->brnqhe', p, vb.astype(jnp.float32))
    o = o / jnp.swapaxes(den, 3, 4)[..., None]

    def from_sub(t):
        tail = t.shape[4:]
        t = t.reshape((B, dil, Lp) + tail)[:, :, :L]
        return jnp.swapaxes(t, 1, 2).reshape((B, S) + tail)

    return (from_sub(o), from_sub(jnp.swapaxes(m[..., 0], 3, 4)), from_sub(jnp.swapaxes(den, 3, 4)))


def token_mixer(h, w_in, rel_bias, w_dw, b_dw, conv_ln_g, conv_ln_b, w_o_attn, w_o_conv, w_out):
    B, S, _ = h.shape
    proj = jnp.matmul(h, w_in)
    q, k, v, u, g_attn, g_conv = jnp.split(
        proj, [QKV_WIDTH, 2 * QKV_WIDTH, 3 * QKV_WIDTH, 3 * QKV_WIDTH + 2 * CONV_CH,
               3 * QKV_WIDTH + 2 * CONV_CH + D_MODEL], axis=-1)
    q = q.reshape(B, S, N_GROUPS, HEADS_PER_GROUP, HEAD_DIM)
    k = k.reshape(B, S, N_GROUPS, HEADS_PER_GROUP, HEAD_DIM)
    v = v.reshape(B, S, N_GROUPS, HEADS_PER_GROUP, HEAD_DIM)

    outs, maxes, dens = [], [], []
    for gi, (window, dil) in enumerate(ATT_GROUPS):
        table = rel_bias[:, gi * HEADS_PER_GROUP:(gi + 1) * HEADS_PER_GROUP]
        o_g, m_g, d_g = dilated_window_attention(q[:, :, gi], k[:, :, gi], v[:, :, gi], table, window, dil)
        outs.append(o_g)
        maxes.append(m_g)
        dens.append(d_g)
    outs = jnp.stack(outs)
    maxes = jnp.stack(maxes)
    dens = jnp.stack(dens)
    wts = dens * jnp.exp(maxes - jnp.max(maxes, axis=0, keepdims=True))
    attn = jnp.sum(wts[..., None] * outs, axis=0) / jnp.sum(wts, axis=0)[..., None]
    attn = attn.reshape(B, S, ATT_OUT_WIDTH).astype(h.dtype)

    u_val, u_gate = jnp.split(u, 2, axis=-1)
    glu = u_val * jax.nn.sigmoid(u_gate)
    dw = lax.conv_general_dilated(glu, w_dw, window_strides=(1,), padding=[(CONV_WIDTH - 1, 0)],
                                  dimension_numbers=('NWC', 'WIO', 'NWC'), feature_group_count=CONV_CH) + b_dw
    conv = jax.nn.silu(layer_norm(dw, conv_ln_g, conv_ln_b))

    merged = (jax.nn.sigmoid(g_attn) * jnp.matmul(attn, w_o_attn)
              + jax.nn.sigmoid(g_conv) * jnp.matmul(conv, w_o_conv))
    return jnp.matmul(merged, w_out)


def routed_experts(h, w_router, b_router, w_gate_up, b_gate_up, w_down, b_down):
    B, S, D = h.shape
    T = B * S
    xf = h.reshape(T, D)
    logits = jnp.matmul(xf, w_router, preferred_element_type=jnp.float32) + b_router.astype(jnp.float32)
    top_v, top_e = lax.top_k(logits, TOP_K)
    gates = jax.nn.softmax(top_v, axis=-1)

    flat_e = top_e.reshape(-1)
    flat_g = gates.reshape(-1)
    flat_tok = jnp.arange(T * TOP_K, dtype=jnp.int32) // TOP_K
    order = jnp.argsort(flat_e)
    sorted_e = flat_e[order]
    counts = jnp.bincount(flat_e, length=N_EXPERTS)
    starts = jnp.cumsum(counts) - counts
    padded = (counts + MOE_BLOCK - 1) // MOE_BLOCK * MOE_BLOCK
    pad_ends = jnp.cumsum(padded)
    pad_starts = pad_ends - padded
    dest = pad_starts[sorted_e] + jnp.arange(T * TOP_K, dtype=jnp.int32) - starts[sorted_e]
    n_rows = T * TOP_K + N_EXPERTS * MOE_BLOCK
    n_blocks = n_rows // MOE_BLOCK
    row_tok = jnp.zeros((n_rows,), jnp.int32).at[dest].set(flat_tok[order])
    row_w = jnp.zeros((n_rows,), jnp.float32).at[dest].set(flat_g[order])
    blk_e = jnp.minimum(jnp.searchsorted(pad_ends, jnp.arange(n_blocks) * MOE_BLOCK, side='right'), N_EXPERTS - 1)
    xs = xf[row_tok].reshape(n_blocks, MOE_BLOCK, D)
    ws = row_w.reshape(n_blocks, MOE_BLOCK)

    def expert_block(args):
        xb, wb, e = args
        hgu = jnp.matmul(xb, w_gate_up[e]) + b_gate_up[e]
        gate = jnp.minimum(hgu[:, :D_FF], SWIGLU_LIMIT)
        up = jnp.clip(hgu[:, D_FF:], -SWIGLU_LIMIT, SWIGLU_LIMIT)
        act = (up + 1.0) * gate * jax.nn.sigmoid(SWIGLU_ALPHA * gate)
        y = jnp.matmul(act, w_down[e]) + b_down[e]
        return y.astype(jnp.float32) * wb[:, None]

    ys = lax.map(expert_block, (xs, ws, blk_e)).reshape(n_rows, D)
    out = jax.ops.segment_sum(ys, row_tok, num_segments=T)
    return out.reshape(B, S, D).astype(h.dtype)


def setup_inputs(seed: int = 0) -> dict:
    key = jax.random.key(seed)
    ks = jax.random.split(key, 20)
    D = D_MODEL
    f32 = jnp.float32

    def nrm(k, shape, scale):
        return jax.random.normal(k, shape, f32) * scale

    col_scale = jnp.concatenate([jnp.ones((2 * QKV_WIDTH,), f32), jnp.full((QKV_WIDTH,), DEEPNORM_BETA, f32),
                                 jnp.ones((2 * CONV_CH + 2 * D,), f32)])
    return {
        'x': nrm(ks[0], (BATCH, SEQ, D), 1.0),
        'w_in': nrm(ks[1], (DEPTH, D, IN_WIDTH), D ** -0.5) * col_scale,
        'rel_bias': nrm(ks[2], (N_BUCKETS, N_ATT_HEADS), 0.5),
        'w_dw': nrm(ks[3], (DEPTH, CONV_WIDTH, 1, CONV_CH), CONV_WIDTH ** -0.5),
        'b_dw': nrm(ks[4], (DEPTH, CONV_CH), 0.02),
        'conv_ln_g': 1.0 + nrm(ks[5], (DEPTH, CONV_CH), 0.02),
        'conv_ln_b': nrm(ks[6], (DEPTH, CONV_CH), 0.02),
        'w_o_attn': nrm(ks[7], (DEPTH, ATT_OUT_WIDTH, D), ATT_OUT_WIDTH ** -0.5 * DEEPNORM_BETA),
        'w_o_conv': nrm(ks[8], (DEPTH, CONV_CH, D), CONV_CH ** -0.5 * DEEPNORM_BETA),
        'w_out': nrm(ks[9], (DEPTH, D, D), D ** -0.5 * DEEPNORM_BETA),
        'ln1_g': 1.0 + nrm(ks[10], (DEPTH, D), 0.02),
        'ln1_b': nrm(ks[11], (DEPTH, D), 0.02),
        'w_router': nrm(ks[12], (DEPTH, D, N_EXPERTS), D ** -0.5),
        'b_router': nrm(ks[13], (DEPTH, N_EXPERTS), 0.01),
        'w_gate_up': nrm(ks[14], (DEPTH, N_EXPERTS, D, 2 * D_FF), D ** -0.5 * DEEPNORM_BETA),
        'b_gate_up': nrm(ks[15], (DEPTH, N_EXPERTS, 2 * D_FF), 0.01),
        'w_down': nrm(ks[16], (DEPTH, N_EXPERTS, D_FF, D), D_FF ** -0.5 * DEEPNORM_BETA),
        'b_down': nrm(ks[17], (DEPTH, N_EXPERTS, D), 0.01),
        'ln2_g': 1.0 + nrm(ks[18], (DEPTH, D), 0.02),
        'ln2_b': nrm(ks[19], (DEPTH, D), 0.02),
    }


def reference(x, w_in, rel_bias, w_dw, b_dw, conv_ln_g, conv_ln_b, w_o_attn, w_o_conv, w_out,
              ln1_g, ln1_b, w_router, b_router, w_gate_up, b_gate_up, w_down, b_down, ln2_g, ln2_b):
    h = x
    for l in range(DEPTH):
        mix = token_mixer(h, w_in[l], rel_bias, w_dw[l], b_dw[l], conv_ln_g[l], conv_ln_b[l],
                          w_o_attn[l], w_o_conv[l], w_out[l])
        h = layer_norm(DEEPNORM_ALPHA * h + mix, ln1_g[l], ln1_b[l])
        ffn = routed_experts(h, w_router[l], b_router[l], w_gate_up[l], b_gate_up[l], w_down[l], b_down[l])
        h = layer_norm(DEEPNORM_ALPHA * h + ffn, ln2_g[l], ln2_b[l])
    return h
```

```python
import math
from contextlib import ExitStack
import numpy as np
import concourse.bass as bass
import concourse.mybir as mybir
from concourse.bass_utils import run_bass_kernel_spmd

F32 = mybir.dt.float32
BF16 = mybir.dt.bfloat16
I32 = mybir.dt.int32
AF = mybir.ActivationFunctionType
ALU = mybir.AluOpType

NCORES = 8
D = 1024
OWN = 4096
HALO = 2048
NP = OWN + HALO
CAP = 704
CAPT = 768
NE = 32
ALPHA = 2.0 ** 0.25
EPS = 1e-5
ENGS = ("pe", "act", "dve", "pool", "sp")
DBG = {"iters": 99, "units": True, "final": True, "proj": 3, "pv": True, "slevel": 3, "hb": True, "pvacc": True}


class Sched:
    EPOCH = 8000

    def __init__(self, nc, es):
        self.nc = nc
        self.es = es
        self.loc = es
        self.streams = {e: [] for e in ENGS}
        self.cnt = {e: 0 for e in ENGS}
        self.esems = {e: [] for e in ENGS}
        self.res = {}
        self.waited = {e: {} for e in ENGS}
        self.dmasems = {}
        self.dmarr = {}
        for q, n in {"sp": 10, "act": 30, "pool": 10}.items():
            self.dmasems[q] = [[self.sem(f"dma_{q}{i}"), 0] for i in range(n)]
            self.dmarr[q] = 0

    def sem(self, name):
        return self.es.enter_context(self.nc.semaphore(name))

    def sb(self, name, shape, dt):
        return self.loc.enter_context(self.nc.sbuf_tensor(name, list(shape), dt))

    def ps(self, name, shape=(128, 512), dt=F32):
        return self.loc.enter_context(self.nc.psum_tensor(name, list(shape), dt))

    def _collect(self, eng, reads, writes):
        deps = []
        for r in reads:
            st = self.res.get(r)
            if st and st["w"] is not None:
                deps.append(st["w"])
        for w in writes:
            st = self.res.get(w)
            if st:
                if st["w"] is not None:
                    deps.append(st["w"])
                deps.extend(st["r"])
        know = self.waited[eng]
        out = []
        for (sem, val, peng, vc) in deps:
            if peng == "pe" and eng == "pe":
                continue
            if know.get(id(sem), 0) >= val:
                continue
            out.append((sem, val))
            for k, v in vc.items():
                if know.get(k, 0) < v:
                    know[k] = v
        return out

    def _record(self, tok, reads, writes):
        for r in reads:
            st = self.res.setdefault(r, {"w": None, "r": []})
            st["r"].append(tok)
        for w in writes:
            self.res[w] = {"w": tok, "r": []}

    def op(self, eng, fn, reads=(), writes=(), attach=None):
        waits = self._collect(eng, reads, writes)
        n = self.cnt[eng]
        ep = n // self.EPOCH
        while len(self.esems[eng]) <= ep:
            self.esems[eng].append(self.sem(f"c_{eng}{len(self.esems[eng])}"))
        sem = self.esems[eng][ep]
        val = n - ep * self.EPOCH + 1
        self.cnt[eng] = n + 1
        for w in waits:
            self.streams[eng].append(("wait", w[0], w[1]))
        self.streams[eng].append(("op", fn, sem, 1, (eng != "pe") if attach is None else attach))
        vc = dict(self.waited[eng])
        vc[id(sem)] = val
        for pe_ in range(ep):
            vc[id(self.esems[eng][pe_])] = self.EPOCH
        tok = (sem, val, eng, vc)
        self._record(tok, reads, writes)
        return tok

    def dma(self, q, fn, reads=(), writes=()):
        waits = self._collect(q, reads, writes)
        slot = self.dmasems[q][self.dmarr[q] % len(self.dmasems[q])]
        self.dmarr[q] += 1
        sem, total = slot
        if total > 0 and self.waited[q].get(id(sem), 0) < total:
            self.waited[q][id(sem)] = total
            waits.append((sem, total))
        total += 16
        slot[1] = total
        for w in waits:
            self.streams[q].append(("wait", w[0], w[1]))
        self.streams[q].append(("op", fn, sem, 16, False))
        vc = dict(self.waited[q])
        vc[id(sem)] = total
        tok = (sem, total, "dma", vc)
        self._record(tok, reads, writes)
        return tok

    def barrier(self):
        keys = list(self.res.keys())
        for eng in ENGS:
            for w in self._collect(eng, keys, keys):
                self.streams[eng].append(("wait", w[0], w[1]))
        self.res = {}

    def emit(self):
        streams = self.streams
        self.regcache = {}

        def run(e, items):
            pend = []
            for it in items:
                if it[0] == "wait":
                    pend.append(it)
                    continue
                attach = pend.pop() if (it[4] and pend) else None
                for w in pend:
                    e.wait_ge(w[1], w[2])
                pend = []
                ins = it[1](e)
                first, last = ins if isinstance(ins, tuple) else (ins, ins)
                if attach is not None:
                    first._wait_ge(attach[1], attach[2])
                last.then_inc(it[2], it[3])
            for w in pend:
                e.wait_ge(w[1], w[2])

        with self.nc.Block() as block:
            @block.sync
            def _(e):
                run(e, streams["sp"])

            @block.scalar
            def _(e):
                run(e, streams["act"])

            @block.vector
            def _(e):
                run(e, streams["dve"])

            @block.gpsimd
            def _(e):
                run(e, streams["pool"])

            @block.tensor
            def _(e):
                run(e, streams["pe"])
        self.streams = {e: [] for e in ENGS}

    def mm(self, out, pairs, reads, writes):
        def fn(e):
            n = len(pairs)
            ins = first = None
            for i, (l, r) in enumerate(pairs):
                ins = e.matmul(out, lhsT=l, rhs=r, start=(i == 0), stop=(i == n - 1))
                if first is None:
                    first = ins
            return first, ins
        return self.op("pe", fn, reads, writes, attach=True)


def _bc(s, e):
    if "bc" not in s.regcache:
        s.regcache["bc"] = e.to_reg(NE * CAP - 1)
    return s.regcache["bc"]


def _copy(s, eng, out, in_, reads, writes):
    if eng == "act":
        return s.op("act", lambda e: e.activation(out=out, in_=in_, func=AF.Identity), reads, writes)
    return s.op(eng, lambda e: e.tensor_copy(out=out, in_=in_), reads, writes)


def build(stop_after=99, debug=False):
    nc = bass.Bass("TRN2", target_bir_lowering=False)

    def din(name, shape, dt=F32):
        return nc.dram_tensor(name, list(shape), dt, kind="ExternalInput").ap()

    xT_d = din("xT", [D, NP])
    xown_d = din("xown", [OWN, D])
    hb_d = din("hbias", [128, 1])
    ab_d = din("abias", [24, 128, 256])
    win_d = din("w_in", [D, 8192])
    wdw_d = din("w_dw", [128, 6, 31])
    bdw_d = din("b_dw", [128, 6])
    clg_d = din("conv_ln_g", [128, 6])
    clb_d = din("conv_ln_b", [128, 6])
    woa_d = din("w_o_attn", [512, D])
    woc_d = din("w_o_conv", [768, D])
    wout_d = din("w_out", [D, D])
    ln1g_d = din("ln1_g", [1, D])
    ln1b_d = din("ln1_b", [1, D])
    wr_d = din("w_router", [D, NE])
    br_d = din("b_router", [1, NE])
    wgu_d = din("w_gate_up", [NE, D, 2048]) if stop_after >= 6 else None
    bgu_d = din("b_gate_up", [NE, 128, 16])
    wd_d = din("w_down", [NE, D, D]) if stop_after >= 6 else None
    bd_d = din("b_down", [NE, D])
    ln2g_d = din("ln2_g", [1, D])
    ln2b_d = din("ln2_b", [1, D])
    out_d = nc.dram_tensor("out", [OWN, D], F32, kind="ExternalOutput").ap()
    skind = "ExternalOutput" if debug else "Internal"
    attnT_d = nc.dram_tensor("attnT_s", [512, OWN], BF16, kind=skind).ap()
    convT_d = nc.dram_tensor("convT_s", [768, OWN], BF16, kind=skind).ap()
    mrgT_d = nc.dram_tensor("mrgT_s", [D, OWN], BF16, kind=skind).ap()
    h1_d = nc.dram_tensor("h1_s", [OWN, D], F32, kind=skind).ap()
    xs_d = nc.dram_tensor("xs_s", [NE * CAP + 128, D], BF16, kind="Internal").ap()
    y_d = nc.dram_tensor("y_s", [NE * CAP, D], F32, kind="Internal").ap()
    rt_d = nc.dram_tensor("rt_s", [128, 32, 8], F32, kind=skind).ap()

    win_v = win_d.rearrange("(kc p) n -> p kc n", p=128)

    with ExitStack() as es:
        s = Sched(nc, es)
        identb = s.sb("identb", [128, 128], BF16)
        identf = s.sb("identf", [128, 128], F32)
        idx_all = s.sb("idx_all", [128, 128], I32)
        gk_all = s.sb("gk_all", [128, 32, 4], F32)
        G_all = s.sb("G_all", [128, 32, NE], F32)
        for t, nm in ((identb, "identb"), (identf, "identf")):
            s.op("pool", lambda e, t=t: e.memset(t[:], 1.0), writes=[nm])
            s.op("pool", lambda e, t=t: e.affine_select(out=t[:], in_=t[:], pattern=[[-1, 128]],
                                                         compare_op=ALU.is_equal, fill=0.0, base=0,
                                                         channel_multiplier=1), reads=[nm], writes=[nm])

        with ExitStack() as es_x:
            s.loc = es_x
            xTb = s.sb("xTb", [128, 8, NP], BF16)
            for j in (1, 0, 2):
                for kc in range(8):
                    s.dma("pool", lambda e, kc=kc, j=j: e.dma_start(
                        out=xTb[:, kc, j * 2048:(j + 1) * 2048],
                        in_=xT_d[kc * 128:(kc + 1) * 128, j * 2048:(j + 1) * 2048]),
                        writes=[("x", kc, j)])
            XR = [("x", kc, j) for kc in range(8) for j in range(3)]

            if stop_after >= 2:
                with ExitStack() as es_p:
                    s.loc = es_p
                    phase_attn(nc, s, xTb, XR, win_v, ab_d, hb_d, attnT_d)
                    s.barrier()
                    s.emit()
            if stop_after >= 3:
                with ExitStack() as es_p:
                    s.loc = es_p
                    phase_conv(nc, s, xTb, XR, win_v, wdw_d, bdw_d, clg_d, clb_d, convT_d, identb)
                    s.barrier()
                    s.emit()
            if stop_after < 3:
                s.barrier()
                s.emit()
            if stop_after >= 4:
                with ExitStack() as es_p:
                    s.loc = es_p
                    phase_merge(nc, s, xTb, XR, win_v, woa_d, woc_d, attnT_d, convT_d, mrgT_d, xs_d)
                    s.barrier()
                    s.emit()
        if stop_after >= 5:
            with ExitStack() as es_p:
                s.loc = es_p
                phase_out_router(nc, s, mrgT_d, wout_d, xown_d, ln1g_d, ln1b_d, wr_d, br_d, h1_d, xs_d,
                                 identf, idx_all, gk_all, G_all, rt_d)
                s.barrier()
                s.emit()
        if stop_after >= 6:
            with ExitStack() as es_p:
                s.loc = es_p
                phase_experts(nc, s, xs_d, y_d, wgu_d, bgu_d, wd_d, bd_d, identb)
                s.barrier()
                s.emit()
        if stop_after >= 7:
            with ExitStack() as es_p:
                s.loc = es_p
                phase_combine(nc, s, y_d, h1_d, bd_d, ln2g_d, ln2b_d, out_d, identf, idx_all, gk_all, G_all)
                s.barrier()
                s.emit()
    return nc


def phase_attn(nc, s, xTb, XR, win_v, ab_d, hb_d, attnT_d):
    acc = [s.sb(f"acc{h}", [128, 2048], F32) for h in range(2)]
    qT = [[s.sb(f"qT{b}_{h}", [128, 2048], BF16) for h in range(2)] for b in range(2)]
    kT = [s.sb(f"kT{b}", [128, 4096], BF16) for b in range(2)]
    vB = [s.sb(f"vB{b}", [128, 32, 2, 128], BF16) for b in range(2)]
    wq = s.sb("wq", [128, 8, 128], BF16)
    wk = s.sb("wk", [128, 8, 128], BF16)
    wv = s.sb("wv", [128, 8, 128], BF16)
    ab = s.sb("ab", [128, 3, 2, 256], F32)
    abh = s.sb("abh", [128, 3, 2, 128], F32)
    tmp = [s.sb(f"tmp{i}", [128, 512], F32) for i in range(3)]
    pt = [s.sb(f"pt{i}", [128, 512], BF16) for i in range(3)]
    rec = tmp[0][0:64, :]
    hb = s.sb("hb", [128, 1], F32)
    psA = [s.ps(f"psA{i}") for i in range(2)]
    psV = s.ps("psV")
    psS = [s.ps(f"psS{i}") for i in range(3)]
    psO = [s.ps("psO0"), s.ps("psO1")]

    s.dma("sp", lambda e: e.dma_start(out=hb[:], in_=hb_d), writes=["hb"])
    for b in range(2):
        s.op("pool", lambda e, b=b: e.memset(vB[b][:], 1.0), writes=[("v", b, i) for i in range(8)])
        for h in range(2):
            s.op("pool", lambda e, b=b, h=h: e.memset(qT[b][h][:], 0.0), writes=[("q", b, h, tc) for tc in range(4)])

    def xr(lo, hi):
        return [("x", kc, j) for kc in range(8) for j in range(lo // 2048, (hi - 1) // 2048 + 1)]

    cnt = {"pa": 0, "ev": 0, "si": 0}
    iters = [(hp, half, g) for hp in range(4) for half in range(2) for g in range(3)][:DBG["iters"]]

    def make_proj(idx):
        hp, half, g = iters[idx]
        dil = (1, 4, 16)[g]
        halo = 128 * dil
        p0 = HALO + half * 2048
        b = idx % 2
        nK = halo + 2048
        kb0 = p0 - halo
        bpr = 16 // dil + 1
        nblk = dil * bpr
        steps = []

        def loads():
            for (wt, base, nm) in ((wq, 0, "wq"), (wk, 1536, "wk"), (wv, 3072, "wv")):
                c0 = base + g * 512 + hp * 128
                s.dma("pool", lambda e, wt=wt, c0=c0: e.dma_start(out=wt[:], in_=win_v[:, :, c0:c0 + 128]), writes=[nm])
        steps.append(loads)

        def qstep(tc):
            def f():
                pi = cnt["pa"] % 2
                cnt["pa"] += 1
                ps, pkey = psA[pi], ("psA", pi)
                s.mm(ps[:, 0:512], [(wq[:, kc, :], xTb[:, kc, p0 + tc * 512:p0 + (tc + 1) * 512]) for kc in range(8)],
                     reads=xr(p0 + tc * 512, p0 + (tc + 1) * 512) + ["wq"], writes=[pkey])
                _copy(s, "act", qT[b][0][0:64, tc * 512:(tc + 1) * 512], ps[0:64, 0:512], reads=[pkey], writes=[("q", b, 0, tc)])
                _copy(s, "act", qT[b][1][64:128, tc * 512:(tc + 1) * 512], ps[64:128, 0:512], reads=[pkey],
                      writes=[("q", b, 1, tc)])
            return f
        for tc in range(4):
            steps.append(qstep(tc))

        def kstep(off, n, ci):
            def f():
                pi = cnt["pa"] % 2
                cnt["pa"] += 1
                ps, pkey = psA[pi], ("psA", pi)
                s.mm(ps[:, 0:n], [(wk[:, kc, :], xTb[:, kc, kb0 + off:kb0 + off + n]) for kc in range(8)],
                     reads=xr(kb0 + off, kb0 + off + n) + ["wk"], writes=[pkey])
                _copy(s, ("act", "act", "dve")[cnt["ev"] % 3], kT[b][:, off:off + n], ps[:, 0:n], reads=[pkey], writes=[("k", b, ci)])
                cnt["ev"] += 1
            return f
        off = 0
        ci = 0
        while off < nK:
            n = min(512, nK - off)
            steps.append(kstep(off, n, ci))
            off += n
            ci += 1

        def vstep(blk0):
            def f():
                nb = min(4, nblk - blk0)
                for j in range(nb):
                    blk = blk0 + j
                    r, mi = blk // bpr, blk % bpr
                    st = p0 + r + dil * 128 * (mi - 1)
                    s.mm(psV[:, j * 128:(j + 1) * 128],
                         [(xTb[:, kc, st:st + 127 * dil + 1:dil], wv[:, kc, :]) for kc in range(8)],
                         reads=xr(st, st + 127 * dil + 1) + ["wv"], writes=["psV"])
                _copy(s, ("act", "act", "dve")[cnt["ev"] % 3], vB[b][:, blk0:blk0 + nb, :, 0:64],
                      psV[:, 0:nb * 128].rearrange("p (a b c) -> p a b c", a=nb, b=2),
                      reads=["psV"], writes=[("v", b, blk0 // 4)])
                cnt["ev"] += 1
            return f
        for blk0 in range(0, nblk, 4):
            steps.append(vstep(blk0))
        return steps

    def emit_S2(pair, i, b, g, dil, halo, nK, half):
        hh = pair[0][0]
        pS, tm, pT = psS[i], tmp[i], pt[i]
        mms = []
        rd = []
        flags = []
        for ui, (_, r, m) in enumerate(pair):
            q0 = r + dil * 128 * m
            qap = qT[b][hh][:, q0:q0 + 127 * dil + 1:dil]
            kp = halo + r + dil * 128 * (m - 1)
            kc_ = halo + r + dil * 128 * m
            kprev = kT[b][:, kp:kp + 127 * dil + 1:dil]
            kcur = kT[b][:, kc_:kc_ + 127 * dil + 1:dil]
            rd += [("q", b, hh, c) for c in range(q0 // 512, (q0 + 128 * dil - 1) // 512 + 1)]
            rd += [("k", b, c) for c in range(kp // 512, min((kc_ + 128 * dil - 1) // 512, (nK - 1) // 512) + 1)]
            mms.append((pS[:, ui * 256:ui * 256 + 128], kprev, qap))
            mms.append((pS[:, ui * 256 + 128:ui * 256 + 256], kcur, qap))
            flags.append(half == 0 and m == 0)

        def fn(e):
            ins = first = None
            for (o_, l_, r_) in mms:
                ins = e.matmul(o_, lhsT=l_, rhs=r_, start=True, stop=True)
                first = first or ins
            return first, ins
        s.op("pe", fn, reads=list(dict.fromkeys(rd)), writes=[("psS", i)], attach=True)
        tkeys = [("tmp", i, 0), ("tmp", i, 1)]
        if not any(flags):
            in1 = ab[:, g, hh:hh + 1, :].broadcast_to([128, 2, 256])
            s.op("dve", lambda e: e.scalar_tensor_tensor(out=tm[:].rearrange("p (a b) -> p a b", a=2),
                                                         in0=pS[:, 0:512].rearrange("p (a b) -> p a b", a=2), scalar=0.125,
                                                         in1=in1, op0=ALU.mult, op1=ALU.add),
                 reads=[("psS", i), ("ab", g, hh)], writes=tkeys)
        else:
            for ui in range(2):
                c0 = ui * 256
                if flags[ui]:
                    s.op("dve", lambda e, c0=c0: e.scalar_tensor_tensor(out=tm[:, c0:c0 + 128], in0=pS[:, c0:c0 + 128], scalar=0.125,
                                                                        in1=abh[:, g, hh, :], op0=ALU.mult, op1=ALU.add),
                         reads=[("psS", i), ("abh", g, hh)], writes=[("tmp", i, ui)])
                    s.op("dve", lambda e, c0=c0: e.scalar_tensor_tensor(out=tm[:, c0 + 128:c0 + 256], in0=pS[:, c0 + 128:c0 + 256],
                                                                        scalar=0.125, in1=ab[:, g, hh, 128:256], op0=ALU.mult,
                                                                        op1=ALU.add),
                         reads=[("psS", i), ("ab", g, hh), ("tmp", i, ui)], writes=[("tmp", i, ui)])
                else:
                    s.op("dve", lambda e, c0=c0: e.scalar_tensor_tensor(out=tm[:, c0:c0 + 256], in0=pS[:, c0:c0 + 256], scalar=0.125,
                                                                        in1=ab[:, g, hh, :], op0=ALU.mult, op1=ALU.add),
                         reads=[("psS", i), ("ab", g, hh)], writes=[("tmp", i, ui)])
        s.op("act", lambda e: e.activation(out=pT[:], in_=tm[:], func=AF.Exp), reads=tkeys, writes=[("pt", i)])

    def emit_PV2(pair, i, o, b, g, dil, bpr):
        hh = pair[0][0]
        pT = pt[i]
        mms = []
        rd = [("pt", i)]
        for ui, (_, r, m) in enumerate(pair):
            bp = r * bpr + m
            po = psO[o][:, ui * 128:(ui + 1) * 128]
            mms.append((po, vB[b][:, bp, hh, :], pT[:, ui * 256:ui * 256 + 128], True, False))
            mms.append((po, vB[b][:, bp + 1, hh, :], pT[:, ui * 256 + 128:ui * 256 + 256], False, True))
            rd += [("v", b, bp // 4), ("v", b, (bp + 1) // 4)]

        def fn(e):
            ins = first = None
            for (o_, l_, r_, st_, sp_) in mms:
                ins = e.matmul(o_, lhsT=l_, rhs=r_, start=st_, stop=sp_)
                first = first or ins
            return first, ins
        s.op("pe", fn, reads=list(dict.fromkeys(rd)), writes=[("psO", o)], attach=True)
        av = acc[hh][:].rearrange("p (a j d) -> p a d j", a=16 // dil, j=128, d=dil)
        (_, r0_, m0_), (_, r1_, m1_) = pair
        if m1_ != m0_:
            aap = av[:, m0_:m0_ + 2, r0_, :]
        else:
            aap = av[:, m0_, r0_:r0_ + 2, :]
        pin = psO[o][:, 0:256].rearrange("p (a b) -> p a b", a=2)
        if g == 0:
            s.op("dve", lambda e: e.tensor_copy(out=aap, in_=pin), reads=[("psO", o)], writes=[("acc", hh)])
        else:
            s.op("dve", lambda e: e.tensor_tensor(out=aap, in0=pin, in1=aap, op=ALU.add),
                 reads=[("psO", o), ("acc", hh)], writes=[("acc", hh)])

    def load_ab(hp):
        for g2 in range(3):
            for hh in range(2):
                hd = g2 * 8 + hp * 2 + hh
                s.dma("sp", lambda e, g2=g2, hh=hh, hd=hd: e.dma_start(out=ab[:, g2, hh, :], in_=ab_d[hd]),
                      writes=[("ab", g2, hh)])
                s.op("pool", lambda e, g2=g2, hh=hh: e.tensor_scalar(out=abh[:, g2, hh, :], in0=ab[:, g2, hh, 0:128],
                                                                    scalar1=hb[:, 0:1], scalar2=None, op0=ALU.add),
                     reads=[("ab", g2, hh), "hb"], writes=[("abh", g2, hh)])

    load_ab(0)
    for f in make_proj(0):
        f()
    for idx, (hp, half, g) in enumerate(iters):
        dil = (1, 4, 16)[g]
        halo = 128 * dil
        b = idx % 2
        nK = halo + 2048
        bpr = 16 // dil + 1
        nxt = make_proj(idx + 1) if idx + 1 < len(iters) else []
        units = [(hh, r, m) for hh in range(2) for r in range(dil) for m in range(16 // dil)]
        pairs = [(units[k], units[k + 1]) for k in range(0, len(units), 2)]
        pend = []
        for pi2, pr in enumerate(pairs):
            i = cnt["si"] % 3
            cnt["si"] += 1
            emit_S2(pr, i, b, g, dil, halo, nK, half)
            pend.append((pr, i, pi2 % 2))
            if len(pend) > 2:
                pp, pi_, po_ = pend.pop(0)
                emit_PV2(pp, pi_, po_, b, g, dil, bpr)
            if nxt and pi2 == 0:
                nxt.pop(0)()
            if pi2 >= 5:
                for _ in range(2):
                    if nxt:
                        nxt.pop(0)()
        while pend:
            pp, pi_, po_ = pend.pop(0)
            emit_PV2(pp, pi_, po_, b, g, dil, bpr)
        if idx + 1 < len(iters) and iters[idx + 1][0] != hp:
            load_ab(iters[idx + 1][0])
        while nxt:
            nxt.pop(0)()
        if g == 2:
            for hh in range(2):
                for c in range(4):
                    cs = slice(c * 512, (c + 1) * 512)
                    s.op("dve", lambda e, hh=hh, cs=cs: e.tensor_copy(out=rec, in_=acc[hh][64:128, cs]),
                         reads=[("acc", hh)], writes=[("tmp", 0, 0), ("tmp", 0, 1)])
                    s.op("act", lambda e: e.activation(out=rec, in_=rec, func=AF.Ln),
                         reads=[("tmp", 0, 0), ("tmp", 0, 1)], writes=[("tmp", 0, 0), ("tmp", 0, 1)])
                    s.op("act", lambda e: e.activation(out=rec, in_=rec, func=AF.Exp, scale=-1.0),
                         reads=[("tmp", 0, 0), ("tmp", 0, 1)], writes=[("tmp", 0, 0), ("tmp", 0, 1)])
                    s.op("dve", lambda e, hh=hh, cs=cs: e.tensor_tensor(out=acc[hh][0:64, cs], in0=acc[hh][0:64, cs], in1=rec,
                                                                        op=ALU.mult),
                         reads=[("tmp", 0, 0), ("tmp", 0, 1), ("acc", hh)], writes=[("acc", hh)])
                row = (hp * 2 + hh) * 64
                s.dma("pool", lambda e, hh=hh, row=row, half=half: e.dma_start(
                    out=attnT_d[row:row + 64, half * 2048:(half + 1) * 2048], in_=acc[hh][0:64, :]),
                    reads=[("acc", hh)], writes=[("attnT_d", row, half)])


def phase_conv(nc, s, xTb, XR, win_v, wdw_d, bdw_d, clg_d, clb_d, convT_d, identb):
    dw = s.sb("dw", [128, 6, 2048], F32)
    glu = [s.sb(f"glu{i}", [128, 32 + 2048], BF16) for i in range(2)]
    dg = [s.sb(f"dg{i}", [128, 31, 128], BF16) for i in range(2)]
    wu = [s.sb(f"wu{i}", [128, 8, 128], BF16) for i in range(2)]
    wg = [s.sb(f"wg{i}", [128, 8, 128], BF16) for i in range(2)]
    sgt = [s.sb(f"sgt{i}", [128, 512], F32) for i in range(2)]
    sq = [s.sb(f"sq{i}", [128, 512], F32) for i in range(2)]
    sd = s.sb("sd", [128, 512], F32)
    rstd = s.sb("rstd", [128, 512], F32)
    cst = [s.sb(f"cst{i}", [128, 6, 512], BF16) for i in range(2)]
    wdw = s.sb("wdw", [128, 6, 31], F32)
    bdw = s.sb("bdw", [128, 6], F32)
    clg = s.sb("clg", [128, 6], F32)
    clb = s.sb("clb", [128, 6], F32)
    onesf = s.sb("onesf", [128, 128], F32)
    psU = [s.ps(f"psU{i}") for i in range(2)]
    psG = [s.ps(f"psG{i}") for i in range(2)]
    psM = s.ps("psM")
    psV2 = s.ps("psV2")
    psC = [s.ps("psC0"), s.ps("psC1")]
    for t, d_, nm in ((wdw, wdw_d, "wdw"), (bdw, bdw_d, "bdw"), (clg, clg_d, "clg"), (clb, clb_d, "clb")):
        s.dma("sp", lambda e, t=t, d_=d_: e.dma_start(out=t[:], in_=d_), writes=[nm])
    s.op("pool", lambda e: e.memset(onesf[:], 1.0 / 768.0), writes=["onesf"])
    convT_v = convT_d.rearrange("(cc p) t -> p cc t", p=128)
    it = 0
    pu = 0
    ci = 0
    for half in range(2):
        p0 = HALO + half * 2048
        for cc in range(6):
            b = it % 2
            it += 1
            cv = 4608 + cc * 128
            cg = 4608 + 768 + cc * 128
            s.dma("pool", lambda e, b=b, cv=cv: e.dma_start(out=wu[b][:], in_=win_v[:, :, cv:cv + 128]), writes=[("wu", b)])
            s.dma("pool", lambda e, b=b, cg=cg: e.dma_start(out=wg[b][:], in_=win_v[:, :, cg:cg + 128]), writes=[("wg", b)])
            for (off, n) in [(0, 32)] + [(32 + i * 512, 512) for i in range(4)]:
                pi = pu % 2
                pu += 1
                t0 = p0 - 32 + off
                s.mm(psU[pi][:, 0:n], [(wu[b][:, kc, :], xTb[:, kc, t0:t0 + n]) for kc in range(8)],
                     reads=XR + [("wu", b)], writes=[("psU", pi)])
                s.mm(psG[pi][:, 0:n], [(wg[b][:, kc, :], xTb[:, kc, t0:t0 + n]) for kc in range(8)],
                     reads=XR + [("wg", b)], writes=[("psG", pi)])
                s.op("act", lambda e, pi=pi, n=n: e.activation(out=sgt[pi][:, 0:n], in_=psG[pi][:, 0:n], func=AF.Sigmoid),
                     reads=[("psG", pi)], writes=[("sgt", pi)])
                s.op("dve", lambda e, pi=pi, n=n, off=off, b=b: e.tensor_tensor(out=glu[b][:, off:off + n], in0=psU[pi][:, 0:n],
                                                                                in1=sgt[pi][:, 0:n], op=ALU.mult),
                     reads=[("psU", pi), ("sgt", pi)], writes=[("glu", b)])
            for j in range(31):
                s.op("dve", lambda e, b=b, cc=cc, j=j: e.tensor_scalar(out=dg[b][:, j, :], in0=identb[:], scalar1=wdw[:, cc, j:j + 1],
                                                                       scalar2=None, op0=ALU.mult),
                     reads=["identb", "wdw"], writes=[("dg", b)])
            for tc in range(4):
                pc = (it * 4 + tc) % 2
                s.mm(psC[pc][:, :], [(dg[b][:, j, :], glu[b][:, 2 + j + tc * 512:2 + j + (tc + 1) * 512]) for j in range(31)],
                     reads=[("dg", b), ("glu", b)], writes=[("psC", pc)])
                s.op("dve", lambda e, cc=cc, tc=tc, pc=pc: e.tensor_scalar(out=dw[:, cc, tc * 512:(tc + 1) * 512], in0=psC[pc][:, :],
                                                                          scalar1=bdw[:, cc:cc + 1], scalar2=None, op0=ALU.add),
                     reads=[("psC", pc), "bdw"], writes=[("dw", cc)])
        for tc in range(4):
            ts_ = slice(tc * 512, (tc + 1) * 512)
            s.mm(psM[:, :], [(onesf[:], dw[:, cc, ts_]) for cc in range(6)], reads=[("dw", cc) for cc in range(6)] + ["onesf"],
                 writes=["psM"])
            for cc in range(6):
                s.op("dve", lambda e, cc=cc, ts_=ts_: e.tensor_tensor(out=dw[:, cc, ts_], in0=dw[:, cc, ts_], in1=psM[:, :],
                                                                      op=ALU.subtract),
                     reads=["psM", ("dw", cc)], writes=[("dw", cc)])
            sqt = []
            for cc in range(6):
                qi = cc % 2
                s.op("act", lambda e, cc=cc, qi=qi, ts_=ts_: e.activation(out=sq[qi][:], in_=dw[:, cc, ts_], func=AF.Square),
                     reads=[("dw", cc)], writes=[("sq", qi)])
                def fn(e, cc=cc, qi=qi):
                    return e.matmul(psV2[:, :], lhsT=onesf[:], rhs=sq[qi][:], start=(cc == 0), stop=(cc == 5))
                s.op("pe", fn, reads=[("sq", qi), "onesf"], writes=["psV2"], attach=True)
            s.op("act", lambda e: e.activation(out=sd[:], in_=psV2[:, :], func=AF.Sqrt, bias=EPS), reads=["psV2"], writes=["sd"])
            s.op("dve", lambda e: e.reciprocal(out=rstd[:], in_=sd[:]), reads=["sd"], writes=["rstd"])
            cb = ci % 2
            ci += 1
            for cc in range(6):
                s.op("dve", lambda e, cc=cc, ts_=ts_: e.tensor_tensor(out=dw[:, cc, ts_], in0=dw[:, cc, ts_], in1=rstd[:],
                                                                      op=ALU.mult),
                     reads=["rstd", ("dw", cc)], writes=[("dw", cc)])
                s.op("dve", lambda e, cc=cc, ts_=ts_: e.tensor_scalar(out=dw[:, cc, ts_], in0=dw[:, cc, ts_], scalar1=clg[:, cc:cc + 1],
                                                                      scalar2=clb[:, cc:cc + 1], op0=ALU.mult, op1=ALU.add),
                     reads=[("dw", cc), "clg", "clb"], writes=[("dw", cc)])
                s.op("act", lambda e, cc=cc, ts_=ts_, cb=cb: e.activation(out=cst[cb][:, cc, :], in_=dw[:, cc, ts_], func=AF.Silu),
                     reads=[("dw", cc)], writes=[("cst", cb)])
            t0 = half * 2048 + tc * 512
            s.dma("sp", lambda e, cb=cb, t0=t0: e.dma_start(out=convT_v[:, :, t0:t0 + 512], in_=cst[cb][:]),
                  reads=[("cst", cb)], writes=[("convT_d", t0)])


def phase_merge(nc, s, xTb, XR, win_v, woa_d, woc_d, attnT_d, convT_d, mrgT_d, xs_d):
    woa = s.sb("woa", [128, 4, D], BF16)
    woc = s.sb("woc", [128, 6, D], BF16)
    wga = s.sb("wga", [128, 8, D], BF16)
    wgc = s.sb("wgc", [128, 8, D], BF16)
    at = [s.sb(f"at{i}", [128, 4, 512], BF16) for i in range(2)]
    cv = [s.sb(f"cv{i}", [128, 6, 512], BF16) for i in range(1)]
    mg = [s.sb(f"mg{i}", [128, 8, 512], BF16) for i in range(1)]
    sga = [s.sb(f"sga{i}", [128, 512], F32) for i in range(2)]
    sgc = [s.sb(f"sgc{i}", [128, 512], F32) for i in range(2)]
    psa = [s.ps(f"psa{i}") for i in range(2)]
    psc = [s.ps(f"psc{i}") for i in range(2)]
    psga = [s.ps(f"psga{i}") for i in range(2)]
    psgc = [s.ps(f"psgc{i}") for i in range(2)]
    for kc in range(8):
        s.dma("pool", lambda e, kc=kc: e.dma_start(out=wga[:, kc, :], in_=win_v[:, kc, 6144:7168]), writes=[("wga", kc)])
    for kc in range(8):
        s.dma("pool", lambda e, kc=kc: e.dma_start(out=wgc[:, kc, :], in_=win_v[:, kc, 7168:8192]), writes=[("wgc", kc)])
    s.dma("pool", lambda e: e.dma_start(out=woa[:], in_=woa_d.rearrange("(h p) n -> p h n", p=128)), writes=["woa"])
    s.dma("pool", lambda e: e.dma_start(out=woc[:], in_=woc_d.rearrange("(c p) n -> p c n", p=128)), writes=["woc"])
    zt = s.sb("zt", [128, D], BF16)
    s.op("dve", lambda e: e.memset(zt[:], 0.0), writes=["zt"])
    xs_z = xs_d.rearrange("(p j) d -> p j d", p=128)
    nrow = (NE * CAP + 128) // 128
    zq = list(range(nrow))

    def zero_some(n):
        for _ in range(n):
            if zq:
                c = zq.pop(0)
                s.dma("act", lambda e, c=c: e.dma_start(out=xs_z[:, c, :], in_=zt[:]), reads=["zt"], writes=[("xs_zero", c)])
    WGA = [("wga", kc) for kc in range(8)]
    WGC = [("wgc", kc) for kc in range(8)]
    attn_v = attnT_d.rearrange("(h p) t -> p h t", p=128)
    conv_v = convT_d.rearrange("(c p) t -> p c t", p=128)
    mrg_v = mrgT_d.rearrange("(c p) t -> p c t", p=128)
    pi = 0
    for tc in range(8):
        b = tc % 2
        ts_ = slice(tc * 512, (tc + 1) * 512)
        xs_ = slice(HALO + tc * 512, HALO + (tc + 1) * 512)
        s.dma("sp", lambda e, b=b, ts_=ts_: e.dma_start(out=at[b][:], in_=attn_v[:, :, ts_]), writes=[("at", b)])
        s.dma("sp", lambda e, ts_=ts_: e.dma_start(out=cv[0][:], in_=conv_v[:, :, ts_]), writes=[("cv", 0)])
        for fc in range(8):
            fs = slice(fc * 128, (fc + 1) * 128)
            p = pi % 2
            pi += 1
            s.mm(psga[p][:, :], [(wga[:, kc, fs], xTb[:, kc, xs_]) for kc in range(8)], reads=XR + WGA, writes=[("psga", p)])
            s.mm(psgc[p][:, :], [(wgc[:, kc, fs], xTb[:, kc, xs_]) for kc in range(8)], reads=XR + WGC, writes=[("psgc", p)])
            s.mm(psa[p][:, :], [(woa[:, h, fs], at[b][:, h, :]) for h in range(4)], reads=["woa", ("at", b)],
                 writes=[("psa", p)])
            s.mm(psc[p][:, :], [(woc[:, c, fs], cv[0][:, c, :]) for c in range(6)], reads=["woc", ("cv", 0)],
                 writes=[("psc", p)])
            s.op("act", lambda e, p=p: e.activation(out=sga[p][:], in_=psga[p][:, :], func=AF.Sigmoid),
                 reads=[("psga", p)], writes=[("sga", p)])
            s.op("act", lambda e, p=p: e.activation(out=sgc[p][:], in_=psgc[p][:, :], func=AF.Sigmoid),
                 reads=[("psgc", p)], writes=[("sgc", p)])
            s.op("dve", lambda e, p=p: e.tensor_tensor(out=sga[p][:], in0=psa[p][:, :], in1=sga[p][:], op=ALU.mult),
                 reads=[("psa", p), ("sga", p)], writes=[("sga", p)])
            s.op("dve", lambda e, p=p: e.tensor_tensor(out=sgc[p][:], in0=psc[p][:, :], in1=sgc[p][:], op=ALU.mult),
                 reads=[("psc", p), ("sgc", p)], writes=[("sgc", p)])
            s.op("dve", lambda e, p=p, fc=fc: e.tensor_tensor(out=mg[0][:, fc, :], in0=sga[p][:], in1=sgc[p][:], op=ALU.add),
                 reads=[("sga", p), ("sgc", p)], writes=[("mg", 0)])
            zero_some(3)
        s.dma("sp", lambda e, ts_=ts_: e.dma_start(out=mrg_v[:, :, ts_], in_=mg[0][:]), reads=[("mg", 0)],
              writes=[("mrg_d", tc)])
    zero_some(len(zq))


def phase_out_router(nc, s, mrgT_d, wout_d, xown_d, ln1g_d, ln1b_d, wr_d, br_d, h1_d, xs_d,
                     identf, idx_all, gk_all, G_all, rt_d):
    wout = s.sb("wout", [128, 8, D], BF16)
    lng = s.sb("lng", [128, D], F32)
    lnb = s.sb("lnb", [128, D], F32)
    wr = s.sb("wr", [128, 8, NE], F32)
    brb = s.sb("brb", [128, NE], F32)
    mg = [s.sb(f"mgc{i}", [128, 8, 512], BF16) for i in range(2)]
    xo = [s.sb(f"xo{i}", [128, D], F32) for i in range(2)]
    z = [s.sb(f"z{i}", [128, D], F32) for i in range(2)]
    h1 = [s.sb(f"h1{i}", [128, D], F32) for i in range(2)]
    h1b = [s.sb(f"h1b{i}", [128, D], BF16) for i in range(2)]
    h1T = [s.sb(f"h1T{i}", [128, 8, 128], F32) for i in range(2)]

    def two(name, shape, dt=F32):
        return [s.sb(f"{name}{i}", shape, dt) for i in range(2)]
    st6 = two("st6", [128, 2, 6])
    mv = two("mv", [128, 2])
    rs = two("rs", [128, 1])
    lg = two("lg", [128, NE])
    m8 = two("m8", [128, 8])
    selm = two("selm", [128, NE])
    selb = two("selb", [128, NE], BF16)
    ex = two("ex", [128, NE])
    den = two("den", [128, 1])
    rden = two("rden", [128, 1])
    pos = two("pos", [128, NE])
    key = two("key", [128, NE])
    k8 = two("k8", [128, 8])
    junk = two("junk", [128, NE])
    run = s.sb("run", [128, NE], F32)
    ustr = s.sb("ustr", [128, 128], BF16)
    onesb = s.sb("onesb", [128, 128], BF16)
    rtst = s.sb("rtst", [128, 32, 8], F32)
    pso = [[s.ps(f"pso{i}_{h}") for h in range(2)] for i in range(2)]
    pst = [s.ps(f"pst{i}") for i in range(2)]
    pslp = [s.ps(f"pslp{i}") for i in range(2)]

    for kc in range(8):
        s.dma("pool", lambda e, kc=kc: e.dma_start(out=wout[:, kc, :], in_=wout_d[kc * 128:(kc + 1) * 128, :]),
              writes=[("wout", kc)])
    WO = [("wout", kc) for kc in range(8)]
    s.dma("sp", lambda e: e.dma_start(out=lng[:], in_=ln1g_d.broadcast_to([128, D])), writes=["lng"])
    s.dma("sp", lambda e: e.dma_start(out=lnb[:], in_=ln1b_d.broadcast_to([128, D])), writes=["lnb"])
    s.dma("sp", lambda e: e.dma_start(out=wr[:], in_=wr_d.rearrange("(kc p) n -> p kc n", p=128)), writes=["wr"])
    s.dma("sp", lambda e: e.dma_start(out=brb[:], in_=br_d.broadcast_to([128, NE])), writes=["brb"])
    s.op("pool", lambda e: e.memset(ustr[:], 1.0), writes=["ustr"])
    s.op("pool", lambda e: e.affine_select(out=ustr[:], in_=ustr[:], pattern=[[1, 128]], compare_op=ALU.is_gt, fill=0.0,
                                           base=0, channel_multiplier=-1), reads=["ustr"], writes=["ustr"])
    s.op("pool", lambda e: e.memset(onesb[:], 1.0), writes=["onesb"])
    s.op("pool", lambda e: e.iota(run[:], pattern=[[CAP, NE]], base=1, channel_multiplier=0,
                                  allow_small_or_imprecise_dtypes=True), writes=["run"])
    mrg_v = mrgT_d.rearrange("(c p) t -> p c t", p=128)

    def stage_a(ti):
        tc, tt = ti // 4, ti % 4
        b = tc % 2
        tb = ti % 2
        r0 = ti * 128
        if tt == 0:
            ts_ = slice(tc * 512, (tc + 1) * 512)
            s.dma("sp", lambda e: e.dma_start(out=mg[b][:], in_=mrg_v[:, :, ts_]), writes=[("mgc", b)])
        s.dma("sp", lambda e: e.dma_start(out=xo[tb][:], in_=xown_d[r0:r0 + 128, :]), writes=[("xo", tb)])
        for hf in range(2):
            hs = slice(hf * 512, (hf + 1) * 512)
            s.mm(pso[tb][hf][:, :], [(mg[b][:, kc, tt * 128:(tt + 1) * 128], wout[:, kc, hs]) for kc in range(8)],
                 reads=[("mgc", b)] + WO, writes=[("pso", tb, hf)])
            s.op("dve", lambda e, hf=hf, hs=hs: e.scalar_tensor_tensor(out=z[tb][:, hs], in0=xo[tb][:, hs], scalar=ALPHA,
                                                                       in1=pso[tb][hf][:, :], op0=ALU.mult, op1=ALU.add),
                 reads=[("xo", tb), ("pso", tb, hf)], writes=[("z", tb, hf)])
            s.op("dve", lambda e, hf=hf, hs=hs: e.bn_stats(out=st6[tb][:, hf, :], in_=z[tb][:, hs]),
                 reads=[("z", tb, hf)], writes=[("st6", tb, hf)])
        _ln_tail(s, z[tb], [("z", tb, 0), ("z", tb, 1)], st6[tb], [("st6", tb, 0), ("st6", tb, 1)], mv[tb], rs[tb],
                 lng, lnb, h1[tb], ("h1", tb), f"r{tb}")
        s.dma("sp", lambda e: e.dma_start(out=h1_d[r0:r0 + 128, :], in_=h1[tb][:]), reads=[("h1", tb)], writes=[("h1_d", ti)])
        s.op("act", lambda e: e.activation(out=h1b[tb][:], in_=h1[tb][:], func=AF.Identity), reads=[("h1", tb)],
             writes=[("h1b", tb)])
        for hf in range(2):
            def fn(e, hf=hf):
                ins = None
                for k in range(4):
                    kc = hf * 4 + k
                    ins = e.transpose(out=pst[hf][:, k * 128:(k + 1) * 128], in_=h1[tb][:, kc * 128:(kc + 1) * 128],
                                      identity=identf[:])
                return ins
            s.op("pe", fn, reads=[("h1", tb), "identf"], writes=[("pst", hf)])
            _copy(s, "act", h1T[tb][:, hf * 4:(hf + 1) * 4, :], pst[hf][:, :].rearrange("p (a b) -> p a b", a=4),
                  reads=[("pst", hf)], writes=[("h1T", tb, hf)])

    def stage_b(ti):
        tb = ti % 2
        P = pslp[tb]
        pk = ("pslp", tb)
        lg_, m8_, sel_, selb_, ex_, den_, rden_, pos_, key_, k8_, junk_ = (lg[tb], m8[tb], selm[tb], selb[tb], ex[tb], den[tb],
                                                                          rden[tb], pos[tb], key[tb], k8[tb], junk[tb])
        T = lambda n: (n, tb)
        s.mm(P[:, 0:NE], [(h1T[tb][:, kc, :], wr[:, kc, :]) for kc in range(8)], reads=[("h1T", tb, 0), ("h1T", tb, 1), "wr"],
             writes=[pk])
        s.op("dve", lambda e: e.tensor_tensor(out=lg_[:], in0=P[:, 0:NE], in1=brb[:], op=ALU.add), reads=[pk, "brb"],
             writes=[T("lg")])
        s.op("dve", lambda e: e.max(out=m8_[:], in_=lg_[:]), reads=[T("lg")], writes=[T("m8")])
        s.op("dve", lambda e: e.tensor_scalar(out=sel_[:], in0=lg_[:], scalar1=m8_[:, 3:4], scalar2=None, op0=ALU.is_ge),
             reads=[T("lg"), T("m8")], writes=[T("selm")])
        s.op("dve", lambda e: e.tensor_scalar(out=ex_[:], in0=lg_[:], scalar1=m8_[:, 0:1], scalar2=None, op0=ALU.subtract),
             reads=[T("lg"), T("m8")], writes=[T("ex")])
        s.op("act", lambda e: e.activation(out=ex_[:], in_=ex_[:], func=AF.Exp), reads=[T("ex")], writes=[T("ex")])
        s.op("dve", lambda e: e.scalar_tensor_tensor(out=ex_[:], in0=ex_[:], scalar=1.0, in1=sel_[:], op0=ALU.mult,
                                                     op1=ALU.mult, accum_out=den_[:]),
             reads=[T("ex"), T("selm")], writes=[T("ex"), T("den")])
        s.op("dve", lambda e: e.reciprocal(out=rden_[:], in_=den_[:]), reads=[T("den")], writes=[T("rden")])
        s.op("dve", lambda e: e.tensor_scalar(out=G_all[:, ti, :], in0=ex_[:], scalar1=rden_[:, 0:1], scalar2=None,
                                              op0=ALU.mult),
             reads=[T("ex"), T("rden")], writes=[("G", ti)])
        s.op("act", lambda e: e.activation(out=selb_[:], in_=sel_[:], func=AF.Identity), reads=[T("selm")], writes=[T("selb")])

        def fnp(e):
            e.matmul(P[:, 64:64 + NE], lhsT=ustr[:], rhs=selb_[:], start=True, stop=True)
            return e.matmul(P[:, 128:128 + NE], lhsT=onesb[:], rhs=selb_[:], start=True, stop=True)
        s.op("pe", fnp, reads=["ustr", "onesb", T("selb"), T("lg")], writes=[pk])
        s.op("dve", lambda e: e.tensor_tensor(out=pos_[:], in0=P[:, 64:64 + NE], in1=run[:], op=ALU.add),
             reads=[pk, "run"], writes=[T("pos")])
        s.op("dve", lambda e: e.tensor_tensor(out=run[:], in0=P[:, 128:128 + NE], in1=run[:], op=ALU.add),
             reads=[pk, "run", T("pos")], writes=["run"])
        s.op("dve", lambda e: e.tensor_tensor(out=key_[:], in0=pos_[:], in1=sel_[:], op=ALU.mult),
             reads=[T("pos"), T("selm")], writes=[T("key")])
        s.op("dve", lambda e: e.max(out=k8_[:], in_=key_[:]), reads=[T("key")], writes=[T("k8")])
        s.op("dve", lambda e: e.tensor_scalar(out=idx_all[:, ti * 4:ti * 4 + 4], in0=k8_[:, 0:4], scalar1=-1.0, scalar2=None,
                                              op0=ALU.add),
             reads=[T("k8")], writes=[("idx", ti)])
        for k in range(4):
            s.op("dve", lambda e, k=k: e.scalar_tensor_tensor(out=junk_[:], in0=key_[:], scalar=k8_[:, k:k + 1],
                                                              in1=G_all[:, ti, :], op0=ALU.is_equal, op1=ALU.mult,
                                                              accum_out=gk_all[:, ti, k:k + 1]),
                 reads=[T("key"), T("k8"), ("G", ti), T("junk")], writes=[T("junk"), ("gk", ti, k)])
        s.op("dve", lambda e: e.tensor_copy(out=rtst[:, ti, 0:4], in_=k8_[:, 0:4]), reads=[T("k8")], writes=[("rt", ti, 0)])
        s.op("dve", lambda e: e.tensor_copy(out=rtst[:, ti, 4:8], in_=gk_all[:, ti, :]),
             reads=[("gk", ti, k) for k in range(4)], writes=[("rt", ti, 1)])
        for k in range(4):
            s.dma("pool", lambda e, k=k: e.indirect_dma_start(
                out=xs_d, out_offset=bass.IndirectOffsetOnAxis(ap=idx_all[:, ti * 4 + k:ti * 4 + k + 1], axis=0),
                in_=h1b[tb][:, :], in_offset=None, bounds_check=_bc(s, e), oob_is_err=False),
                reads=[("h1b", tb), ("idx", ti)], writes=[("xs_d", ti, k)])

    stage_a(0)
    for ti in range(32):
        if ti + 1 < 32:
            stage_a(ti + 1)
        stage_b(ti)
    s.dma("sp", lambda e: e.dma_start(out=rt_d, in_=rtst[:]), reads=[("rt", ti, j) for ti in range(32) for j in range(2)],
          writes=["rt_d"])


def _ln_tail(s, z, zkeys, st6, skeys, mv, rs, lng, lnb, out, okey, tag):
    s.op("dve", lambda e: e.bn_aggr(out=mv[:], in_=st6[:].rearrange("p a b -> p (a b)")), reads=skeys, writes=["mv" + tag])
    s.op("act", lambda e: e.activation(out=rs[:], in_=mv[:, 1:2], func=AF.Ln, bias=EPS), reads=["mv" + tag], writes=["rs0" + tag])
    s.op("act", lambda e: e.activation(out=rs[:], in_=rs[:], func=AF.Exp, scale=-0.5), reads=["rs0" + tag], writes=["rs" + tag])
    s.op("dve", lambda e: e.tensor_scalar(out=z[:], in0=z[:], scalar1=mv[:, 0:1], scalar2=rs[:, 0:1], op0=ALU.subtract,
                                          op1=ALU.mult),
         reads=zkeys + ["mv" + tag, "rs" + tag], writes=zkeys)
    s.op("dve", lambda e: e.tensor_tensor(out=z[:], in0=z[:], in1=lng[:], op=ALU.mult), reads=zkeys + ["lng"], writes=zkeys)
    s.op("dve", lambda e: e.tensor_tensor(out=out[:], in0=z[:], in1=lnb[:], op=ALU.add), reads=zkeys + ["lnb"], writes=[okey])


def phase_experts(nc, s, xs_d, y_d, wgu_d, bgu_d, wd_d, bd_d, identb):
    wgu = [s.sb(f"wgu{i}", [128, 8, 2048], BF16) for i in range(2)]
    wdn = [s.sb(f"wdn{i}", [128, 8, D], BF16) for i in range(2)]
    bgu = [s.sb(f"bgu{i}", [128, 16], F32) for i in range(2)]
    bdb = [s.sb(f"bdb{i}", [128, D], F32) for i in range(2)]
    bgu1 = [s.sb(f"bgu1_{i}", [128, 8], F32) for i in range(2)]
    xs = [s.sb(f"xs{i}", [128, 6, D], BF16) for i in range(2)]
    xTe = s.sb("xTe", [128, 8, CAPT], BF16)
    aT = s.sb("aT", [128, 8, CAPT], BF16)
    NH = CAP // 2
    gt = [s.sb(f"gt{i}", [128, NH], F32) for i in range(2)]
    sg = [s.sb(f"sg{i}", [128, NH], F32) for i in range(2)]
    ut = [s.sb(f"ut{i}", [128, NH], F32) for i in range(2)]
    yst = [s.sb(f"yst{i}", [128, D], F32) for i in range(2)]
    NST = 4
    stg = [s.sb(f"stg{i}", [128, 2048], F32) for i in range(NST)]
    pstr = [s.ps(f"pstr{i}", [128, 1024], BF16) for i in range(2)]
    psg = [s.ps(f"psg{i}") for i in range(2)]
    psu = [s.ps(f"psu{i}") for i in range(2)]
    psy = [s.ps(f"psy{i}") for i in range(2)]

    chunks = []
    for ex in range(NE):
        chunks += [(ex, 0, kc) for kc in range(8)] + [(ex, 1, kc) for kc in range(8)]
    st = {"dma": 0, "cast": 0}

    def emit_dma():
        c = st["dma"]
        if c >= len(chunks):
            return
        st["dma"] = c + 1
        ex, kind, kc = chunks[c]
        t = c % NST
        if kind == 0:
            s.dma("sp", lambda e: e.dma_start(out=stg[t][:, :], in_=wgu_d[ex, kc * 128:(kc + 1) * 128, :]), writes=[("stg", t)])
        else:
            s.dma("sp", lambda e: e.dma_start(out=stg[t][:, 0:D], in_=wd_d[ex, kc * 128:(kc + 1) * 128, :]), writes=[("stg", t)])

    def pump():
        c = st["cast"]
        if c >= len(chunks):
            return
        while st["dma"] < min(c + NST, len(chunks)):
            emit_dma()
        st["cast"] = c + 1
        ex, kind, kc = chunks[c]
        t = c % NST
        b = ex % 2
        if kind == 0:
            s.op("act", lambda e: e.activation(out=wgu[b][:, kc, :], in_=stg[t][:, :], func=AF.Identity), reads=[("stg", t)],
                 writes=[("wgu", b, kc)])
        else:
            s.op("act", lambda e: e.activation(out=wdn[b][:, kc, :], in_=stg[t][:, 0:D], func=AF.Identity), reads=[("stg", t)],
                 writes=[("wdn", b, kc)])

    def small_loads(ex):
        b = ex % 2
        s.dma("sp", lambda e: e.dma_start(out=bgu[b][:], in_=bgu_d[ex]), writes=[("bgu", b)])
        s.op("dve", lambda e: e.tensor_scalar(out=bgu1[b][:], in0=bgu[b][:, 8:16], scalar1=1.0, scalar2=None, op0=ALU.add),
             reads=[("bgu", b)], writes=[("bgu1", b)])
        s.dma("sp", lambda e: e.dma_start(out=bdb[b][:], in_=bd_d[ex:ex + 1, :].broadcast_to([128, D])), writes=[("bdb", b)])
        s.dma("sp", lambda e: e.dma_start(
            out=xs[b][:], in_=xs_d[ex * CAP:ex * CAP + CAPT, :].rearrange("(j p) d -> p j d", p=128)), writes=[("xs", b)])

    s.op("pool", lambda e: e.memset(aT[:], 0.0), writes=[("aT", fc, h) for fc in range(8) for h in range(2)])
    small_loads(0)
    for _ in range(16):
        pump()
    ti_ = 0
    gi = 0
    yi = 0
    for ex in range(NE):
        b = ex % 2
        if ex + 1 < NE:
            small_loads(ex + 1)
        WGU = [("wgu", b, kc) for kc in range(8)]
        WDN = [("wdn", b, kc) for kc in range(8)]
        for j in range(6):
            p = ti_ % 2
            ti_ += 1

            def fn(e, p=p, j=j, b=b):
                ins = None
                for kc in range(8):
                    ins = e.transpose(out=pstr[p][:, kc * 128:(kc + 1) * 128], in_=xs[b][:, j, kc * 128:(kc + 1) * 128],
                                      identity=identb[:])
                return ins
            s.op("pe", fn, reads=[("xs", b), "identb"], writes=[("pstr", p)])
            _copy(s, "act", xTe[:, :, j * 128:(j + 1) * 128], pstr[p][:, :].rearrange("p (a b) -> p a b", a=8),
                  reads=[("pstr", p)], writes=[("xTe", j)])
        for fcp in range(8):
            for nh in range(2):
                p = gi % 2
                gi += 1
                ns = slice(nh * NH, (nh + 1) * NH)
                xk = [("xTe", j) for j in ((0, 1, 2) if nh == 0 else (2, 3, 4, 5))]
                s.mm(psg[p][:, 0:NH], [(wgu[b][:, kc, fcp * 128:(fcp + 1) * 128], xTe[:, kc, ns]) for kc in range(8)],
                     reads=WGU + xk, writes=[("psg", p)])
                s.mm(psu[p][:, 0:NH], [(wgu[b][:, kc, D + fcp * 128:D + (fcp + 1) * 128], xTe[:, kc, ns]) for kc in range(8)],
                     reads=WGU + xk, writes=[("psu", p)])
                s.op("dve", lambda e, p=p, b=b, fcp=fcp: e.tensor_scalar(out=gt[p][:], in0=psg[p][:, 0:NH],
                                                                        scalar1=bgu[b][:, fcp:fcp + 1], scalar2=7.0,
                                                                        op0=ALU.add, op1=ALU.min),
                     reads=[("psg", p), ("bgu", b)], writes=[("gt", p)])
                s.op("act", lambda e, p=p: e.activation(out=sg[p][:], in_=gt[p][:], func=AF.Sigmoid, scale=1.702),
                     reads=[("gt", p)], writes=[("sg", p)])
                pump()
                s.op("dve", lambda e, p=p, b=b, fcp=fcp: e.tensor_scalar(out=ut[p][:], in0=psu[p][:, 0:NH],
                                                                        scalar1=bgu1[b][:, fcp:fcp + 1], scalar2=8.0,
                                                                        op0=ALU.add, op1=ALU.min),
                     reads=[("psu", p), ("bgu1", b)], writes=[("ut", p)])
                s.op("dve", lambda e, p=p: e.scalar_tensor_tensor(out=ut[p][:], in0=ut[p][:], scalar=-6.0, in1=gt[p][:],
                                                                  op0=ALU.max, op1=ALU.mult),
                     reads=[("ut", p), ("gt", p)], writes=[("ut", p)])
                s.op("dve", lambda e, p=p, fcp=fcp, ns=ns: e.tensor_tensor(out=aT[:, fcp, ns], in0=ut[p][:], in1=sg[p][:],
                                                                           op=ALU.mult),
                     reads=[("sg", p), ("ut", p)], writes=[("aT", fcp, nh)])
        for j in range(6):
            yb = yi % 2
            yi += 1
            for dh in range(2):
                s.mm(psy[dh][:, :], [(aT[:, fc, j * 128:(j + 1) * 128], wdn[b][:, fc, dh * 512:(dh + 1) * 512]) for fc in range(8)],
                     reads=WDN + [("aT", fc, h) for fc in range(8) for h in ((0,) if j < 2 else ((0, 1) if j == 2 else (1,)))],
                     writes=[("psy", dh)])
                s.op("dve", lambda e, yb=yb, dh=dh, b=b: e.tensor_tensor(out=yst[yb][:, dh * 512:(dh + 1) * 512], in0=psy[dh][:, :],
                                                                         in1=bdb[b][:, dh * 512:(dh + 1) * 512], op=ALU.add),
                     reads=[("psy", dh), ("bdb", b)], writes=[("yst", yb, dh)])
            r0 = ex * CAP + j * 128
            nr = min(128, CAP - j * 128)
            s.dma("sp", lambda e, yb=yb, r0=r0, nr=nr: e.dma_start(out=y_d[r0:r0 + nr, :], in_=yst[yb][0:nr, :]),
                  reads=[("yst", yb, 0), ("yst", yb, 1)], writes=[("y_d", r0)])


def phase_combine(nc, s, y_d, h1_d, bd_d, ln2g_d, ln2b_d, out_d, identf, idx_all, gk_all, G_all):
    lng = s.sb("lng2", [128, D], F32)
    lnb = s.sb("lnb2", [128, D], F32)
    yk = [[s.sb(f"yk{i}_{k}", [128, D], F32) for k in range(4)] for i in range(2)]
    h1 = [s.sb(f"h1c{i}", [128, D], F32) for i in range(2)]
    z = [s.sb(f"zc{i}", [128, D], F32) for i in range(2)]
    ot = [s.sb(f"ot{i}", [128, D], F32) for i in range(2)]
    st6 = [s.sb(f"st6c{i}", [128, 2, 6], F32) for i in range(2)]
    mv = [s.sb(f"mvc{i}", [128, 2], F32) for i in range(2)]
    rs = [s.sb(f"rsc{i}", [128, 1], F32) for i in range(2)]
    s.dma("sp", lambda e: e.dma_start(out=lng[:], in_=ln2g_d.broadcast_to([128, D])), writes=["lng"])
    s.dma("sp", lambda e: e.dma_start(out=lnb[:], in_=ln2b_d.broadcast_to([128, D])), writes=["lnb"])
    for ti in range(32):
        b = ti % 2
        r0 = ti * 128
        s.dma("sp", lambda e, b=b, r0=r0: e.dma_start(out=h1[b][:], in_=h1_d[r0:r0 + 128, :]), writes=[("h1c", b)])
        for k in range(4):
            s.dma("pool", lambda e, b=b, ti=ti, k=k: e.indirect_dma_start(
                out=yk[b][k][:, :], out_offset=None, in_=y_d,
                in_offset=bass.IndirectOffsetOnAxis(ap=idx_all[:, ti * 4 + k:ti * 4 + k + 1], axis=0),
                bounds_check=_bc(s, e), oob_is_err=False), writes=[("yk", b, k)])
        zk = [("zc", b)]
        s.op("dve", lambda e, b=b, ti=ti: e.tensor_scalar(out=z[b][:], in0=yk[b][0][:], scalar1=gk_all[:, ti, 0:1], scalar2=None,
                                                          op0=ALU.mult),
             reads=[("yk", b, 0)], writes=zk)
        for k in range(1, 4):
            s.op("dve", lambda e, b=b, ti=ti, k=k: e.scalar_tensor_tensor(out=z[b][:], in0=yk[b][k][:], scalar=gk_all[:, ti, k:k + 1],
                                                                           in1=z[b][:], op0=ALU.mult, op1=ALU.add),
                 reads=[("yk", b, k)] + zk, writes=zk)
        s.op("dve", lambda e, b=b: e.scalar_tensor_tensor(out=z[b][:], in0=h1[b][:], scalar=ALPHA, in1=z[b][:], op0=ALU.mult,
                                                          op1=ALU.add),
             reads=[("h1c", b)] + zk, writes=zk)
        for hf in range(2):
            s.op("dve", lambda e, b=b, hf=hf: e.bn_stats(out=st6[b][:, hf, :], in_=z[b][:, hf * 512:(hf + 1) * 512]),
                 reads=zk, writes=[("st6", b, hf)])
        _ln_tail(s, z[b], zk, st6[b], [("st6", b, 0), ("st6", b, 1)], mv[b], rs[b], lng, lnb, ot[b], ("ot", b), f"c{b}")
        s.dma("sp", lambda e, b=b, r0=r0: e.dma_start(out=out_d[r0:r0 + 128, :], in_=ot[b][:]), reads=[("ot", b)],
              writes=[("out_d", ti)])


def _t5_bucket(dist):
    max_exact = 16
    lr = np.log(np.maximum(dist, max_exact).astype(np.float32) / np.float32(max_exact)) / np.float32(math.log(2048 / max_exact))
    large = np.minimum(max_exact + (lr.astype(np.float32) * np.float32(32 - max_exact)).astype(np.int32), 31)
    return np.where(dist < max_exact, dist, large)


def _attn_bias(rel_bias):
    k = np.arange(128)[:, None]
    q = np.arange(128)[None, :]
    out = np.empty((24, 128, 256), np.float32)
    for g, dil in enumerate((1, 4, 16)):
        for kb in range(2):
            dist = q - k + 128 if kb == 0 else q - k
            band = (dist >= 0) & (dist <= 128)
            bk = _t5_bucket(np.maximum(dist, 0) * dil)
            for h in range(8):
                hd = g * 8 + h
                out[hd, :, kb * 128:(kb + 1) * 128] = np.where(band, rel_bias[bk, hd], np.float32(-1e30))
    return out


_NC_CACHE = {}


def prepare_inputs(x, w_in, rel_bias, w_dw, b_dw, conv_ln_g, conv_ln_b, w_o_attn, w_o_conv, w_out,
                   ln1_g, ln1_b, w_router, b_router, w_gate_up, b_gate_up, w_down, b_down, ln2_g, ln2_b):
    f = lambda a: np.ascontiguousarray(np.asarray(a, dtype=np.float32))
    x = f(x)

    def pc(v, n):
        return f(np.asarray(v).reshape(n, 128).T)
    shared = {
        "abias": _attn_bias(f(rel_bias)),
        "w_in": f(w_in[0]),
        "w_dw": f(np.asarray(w_dw)[0, :, 0, :].reshape(31, 6, 128).transpose(2, 1, 0)),
        "b_dw": pc(b_dw[0], 6), "conv_ln_g": pc(conv_ln_g[0], 6), "conv_ln_b": pc(conv_ln_b[0], 6),
        "w_o_attn": f(w_o_attn[0]), "w_o_conv": f(w_o_conv[0]), "w_out": f(w_out[0]),
        "ln1_g": f(ln1_g), "ln1_b": f(ln1_b), "w_router": f(w_router[0]), "b_router": f(b_router),
        "w_gate_up": f(w_gate_up[0]),
        "b_gate_up": f(np.asarray(b_gate_up)[0].reshape(NE, 16, 128).transpose(0, 2, 1)),
        "w_down": f(w_down[0]), "b_down": f(b_down[0]), "ln2_g": f(ln2_g), "ln2_b": f(ln2_b),
    }
    in_maps = []
    for c in range(NCORES):
        bi, hf = c // 2, c % 2
        t0 = hf * OWN
        xh = np.zeros((NP, D), np.float32)
        lo = t0 - HALO
        if lo >= 0:
            xh[:] = x[bi, lo:lo + NP]
        else:
            xh[HALO:] = x[bi, 0:OWN]
        m = dict(shared)
        m["xT"] = np.ascontiguousarray(xh.T)
        m["xown"] = np.ascontiguousarray(x[bi, t0:t0 + OWN])
        m["hbias"] = np.full((128, 1), 0.0 if hf == 1 else -1e30, np.float32)
        in_maps.append(m)
    return in_maps


def kernel(**inputs):
    in_maps = prepare_inputs(**inputs)
    if "nc" not in _NC_CACHE:
        _NC_CACHE["nc"] = build()
    res = run_bass_kernel_spmd(_NC_CACHE["nc"], in_maps, core_ids=list(range(NCORES)))
    out = np.empty((4, 8192, D), np.float32)
    for c in range(NCORES):
        out[c // 2, (c % 2) * OWN:(c % 2 + 1) * OWN] = res.results[c]["out"]
    return out
```

```python
import math
from contextlib import ExitStack
import numpy as np
import concourse.bass as bass
import concourse.mybir as mybir
from concourse.bass_utils import run_bass_kernel_spmd

F32 = mybir.dt.float32
BF16 = mybir.dt.bfloat16
I32 = mybir.dt.int32
AF = mybir.ActivationFunctionType
ALU = mybir.AluOpType

NCORES = 8
D = 1024
OWN = 4096
HALO = 2048
NP = OWN + HALO
CAP = 704
CAPT = 768
NE = 32
ALPHA = 2.0 ** 0.25
EPS = 1e-5
ENGS = ("pe", "act", "dve", "pool", "sp")
DBG = {"iters": 99, "units": True, "final": True, "proj": 3, "pv": True, "slevel": 3, "hb": True, "pvacc": True}


class Sched:
    EPOCH = 8000

    def __init__(self, nc, es):
        self.nc = nc
        self.es = es
        self.loc = es
        self.streams = {e: [] for e in ENGS}
        self.cnt = {e: 0 for e in ENGS}
        self.esems = {e: [] for e in ENGS}
        self.res = {}
        self.waited = {e: {} for e in ENGS}
        self.dmasems = {}
        self.dmarr = {}
        for q, n in {"sp": 10, "act": 30, "pool": 10}.items():
            self.dmasems[q] = [[self.sem(f"dma_{q}{i}"), 0] for i in range(n)]
            self.dmarr[q] = 0

    def sem(self, name):
        return self.es.enter_context(self.nc.semaphore(name))

    def sb(self, name, shape, dt):
        return self.loc.enter_context(self.nc.sbuf_tensor(name, list(shape), dt))

    def ps(self, name, shape=(128, 512), dt=F32):
        return self.loc.enter_context(self.nc.psum_tensor(name, list(shape), dt))

    def _collect(self, eng, reads, writes):
        deps = []
        for r in reads:
            st = self.res.get(r)
            if st and st["w"] is not None:
                deps.append(st["w"])
        for w in writes:
            st = self.res.get(w)
            if st:
                if st["w"] is not None:
                    deps.append(st["w"])
                deps.extend(st["r"])
        know = self.waited[eng]
        out = []
        for (sem, val, peng, vc) in deps:
            if peng == "pe" and eng == "pe":
                continue
            if know.get(id(sem), 0) >= val:
                continue
            out.append((sem, val))
            for k, v in vc.items():
                if know.get(k, 0) < v:
                    know[k] = v
        return out

    def _record(self, tok, reads, writes):
        for r in reads:
            st = self.res.setdefault(r, {"w": None, "r": []})
            st["r"].append(tok)
        for w in writes:
            self.res[w] = {"w": tok, "r": []}

    def op(self, eng, fn, reads=(), writes=(), attach=None):
        waits = self._collect(eng, reads, writes)
        n = self.cnt[eng]
        ep = n // self.EPOCH
        while len(self.esems[eng]) <= ep:
            self.esems[eng].append(self.sem(f"c_{eng}{len(self.esems[eng])}"))
        sem = self.esems[eng][ep]
        val = n - ep * self.EPOCH + 1
        self.cnt[eng] = n + 1
        for w in waits:
            self.streams[eng].append(("wait", w[0], w[1]))
        self.streams[eng].append(("op", fn, sem, 1, (eng != "pe") if attach is None else attach))
        vc = dict(self.waited[eng])
        vc[id(sem)] = val
        for pe_ in range(ep):
            vc[id(self.esems[eng][pe_])] = self.EPOCH
        tok = (sem, val, eng, vc)
        self._record(tok, reads, writes)
        return tok

    def dma(self, q, fn, reads=(), writes=()):
        waits = self._collect(q, reads, writes)
        slot = self.dmasems[q][self.dmarr[q] % len(self.dmasems[q])]
        self.dmarr[q] += 1
        sem, total = slot
        if total > 0 and self.waited[q].get(id(sem), 0) < total:
            self.waited[q][id(sem)] = total
            waits.append((sem, total))
        total += 16
        slot[1] = total
        for w in waits:
            self.streams[q].append(("wait", w[0], w[1]))
        self.streams[q].append(("op", fn, sem, 16, False))
        vc = dict(self.waited[q])
        vc[id(sem)] = total
        tok = (sem, total, "dma", vc)
        self._record(tok, reads, writes)
        return tok

    def barrier(self):
        keys = list(self.res.keys())
        for eng in ENGS:
            for w in self._collect(eng, keys, keys):
                self.streams[eng].append(("wait", w[0], w[1]))
        self.res = {}

    def emit(self):
        streams = self.streams
        self.regcache = {}

        def run(e, items):
            pend = []
            for it in items:
                if it[0] == "wait":
                    pend.append(it)
                    continue
                attach = pend.pop() if (it[4] and pend) else None
                for w in pend:
                    e.wait_ge(w[1], w[2])
                pend = []
                ins = it[1](e)
                first, last = ins if isinstance(ins, tuple) else (ins, ins)
                if attach is not None:
                    first._wait_ge(attach[1], attach[2])
                last.then_inc(it[2], it[3])
            for w in pend:
                e.wait_ge(w[1], w[2])

        with self.nc.Block() as block:
            @block.sync
            def _(e):
                run(e, streams["sp"])

            @block.scalar
            def _(e):
                run(e, streams["act"])

            @block.vector
            def _(e):
                run(e, streams["dve"])

            @block.gpsimd
            def _(e):
                run(e, streams["pool"])

            @block.tensor
            def _(e):
                run(e, streams["pe"])
        self.streams = {e: [] for e in ENGS}

    def mm(self, out, pairs, reads, writes):
        def fn(e):
            n = len(pairs)
            ins = first = None
            for i, (l, r) in enumerate(pairs):
                ins = e.matmul(out, lhsT=l, rhs=r, start=(i == 0), stop=(i == n - 1))
                if first is None:
                    first = ins
            return first, ins
        return self.op("pe", fn, reads, writes, attach=True)


def _bc(s, e):
    if "bc" not in s.regcache:
        s.regcache["bc"] = e.to_reg(NE * CAP - 1)
    return s.regcache["bc"]


def _copy(s, eng, out, in_, reads, writes):
    if eng == "act":
        return s.op("act", lambda e: e.activation(out=out, in_=in_, func=AF.Identity), reads, writes)
    return s.op(eng, lambda e: e.tensor_copy(out=out, in_=in_), reads, writes)


def build(stop_after=99, debug=False):
    nc = bass.Bass("TRN2", target_bir_lowering=False)

    def din(name, shape, dt=F32):
        return nc.dram_tensor(name, list(shape), dt, kind="ExternalInput").ap()

    xT_d = din("xT", [D, NP])
    xown_d = din("xown", [OWN, D])
    hb_d = din("hbias", [128, 1])
    ab_d = din("abias", [24, 128, 256])
    win_d = din("w_in", [D, 8192])
    wdw_d = din("w_dw", [128, 6, 31])
    bdw_d = din("b_dw", [128, 6])
    clg_d = din("conv_ln_g", [128, 6])
    clb_d = din("conv_ln_b", [128, 6])
    woa_d = din("w_o_attn", [512, D])
    woc_d = din("w_o_conv", [768, D])
    wout_d = din("w_out", [D, D])
    ln1g_d = din("ln1_g", [1, D])
    ln1b_d = din("ln1_b", [1, D])
    wr_d = din("w_router", [D, NE])
    br_d = din("b_router", [1, NE])
    wgu_d = din("w_gate_up", [NE, D, 2048]) if stop_after >= 6 else None
    bgu_d = din("b_gate_up", [NE, 128, 16])
    wd_d = din("w_down", [NE, D, D]) if stop_after >= 6 else None
    bd_d = din("b_down", [NE, D])
    ln2g_d = din("ln2_g", [1, D])
    ln2b_d = din("ln2_b", [1, D])
    out_d = nc.dram_tensor("out", [OWN, D], F32, kind="ExternalOutput").ap()
    skind = "ExternalOutput" if debug else "Internal"
    attnT_d = nc.dram_tensor("attnT_s", [512, OWN], BF16, kind=skind).ap()
    convT_d = nc.dram_tensor("convT_s", [768, OWN], BF16, kind=skind).ap()
    mrgT_d = nc.dram_tensor("mrgT_s", [D, OWN], BF16, kind=skind).ap()
    h1_d = nc.dram_tensor("h1_s", [OWN, D], F32, kind=skind).ap()
    xs_d = nc.dram_tensor("xs_s", [NE * CAP + 128, D], BF16, kind="Internal").ap()
    y_d = nc.dram_tensor("y_s", [NE * CAP, D], F32, kind="Internal").ap()
    rt_d = nc.dram_tensor("rt_s", [128, 32, 8], F32, kind=skind).ap()

    win_v = win_d.rearrange("(kc p) n -> p kc n", p=128)

    with ExitStack() as es:
        s = Sched(nc, es)
        identb = s.sb("identb", [128, 128], BF16)
        identf = s.sb("identf", [128, 128], F32)
        idx_all = s.sb("idx_all", [128, 128], I32)
        gk_all = s.sb("gk_all", [128, 32, 4], F32)
        G_all = s.sb("G_all", [128, 32, NE], F32)
        for t, nm in ((identb, "identb"), (identf, "identf")):
            s.op("pool", lambda e, t=t: e.memset(t[:], 1.0), writes=[nm])
            s.op("pool", lambda e, t=t: e.affine_select(out=t[:], in_=t[:], pattern=[[-1, 128]],
                                                         compare_op=ALU.is_equal, fill=0.0, base=0,
                                                         channel_multiplier=1), reads=[nm], writes=[nm])

        with ExitStack() as es_x:
            s.loc = es_x
            xTb = s.sb("xTb", [128, 8, NP], BF16)
            for j in (1, 0, 2):
                for kc in range(8):
                    s.dma("pool", lambda e, kc=kc, j=j: e.dma_start(
                        out=xTb[:, kc, j * 2048:(j + 1) * 2048],
                        in_=xT_d[kc * 128:(kc + 1) * 128, j * 2048:(j + 1) * 2048]),
                        writes=[("x", kc, j)])
            XR = [("x", kc, j) for kc in range(8) for j in range(3)]

            if stop_after >= 2:
                with ExitStack() as es_p:
                    s.loc = es_p
                    phase_attn(nc, s, xTb, XR, win_v, ab_d, hb_d, attnT_d)
                    s.barrier()
                    s.emit()
            if stop_after >= 3:
                with ExitStack() as es_p:
                    s.loc = es_p
                    phase_conv(nc, s, xTb, XR, win_v, wdw_d, bdw_d, clg_d, clb_d, convT_d, identb)
                    s.barrier()
                    s.emit()
            if stop_after < 3:
                s.barrier()
                s.emit()
            if stop_after >= 4:
                with ExitStack() as es_p:
                    s.loc = es_p
                    phase_merge(nc, s, xTb, XR, win_v, woa_d, woc_d, attnT_d, convT_d, mrgT_d, xs_d)
                    s.barrier()
                    s.emit()
        if stop_after >= 5:
            with ExitStack() as es_p:
                s.loc = es_p
                phase_out_router(nc, s, mrgT_d, wout_d, xown_d, ln1g_d, ln1b_d, wr_d, br_d, h1_d, xs_d,
                                 identf, idx_all, gk_all, G_all, rt_d)
                s.barrier()
                s.emit()
        if stop_after >= 6:
            with ExitStack() as es_p:
                s.loc = es_p
                phase_experts(nc, s, xs_d, y_d, wgu_d, bgu_d, wd_d, bd_d, identb)
                s.barrier()
                s.emit()
        if stop_after >= 7:
            with ExitStack() as es_p:
                s.loc = es_p
                phase_combine(nc, s, y_d, h1_d, bd_d, ln2g_d, ln2b_d, out_d, identf, idx_all, gk_all, G_all)
                s.barrier()
                s.emit()
    return nc


def phase_attn(nc, s, xTb, XR, win_v, ab_d, hb_d, attnT_d):
    acc = [s.sb(f"acc{h}", [128, 2048], F32) for h in range(2)]
    qT = [[s.sb(f"qT{b}_{h}", [128, 2048], BF16) for h in range(2)] for b in range(2)]
    kT = [s.sb(f"kT{b}", [128, 4096], BF16) for b in range(2)]
    vB = [s.sb(f"vB{b}", [128, 32, 2, 128], BF16) for b in range(2)]
    wq = s.sb("wq", [128, 8, 128], BF16)
    wk = s.sb("wk", [128, 8, 128], BF16)
    wv = s.sb("wv", [128, 8, 128], BF16)
    ab = s.sb("ab", [128, 3, 2, 256], F32)
    abh = s.sb("abh", [128, 3, 2, 128], F32)
    tmp = [s.sb(f"tmp{i}", [128, 512], F32) for i in range(3)]
    pt = [s.sb(f"pt{i}", [128, 512], BF16) for i in range(3)]
    rec = tmp[0][0:64, :]
    hb = s.sb("hb", [128, 1], F32)
    psA = [s.ps(f"psA{i}") for i in range(2)]
    psV = s.ps("psV")
    psS = [s.ps(f"psS{i}") for i in range(3)]
    psO = [s.ps("psO0"), s.ps("psO1")]

    s.dma("sp", lambda e: e.dma_start(out=hb[:], in_=hb_d), writes=["hb"])
    for b in range(2):
        s.op("pool", lambda e, b=b: e.memset(vB[b][:], 1.0), writes=[("v", b, i) for i in range(8)])
        for h in range(2):
            s.op("pool", lambda e, b=b, h=h: e.memset(qT[b][h][:], 0.0), writes=[("q", b, h, tc) for tc in range(4)])

    def xr(lo, hi):
        return [("x", kc, j) for kc in range(8) for j in range(lo // 2048, (hi - 1) // 2048 + 1)]

    cnt = {"pa": 0, "ev": 0, "si": 0}
    iters = [(hp, half, g) for hp in range(4) for half in range(2) for g in range(3)][:DBG["iters"]]

    def make_proj(idx):
        hp, half, g = iters[idx]
        dil = (1, 4, 16)[g]
        halo = 128 * dil
        p0 = HALO + half * 2048
        b = idx % 2
        nK = halo + 2048
        kb0 = p0 - halo
        bpr = 16 // dil + 1
        nblk = dil * bpr
        steps = []

        def loads():
            for (wt, base, nm) in ((wq, 0, "wq"), (wk, 1536, "wk"), (wv, 3072, "wv")):
                c0 = base + g * 512 + hp * 128
                s.dma("pool", lambda e, wt=wt, c0=c0: e.dma_start(out=wt[:], in_=win_v[:, :, c0:c0 + 128]), writes=[nm])
        steps.append(loads)

        def qstep(tc):
            def f():
                pi = cnt["pa"] % 2
                cnt["pa"] += 1
                ps, pkey = psA[pi], ("psA", pi)
                s.mm(ps[:, 0:512], [(wq[:, kc, :], xTb[:, kc, p0 + tc * 512:p0 + (tc + 1) * 512]) for kc in range(8)],
                     reads=xr(p0 + tc * 512, p0 + (tc + 1) * 512) + ["wq"], writes=[pkey])
                _copy(s, "act", qT[b][0][0:64, tc * 512:(tc + 1) * 512], ps[0:64, 0:512], reads=[pkey], writes=[("q", b, 0, tc)])
                _copy(s, "act", qT[b][1][64:128, tc * 512:(tc + 1) * 512], ps[64:128, 0:512], reads=[pkey],
                      writes=[("q", b, 1, tc)])
            return f
        for tc in range(4):
            steps.append(qstep(tc))

        def kstep(off, n, ci):
            def f():
                pi = cnt["pa"] % 2
                cnt["pa"] += 1
                ps, pkey = psA[pi], ("psA", pi)
                s.mm(ps[:, 0:n], [(wk[:, kc, :], xTb[:, kc, kb0 + off:kb0 + off + n]) for kc in range(8)],
                     reads=xr(kb0 + off, kb0 + off + n) + ["wk"], writes=[pkey])
                _copy(s, ("act", "act", "dve")[cnt["ev"] % 3], kT[b][:, off:off + n], ps[:, 0:n], reads=[pkey], writes=[("k", b, ci)])
                cnt["ev"] += 1
            return f
        off = 0
        ci = 0
        while off < nK:
            n = min(512, nK - off)
            steps.append(kstep(off, n, ci))
            off += n
            ci += 1

        def vstep(blk0):
            def f():
                nb = min(4, nblk - blk0)
                for j in range(nb):
                    blk = blk0 + j
                    r, mi = blk // bpr, blk % bpr
                    st = p0 + r + dil * 128 * (mi - 1)
                    s.mm(psV[:, j * 128:(j + 1) * 128],
                         [(xTb[:, kc, st:st + 127 * dil + 1:dil], wv[:, kc, :]) for kc in range(8)],
                         reads=xr(st, st + 127 * dil + 1) + ["wv"], writes=["psV"])
                _copy(s, ("act", "act", "dve")[cnt["ev"] % 3], vB[b][:, blk0:blk0 + nb, :, 0:64],
                      psV[:, 0:nb * 128].rearrange("p (a b c) -> p a b c", a=nb, b=2),
                      reads=["psV"], writes=[("v", b, blk0 // 4)])
                cnt["ev"] += 1
            return f
        for blk0 in range(0, nblk, 4):
            steps.append(vstep(blk0))
        return steps

    def emit_S2(pair, i, b, g, dil, halo, nK, half):
        hh = pair[0][0]
        pS, tm, pT = psS[i], tmp[i], pt[i]
        mms = []
        rd = []
        flags = []
        for ui, (_, r, m) in enumerate(pair):
            q0 = r + dil * 128 * m
            qap = qT[b][hh][:, q0:q0 + 127 * dil + 1:dil]
            kp = halo + r + dil * 128 * (m - 1)
            kc_ = halo + r + dil * 128 * m
            kprev = kT[b][:, kp:kp + 127 * dil + 1:dil]
            kcur = kT[b][:, kc_:kc_ + 127 * dil + 1:dil]
            rd += [("q", b, hh, c) for c in range(q0 // 512, (q0 + 128 * dil - 1) // 512 + 1)]
            rd += [("k", b, c) for c in range(kp // 512, min((kc_ + 128 * dil - 1) // 512, (nK - 1) // 512) + 1)]
            mms.append((pS[:, ui * 256:ui * 256 + 128], kprev, qap))
            mms.append((pS[:, ui * 256 + 128:ui * 256 + 256], kcur, qap))
            flags.append(half == 0 and m == 0)

        def fn(e):
            ins = first = None
            for (o_, l_, r_) in mms:
                ins = e.matmul(o_, lhsT=l_, rhs=r_, start=True, stop=True)
                first = first or ins
            return first, ins
        s.op("pe", fn, reads=list(dict.fromkeys(rd)), writes=[("psS", i)], attach=True)
        tkeys = [("tmp", i, 0), ("tmp", i, 1)]
        if not any(flags):
            in1 = ab[:, g, hh:hh + 1, :].broadcast_to([128, 2, 256])
            s.op("dve", lambda e: e.scalar_tensor_tensor(out=tm[:].rearrange("p (a b) -> p a b", a=2),
                                                         in0=pS[:, 0:512].rearrange("p (a b) -> p a b", a=2), scalar=0.125,
                                                         in1=in1, op0=ALU.mult, op1=ALU.add),
                 reads=[("psS", i), ("ab", g, hh)], writes=tkeys)
        else:
            for ui in range(2):
                c0 = ui * 256
                if flags[ui]:
                    s.op("dve", lambda e, c0=c0: e.scalar_tensor_tensor(out=tm[:, c0:c0 + 128], in0=pS[:, c0:c0 + 128], scalar=0.125,
                                                                        in1=abh[:, g, hh, :], op0=ALU.mult, op1=ALU.add),
                         reads=[("psS", i), ("abh", g, hh)], writes=[("tmp", i, ui)])
                    s.op("dve", lambda e, c0=c0: e.scalar_tensor_tensor(out=tm[:, c0 + 128:c0 + 256], in0=pS[:, c0 + 128:c0 + 256],
                                                                        scalar=0.125, in1=ab[:, g, hh, 128:256], op0=ALU.mult,
                                                                        op1=ALU.add),
                         reads=[("psS", i), ("ab", g, hh), ("tmp", i, ui)], writes=[("tmp", i, ui)])
                else:
                    s.op("dve", lambda e, c0=c0: e.scalar_tensor_tensor(out=tm[:, c0:c0 + 256], in0=pS[:, c0:c0 + 256], scalar=0.125,
                                                                        in1=ab[:, g, hh, :], op0=ALU.mult, op1=ALU.add),
                         reads=[("psS", i), ("ab", g, hh)], writes=[("tmp", i, ui)])
        s.op("act", lambda e: e.activation(out=pT[:], in_=tm[:], func=AF.Exp), reads=tkeys, writes=[("pt", i)])

    def emit_PV2(pair, i, o, b, g, dil, bpr):
        hh = pair[0][0]
        pT = pt[i]
        mms = []
        rd = [("pt", i)]
        for ui, (_, r, m) in enumerate(pair):
            bp = r * bpr + m
            po = psO[o][:, ui * 128:(ui + 1) * 128]
            mms.append((po, vB[b][:, bp, hh, :], pT[:, ui * 256:ui * 256 + 128], True, False))
            mms.append((po, vB[b][:, bp + 1, hh, :], pT[:, ui * 256 + 128:ui * 256 + 256], False, True))
            rd += [("v", b, bp // 4), ("v", b, (bp + 1) // 4)]

        def fn(e):
            ins = first = None
            for (o_, l_, r_, st_, sp_) in mms:
                ins = e.matmul(o_, lhsT=l_, rhs=r_, start=st_, stop=sp_)
                first = first or ins
            return first, ins
        s.op("pe", fn, reads=list(dict.fromkeys(rd)), writes=[("psO", o)], attach=True)
        av = acc[hh][:].rearrange("p (a j d) -> p a d j", a=16 // dil, j=128, d=dil)
        (_, r0_, m0_), (_, r1_, m1_) = pair
        if m1_ != m0_:
            aap = av[:, m0_:m0_ + 2, r0_, :]
        else:
            aap = av[:, m0_, r0_:r0_ + 2, :]
        pin = psO[o][:, 0:256].rearrange("p (a b) -> p a b", a=2)
        if g == 0:
            s.op("dve", lambda e: e.tensor_copy(out=aap, in_=pin), reads=[("psO", o)], writes=[("acc", hh)])
        else:
            s.op("dve", lambda e: e.tensor_tensor(out=aap, in0=pin, in1=aap, op=ALU.add),
                 reads=[("psO", o), ("acc", hh)], writes=[("acc", hh)])

    def load_ab(hp):
        for g2 in range(3):
            for hh in range(2):
                hd = g2 * 8 + hp * 2 + hh
                s.dma("sp", lambda e, g2=g2, hh=hh, hd=hd: e.dma_start(out=ab[:, g2, hh, :], in_=ab_d[hd]),
                      writes=[("ab", g2, hh)])
                s.op("pool", lambda e, g2=g2, hh=hh: e.tensor_scalar(out=abh[:, g2, hh, :], in0=ab[:, g2, hh, 0:128],
                                                                    scalar1=hb[:, 0:1], scalar2=None, op0=ALU.add),
                     reads=[("ab", g2, hh), "hb"], writes=[("abh", g2, hh)])

    load_ab(0)
    for f in make_proj(0):
        f()
    for idx, (hp, half, g) in enumerate(iters):
        dil = (1, 4, 16)[g]
        halo = 128 * dil
        b = idx % 2
        nK = halo + 2048
        bpr = 16 // dil + 1
        nxt = make_proj(idx + 1) if idx + 1 < len(iters) else []
        units = [(hh, r, m) for hh in range(2) for r in range(dil) for m in range(16 // dil)]
        pairs = [(units[k], units[k + 1]) for k in range(0, len(units), 2)]
        pend = []
        for pi2, pr in enumerate(pairs):
            i = cnt["si"] % 3
            cnt["si"] += 1
            emit_S2(pr, i, b, g, dil, halo, nK, half)
            pend.append((pr, i, pi2 % 2))
            if len(pend) > 2:
                pp, pi_, po_ = pend.pop(0)
                emit_PV2(pp, pi_, po_, b, g, dil, bpr)
            if nxt and pi2 == 0:
                nxt.pop(0)()
            if pi2 >= 5:
                for _ in range(2):
                    if nxt:
                        nxt.pop(0)()
        while pend:
            pp, pi_, po_ = pend.pop(0)
            emit_PV2(pp, pi_, po_, b, g, dil, bpr)
        if idx + 1 < len(iters) and iters[idx + 1][0] != hp:
            load_ab(iters[idx + 1][0])
        while nxt:
            nxt.pop(0)()
        if g == 2:
            for hh in range(2):
                for c in range(4):
                    cs = slice(c * 512, (c + 1) * 512)
                    s.op("dve", lambda e, hh=hh, cs=cs: e.tensor_copy(out=rec, in_=acc[hh][64:128, cs]),
                         reads=[("acc", hh)], writes=[("tmp", 0, 0), ("tmp", 0, 1)])
                    s.op("act", lambda e: e.activation(out=rec, in_=rec, func=AF.Ln),
                         reads=[("tmp", 0, 0), ("tmp", 0, 1)], writes=[("tmp", 0, 0), ("tmp", 0, 1)])
                    s.op("act", lambda e: e.activation(out=rec, in_=rec, func=AF.Exp, scale=-1.0),
                         reads=[("tmp", 0, 0), ("tmp", 0, 1)], writes=[("tmp", 0, 0), ("tmp", 0, 1)])
                    s.op("dve", lambda e, hh=hh, cs=cs: e.tensor_tensor(out=acc[hh][0:64, cs], in0=acc[hh][0:64, cs], in1=rec,
                                                                        op=ALU.mult),
                         reads=[("tmp", 0, 0), ("tmp", 0, 1), ("acc", hh)], writes=[("acc", hh)])
                row = (hp * 2 + hh) * 64
                s.dma("pool", lambda e, hh=hh, row=row, half=half: e.dma_start(
                    out=attnT_d[row:row + 64, half * 2048:(half + 1) * 2048], in_=acc[hh][0:64, :]),
                    reads=[("acc", hh)], writes=[("attnT_d", row, half)])


def phase_conv(nc, s, xTb, XR, win_v, wdw_d, bdw_d, clg_d, clb_d, convT_d, identb):
    dw = s.sb("dw", [128, 6, 2048], F32)
    glu = [s.sb(f"glu{i}", [128, 32 + 2048], BF16) for i in range(2)]
    dg = [s.sb(f"dg{i}", [128, 31, 128], BF16) for i in range(2)]
    wu = [s.sb(f"wu{i}", [128, 8, 128], BF16) for i in range(2)]
    wg = [s.sb(f"wg{i}", [128, 8, 128], BF16) for i in range(2)]
    sgt = [s.sb(f"sgt{i}", [128, 512], F32) for i in range(2)]
    sq = [s.sb(f"sq{i}", [128, 512], F32) for i in range(2)]
    sd = s.sb("sd", [128, 512], F32)
    rstd = s.sb("rstd", [128, 512], F32)
    cst = [s.sb(f"cst{i}", [128, 6, 512], BF16) for i in range(2)]
    wdw = s.sb("wdw", [128, 6, 31], F32)
    bdw = s.sb("bdw", [128, 6], F32)
    clg = s.sb("clg", [128, 6], F32)
    clb = s.sb("clb", [128, 6], F32)
    onesf = s.sb("onesf", [128, 128], F32)
    psU = [s.ps(f"psU{i}") for i in range(2)]
    psG = [s.ps(f"psG{i}") for i in range(2)]
    psM = s.ps("psM")
    psV2 = s.ps("psV2")
    psC = [s.ps("psC0"), s.ps("psC1")]
    for t, d_, nm in ((wdw, wdw_d, "wdw"), (bdw, bdw_d, "bdw"), (clg, clg_d, "clg"), (clb, clb_d, "clb")):
        s.dma("sp", lambda e, t=t, d_=d_: e.dma_start(out=t[:], in_=d_), writes=[nm])
    s.op("pool", lambda e: e.memset(onesf[:], 1.0 / 768.0), writes=["onesf"])
    convT_v = convT_d.rearrange("(cc p) t -> p cc t", p=128)
    it = 0
    pu = 0
    ci = 0
    for half in range(2):
        p0 = HALO + half * 2048
        for cc in range(6):
            b = it % 2
            it += 1
            cv = 4608 + cc * 128
            cg = 4608 + 768 + cc * 128
            s.dma("pool", lambda e, b=b, cv=cv: e.dma_start(out=wu[b][:], in_=win_v[:, :, cv:cv + 128]), writes=[("wu", b)])
            s.dma("pool", lambda e, b=b, cg=cg: e.dma_start(out=wg[b][:], in_=win_v[:, :, cg:cg + 128]), writes=[("wg", b)])
            for (off, n) in [(0, 32)] + [(32 + i * 512, 512) for i in range(4)]:
                pi = pu % 2
                pu += 1
                t0 = p0 - 32 + off
                s.mm(psU[pi][:, 0:n], [(wu[b][:, kc, :], xTb[:, kc, t0:t0 + n]) for kc in range(8)],
                     reads=XR + [("wu", b)], writes=[("psU", pi)])
                s.mm(psG[pi][:, 0:n], [(wg[b][:, kc, :], xTb[:, kc, t0:t0 + n]) for kc in range(8)],
                     reads=XR + [("wg", b)], writes=[("psG", pi)])
                s.op("act", lambda e, pi=pi, n=n: e.activation(out=sgt[pi][:, 0:n], in_=psG[pi][:, 0:n], func=AF.Sigmoid),
                     reads=[("psG", pi)], writes=[("sgt", pi)])
                s.op("dve", lambda e, pi=pi, n=n, off=off, b=b: e.tensor_tensor(out=glu[b][:, off:off + n], in0=psU[pi][:, 0:n],
                                                                                in1=sgt[pi][:, 0:n], op=ALU.mult),
                     reads=[("psU", pi), ("sgt", pi)], writes=[("glu", b)])
            for j in range(31):
                s.op("dve", lambda e, b=b, cc=cc, j=j: e.tensor_scalar(out=dg[b][:, j, :], in0=identb[:], scalar1=wdw[:, cc, j:j + 1],
                                                                       scalar2=None, op0=ALU.mult),
                     reads=["identb", "wdw"], writes=[("dg", b)])
            for tc in range(4):
                pc = (it * 4 + tc) % 2
                s.mm(psC[pc][:, :], [(dg[b][:, j, :], glu[b][:, 2 + j + tc * 512:2 + j + (tc + 1) * 512]) for j in range(31)],
                     reads=[("dg", b), ("glu", b)], writes=[("psC", pc)])
                s.op("dve", lambda e, cc=cc, tc=tc, pc=pc: e.tensor_scalar(out=dw[:, cc, tc * 512:(tc + 1) * 512], in0=psC[pc][:, :],
                                                                          scalar1=bdw[:, cc:cc + 1], scalar2=None, op0=ALU.add),
                     reads=[("psC", pc), "bdw"], writes=[("dw", cc)])
        for tc in range(4):
            ts_ = slice(tc * 512, (tc + 1) * 512)
            s.mm(psM[:, :], [(onesf[:], dw[:, cc, ts_]) for cc in range(6)], reads=[("dw", cc) for cc in range(6)] + ["onesf"],
                 writes=["psM"])
            for cc in range(6):
                s.op("dve", lambda e, cc=cc, ts_=ts_: e.tensor_tensor(out=dw[:, cc, ts_], in0=dw[:, cc, ts_], in1=psM[:, :],
                                                                      op=ALU.subtract),
                     reads=["psM", ("dw", cc)], writes=[("dw", cc)])
            sqt = []
            for cc in range(6):
                qi = cc % 2
                s.op("act", lambda e, cc=cc, qi=qi, ts_=ts_: e.activation(out=sq[qi][:], in_=dw[:, cc, ts_], func=AF.Square),
                     reads=[("dw", cc)], writes=[("sq", qi)])
                def fn(e, cc=cc, qi=qi):
                    return e.matmul(psV2[:, :], lhsT=onesf[:], rhs=sq[qi][:], start=(cc == 0), stop=(cc == 5))
                s.op("pe", fn, reads=[("sq", qi), "onesf"], writes=["psV2"], attach=True)
            s.op("act", lambda e: e.activation(out=sd[:], in_=psV2[:, :], func=AF.Sqrt, bias=EPS), reads=["psV2"], writes=["sd"])
            s.op("dve", lambda e: e.reciprocal(out=rstd[:], in_=sd[:]), reads=["sd"], writes=["rstd"])
            cb = ci % 2
            ci += 1
            for cc in range(6):
                s.op("dve", lambda e, cc=cc, ts_=ts_: e.tensor_tensor(out=dw[:, cc, ts_], in0=dw[:, cc, ts_], in1=rstd[:],
                                                                      op=ALU.mult),
                     reads=["rstd", ("dw", cc)], writes=[("dw", cc)])
                s.op("dve", lambda e, cc=cc, ts_=ts_: e.tensor_scalar(out=dw[:, cc, ts_], in0=dw[:, cc, ts_], scalar1=clg[:, cc:cc + 1],
                                                                      scalar2=clb[:, cc:cc + 1], op0=ALU.mult, op1=ALU.add),
                     reads=[("dw", cc), "clg", "clb"], writes=[("dw", cc)])
                s.op("act", lambda e, cc=cc, ts_=ts_, cb=cb: e.activation(out=cst[cb][:, cc, :], in_=dw[:, cc, ts_], func=AF.Silu),
                     reads=[("dw", cc)], writes=[("cst", cb)])
            t0 = half * 2048 + tc * 512
            s.dma("sp", lambda e, cb=cb, t0=t0: e.dma_start(out=convT_v[:, :, t0:t0 + 512], in_=cst[cb][:]),
                  reads=[("cst", cb)], writes=[("convT_d", t0)])


def phase_merge(nc, s, xTb, XR, win_v, woa_d, woc_d, attnT_d, convT_d, mrgT_d, xs_d):
    woa = s.sb("woa", [128, 4, D], BF16)
    woc = s.sb("woc", [128, 6, D], BF16)
    wga = s.sb("wga", [128, 8, D], BF16)
    wgc = s.sb("wgc", [128, 8, D], BF16)
    at = [s.sb(f"at{i}", [128, 4, 512], BF16) for i in range(2)]
    cv = [s.sb(f"cv{i}", [128, 6, 512], BF16) for i in range(1)]
    mg = [s.sb(f"mg{i}", [128, 8, 512], BF16) for i in range(1)]
    sga = [s.sb(f"sga{i}", [128, 512], F32) for i in range(2)]
    sgc = [s.sb(f"sgc{i}", [128, 512], F32) for i in range(2)]
    psa = [s.ps(f"psa{i}") for i in range(2)]
    psc = [s.ps(f"psc{i}") for i in range(2)]
    psga = [s.ps(f"psga{i}") for i in range(2)]
    psgc = [s.ps(f"psgc{i}") for i in range(2)]
    for kc in range(8):
        s.dma("pool", lambda e, kc=kc: e.dma_start(out=wga[:, kc, :], in_=win_v[:, kc, 6144:7168]), writes=[("wga", kc)])
    for kc in range(8):
        s.dma("pool", lambda e, kc=kc: e.dma_start(out=wgc[:, kc, :], in_=win_v[:, kc, 7168:8192]), writes=[("wgc", kc)])
    s.dma("pool", lambda e: e.dma_start(out=woa[:], in_=woa_d.rearrange("(h p) n -> p h n", p=128)), writes=["woa"])
    s.dma("pool", lambda e: e.dma_start(out=woc[:], in_=woc_d.rearrange("(c p) n -> p c n", p=128)), writes=["woc"])
    zt = s.sb("zt", [128, D], BF16)
    s.op("dve", lambda e: e.memset(zt[:], 0.0), writes=["zt"])
    xs_z = xs_d.rearrange("(p j) d -> p j d", p=128)
    nrow = (NE * CAP + 128) // 128
    zq = list(range(nrow))

    def zero_some(n):
        for _ in range(n):
            if zq:
                c = zq.pop(0)
                s.dma("act", lambda e, c=c: e.dma_start(out=xs_z[:, c, :], in_=zt[:]), reads=["zt"], writes=[("xs_zero", c)])
    WGA = [("wga", kc) for kc in range(8)]
    WGC = [("wgc", kc) for kc in range(8)]
    attn_v = attnT_d.rearrange("(h p) t -> p h t", p=128)
    conv_v = convT_d.rearrange("(c p) t -> p c t", p=128)
    mrg_v = mrgT_d.rearrange("(c p) t -> p c t", p=128)
    pi = 0
    for tc in range(8):
        b = tc % 2
        ts_ = slice(tc * 512, (tc + 1) * 512)
        xs_ = slice(HALO + tc * 512, HALO + (tc + 1) * 512)
        s.dma("sp", lambda e, b=b, ts_=ts_: e.dma_start(out=at[b][:], in_=attn_v[:, :, ts_]), writes=[("at", b)])
        s.dma("sp", lambda e, ts_=ts_: e.dma_start(out=cv[0][:], in_=conv_v[:, :, ts_]), writes=[("cv", 0)])
        for fc in range(8):
            fs = slice(fc * 128, (fc + 1) * 128)
            p = pi % 2
            pi += 1
            s.mm(psga[p][:, :], [(wga[:, kc, fs], xTb[:, kc, xs_]) for kc in range(8)], reads=XR + WGA, writes=[("psga", p)])
            s.mm(psgc[p][:, :], [(wgc[:, kc, fs], xTb[:, kc, xs_]) for kc in range(8)], reads=XR + WGC, writes=[("psgc", p)])
            s.mm(psa[p][:, :], [(woa[:, h, fs], at[b][:, h, :]) for h in range(4)], reads=["woa", ("at", b)],
                 writes=[("psa", p)])
            s.mm(psc[p][:, :], [(woc[:, c, fs], cv[0][:, c, :]) for c in range(6)], reads=["woc", ("cv", 0)],
                 writes=[("psc", p)])
            s.op("act", lambda e, p=p: e.activation(out=sga[p][:], in_=psga[p][:, :], func=AF.Sigmoid),
                 reads=[("psga", p)], writes=[("sga", p)])
            s.op("act", lambda e, p=p: e.activation(out=sgc[p][:], in_=psgc[p][:, :], func=AF.Sigmoid),
                 reads=[("psgc", p)], writes=[("sgc", p)])
            s.op("dve", lambda e, p=p: e.tensor_tensor(out=sga[p][:], in0=psa[p][:, :], in1=sga[p][:], op=ALU.mult),
                 reads=[("psa", p), ("sga", p)], writes=[("sga", p)])
            s.op("dve", lambda e, p=p: e.tensor_tensor(out=sgc[p][:], in0=psc[p][:, :], in1=sgc[p][:], op=ALU.mult),
                 reads=[("psc", p), ("sgc", p)], writes=[("sgc", p)])
            s.op("dve", lambda e, p=p, fc=fc: e.tensor_tensor(out=mg[0][:, fc, :], in0=sga[p][:], in1=sgc[p][:], op=ALU.add),
                 reads=[("sga", p), ("sgc", p)], writes=[("mg", 0)])
            zero_some(3)
        s.dma("sp", lambda e, ts_=ts_: e.dma_start(out=mrg_v[:, :, ts_], in_=mg[0][:]), reads=[("mg", 0)],
              writes=[("mrg_d", tc)])
    zero_some(len(zq))


def phase_out_router(nc, s, mrgT_d, wout_d, xown_d, ln1g_d, ln1b_d, wr_d, br_d, h1_d, xs_d,
                     identf, idx_all, gk_all, G_all, rt_d):
    wout = s.sb("wout", [128, 8, D], BF16)
    lng = s.sb("lng", [128, D], F32)
    lnb = s.sb("lnb", [128, D], F32)
    wr = s.sb("wr", [128, 8, NE], F32)
    brb = s.sb("brb", [128, NE], F32)
    mg = [s.sb(f"mgc{i}", [128, 8, 512], BF16) for i in range(2)]
    xo = [s.sb(f"xo{i}", [128, D], F32) for i in range(2)]
    z = [s.sb(f"z{i}", [128, D], F32) for i in range(2)]
    h1 = [s.sb(f"h1{i}", [128, D], F32) for i in range(2)]
    h1b = [s.sb(f"h1b{i}", [128, D], BF16) for i in range(2)]
    h1T = [s.sb(f"h1T{i}", [128, 8, 128], F32) for i in range(2)]

    def two(name, shape, dt=F32):
        return [s.sb(f"{name}{i}", shape, dt) for i in range(2)]
    st6 = two("st6", [128, 2, 6])
    mv = two("mv", [128, 2])
    rs = two("rs", [128, 1])
    lg = two("lg", [128, NE])
    m8 = two("m8", [128, 8])
    selm = two("selm", [128, NE])
    selb = two("selb", [128, NE], BF16)
    ex = two("ex", [128, NE])
    den = two("den", [128, 1])
    rden = two("rden", [128, 1])
    pos = two("pos", [128, NE])
    key = two("key", [128, NE])
    k8 = two("k8", [128, 8])
    junk = two("junk", [128, NE])
    run = s.sb("run", [128, NE], F32)
    ustr = s.sb("ustr", [128, 128], BF16)
    onesb = s.sb("onesb", [128, 128], BF16)
    rtst = s.sb("rtst", [128, 32, 8], F32)
    pso = [[s.ps(f"pso{i}_{h}") for h in range(2)] for i in range(2)]
    pst = [s.ps(f"pst{i}") for i in range(2)]
    pslp = [s.ps(f"pslp{i}") for i in range(2)]

    for kc in range(8):
        s.dma("pool", lambda e, kc=kc: e.dma_start(out=wout[:, kc, :], in_=wout_d[kc * 128:(kc + 1) * 128, :]),
              writes=[("wout", kc)])
    WO = [("wout", kc) for kc in range(8)]
    s.dma("sp", lambda e: e.dma_start(out=lng[:], in_=ln1g_d.broadcast_to([128, D])), writes=["lng"])
    s.dma("sp", lambda e: e.dma_start(out=lnb[:], in_=ln1b_d.broadcast_to([128, D])), writes=["lnb"])
    s.dma("sp", lambda e: e.dma_start(out=wr[:], in_=wr_d.rearrange("(kc p) n -> p kc n", p=128)), writes=["wr"])
    s.dma("sp", lambda e: e.dma_start(out=brb[:], in_=br_d.broadcast_to([128, NE])), writes=["brb"])
    s.op("pool", lambda e: e.memset(ustr[:], 1.0), writes=["ustr"])
    s.op("pool", lambda e: e.affine_select(out=ustr[:], in_=ustr[:], pattern=[[1, 128]], compare_op=ALU.is_gt, fill=0.0,
                                           base=0, channel_multiplier=-1), reads=["ustr"], writes=["ustr"])
    s.op("pool", lambda e: e.memset(onesb[:], 1.0), writes=["onesb"])
    s.op("pool", lambda e: e.iota(run[:], pattern=[[CAP, NE]], base=1, channel_multiplier=0,
                                  allow_small_or_imprecise_dtypes=True), writes=["run"])
    mrg_v = mrgT_d.rearrange("(c p) t -> p c t", p=128)

    def stage_a(ti):
        tc, tt = ti // 4, ti % 4
        b = tc % 2
        tb = ti % 2
        r0 = ti * 128
        if tt == 0:
            ts_ = slice(tc * 512, (tc + 1) * 512)
            s.dma("sp", lambda e: e.dma_start(out=mg[b][:], in_=mrg_v[:, :, ts_]), writes=[("mgc", b)])
        s.dma("sp", lambda e: e.dma_start(out=xo[tb][:], in_=xown_d[r0:r0 + 128, :]), writes=[("xo", tb)])
        for hf in range(2):
            hs = slice(hf * 512, (hf + 1) * 512)
            s.mm(pso[tb][hf][:, :], [(mg[b][:, kc, tt * 128:(tt + 1) * 128], wout[:, kc, hs]) for kc in range(8)],
                 reads=[("mgc", b)] + WO, writes=[("pso", tb, hf)])
            s.op("dve", lambda e, hf=hf, hs=hs: e.scalar_tensor_tensor(out=z[tb][:, hs], in0=xo[tb][:, hs], scalar=ALPHA,
                                                                       in1=pso[tb][hf][:, :], op0=ALU.mult, op1=ALU.add),
                 reads=[("xo", tb), ("pso", tb, hf)], writes=[("z", tb, hf)])
            s.op("dve", lambda e, hf=hf, hs=hs: e.bn_stats(out=st6[tb][:, hf, :], in_=z[tb][:, hs]),
                 reads=[("z", tb, hf)], writes=[("st6", tb, hf)])
        _ln_tail(s, z[tb], [("z", tb, 0), ("z", tb, 1)], st6[tb], [("st6", tb, 0), ("st6", tb, 1)], mv[tb], rs[tb],
                 lng, lnb, h1[tb], ("h1", tb), f"r{tb}")
        s.dma("sp", lambda e: e.dma_start(out=h1_d[r0:r0 + 128, :], in_=h1[tb][:]), reads=[("h1", tb)], writes=[("h1_d", ti)])
        s.op("act", lambda e: e.activation(out=h1b[tb][:], in_=h1[tb][:], func=AF.Identity), reads=[("h1", tb)],
             writes=[("h1b", tb)])
        for hf in range(2):
            def fn(e, hf=hf):
                ins = None
                for k in range(4):
                    kc = hf * 4 + k
                    ins = e.transpose(out=pst[hf][:, k * 128:(k + 1) * 128], in_=h1[tb][:, kc * 128:(kc + 1) * 128],
                                      identity=identf[:])
                return ins
            s.op("pe", fn, reads=[("h1", tb), "identf"], writes=[("pst", hf)])
            _copy(s, "act", h1T[tb][:, hf * 4:(hf + 1) * 4, :], pst[hf][:, :].rearrange("p (a b) -> p a b", a=4),
                  reads=[("pst", hf)], writes=[("h1T", tb, hf)])

    def stage_b(ti):
        tb = ti % 2
        P = pslp[tb]
        pk = ("pslp", tb)
        lg_, m8_, sel_, selb_, ex_, den_, rden_, pos_, key_, k8_, junk_ = (lg[tb], m8[tb], selm[tb], selb[tb], ex[tb], den[tb],
                                                                          rden[tb], pos[tb], key[tb], k8[tb], junk[tb])
        T = lambda n: (n, tb)
        s.mm(P[:, 0:NE], [(h1T[tb][:, kc, :], wr[:, kc, :]) for kc in range(8)], reads=[("h1T", tb, 0), ("h1T", tb, 1), "wr"],
             writes=[pk])
        s.op("dve", lambda e: e.tensor_tensor(out=lg_[:], in0=P[:, 0:NE], in1=brb[:], op=ALU.add), reads=[pk, "brb"],
             writes=[T("lg")])
        s.op("dve", lambda e: e.max(out=m8_[:], in_=lg_[:]), reads=[T("lg")], writes=[T("m8")])
        s.op("dve", lambda e: e.tensor_scalar(out=sel_[:], in0=lg_[:], scalar1=m8_[:, 3:4], scalar2=None, op0=ALU.is_ge),
             reads=[T("lg"), T("m8")], writes=[T("selm")])
        s.op("dve", lambda e: e.tensor_scalar(out=ex_[:], in0=lg_[:], scalar1=m8_[:, 0:1], scalar2=None, op0=ALU.subtract),
             reads=[T("lg"), T("m8")], writes=[T("ex")])
        s.op("act", lambda e: e.activation(out=ex_[:], in_=ex_[:], func=AF.Exp), reads=[T("ex")], writes=[T("ex")])
        s.op("dve", lambda e: e.scalar_tensor_tensor(out=ex_[:], in0=ex_[:], scalar=1.0, in1=sel_[:], op0=ALU.mult,
                                                     op1=ALU.mult, accum_out=den_[:]),
             reads=[T("ex"), T("selm")], writes=[T("ex"), T("den")])
        s.op("dve", lambda e: e.reciprocal(out=rden_[:], in_=den_[:]), reads=[T("den")], writes=[T("rden")])
        s.op("dve", lambda e: e.tensor_scalar(out=G_all[:, ti, :], in0=ex_[:], scalar1=rden_[:, 0:1], scalar2=None,
                                              op0=ALU.mult),
             reads=[T("ex"), T("rden")], writes=[("G", ti)])
        s.op("act", lambda e: e.activation(out=selb_[:], in_=sel_[:], func=AF.Identity), reads=[T("selm")], writes=[T("selb")])

        def fnp(e):
            e.matmul(P[:, 64:64 + NE], lhsT=ustr[:], rhs=selb_[:], start=True, stop=True)
            return e.matmul(P[:, 128:128 + NE], lhsT=onesb[:], rhs=selb_[:], start=True, stop=True)
        s.op("pe", fnp, reads=["ustr", "onesb", T("selb"), T("lg")], writes=[pk])
        s.op("dve", lambda e: e.tensor_tensor(out=pos_[:], in0=P[:, 64:64 + NE], in1=run[:], op=ALU.add),
             reads=[pk, "run"], writes=[T("pos")])
        s.op("dve", lambda e: e.tensor_tensor(out=run[:], in0=P[:, 128:128 + NE], in1=run[:], op=ALU.add),
             reads=[pk, "run", T("pos")], writes=["run"])
        s.op("dve", lambda e: e.tensor_tensor(out=key_[:], in0=pos_[:], in1=sel_[:], op=ALU.mult),
             reads=[T("pos"), T("selm")], writes=[T("key")])
        s.op("dve", lambda e: e.max(out=k8_[:], in_=key_[:]), reads=[T("key")], writes=[T("k8")])
        s.op("dve", lambda e: e.tensor_scalar(out=idx_all[:, ti * 4:ti * 4 + 4], in0=k8_[:, 0:4], scalar1=-1.0, scalar2=None,
                                              op0=ALU.add),
             reads=[T("k8")], writes=[("idx", ti)])
        for k in range(4):
            s.op("dve", lambda e, k=k: e.scalar_tensor_tensor(out=junk_[:], in0=key_[:], scalar=k8_[:, k:k + 1],
                                                              in1=G_all[:, ti, :], op0=ALU.is_equal, op1=ALU.mult,
                                                              accum_out=gk_all[:, ti, k:k + 1]),
                 reads=[T("key"), T("k8"), ("G", ti), T("junk")], writes=[T("junk"), ("gk", ti, k)])
        if DBG.get("rt"):
            s.op("dve", lambda e: e.tensor_copy(out=rtst[:, ti, 0:4], in_=k8_[:, 0:4]), reads=[T("k8")], writes=[("rt", ti, 0)])
            s.op("dve", lambda e: e.tensor_copy(out=rtst[:, ti, 4:8], in_=gk_all[:, ti, :]),
                 reads=[("gk", ti, k) for k in range(4)], writes=[("rt", ti, 1)])
        for k in range(4):
            s.dma("pool", lambda e, k=k: e.indirect_dma_start(
                out=xs_d, out_offset=bass.IndirectOffsetOnAxis(ap=idx_all[:, ti * 4 + k:ti * 4 + k + 1], axis=0),
                in_=h1b[tb][:, :], in_offset=None, bounds_check=_bc(s, e), oob_is_err=False),
                reads=[("h1b", tb), ("idx", ti)], writes=[("xs_d", ti, k)])

    stage_a(0)
    for ti in range(32):
        if ti + 1 < 32:
            stage_a(ti + 1)
        stage_b(ti)
    if DBG.get("rt"):
        s.dma("sp", lambda e: e.dma_start(out=rt_d, in_=rtst[:]), reads=[("rt", ti, j) for ti in range(32) for j in range(2)],
              writes=["rt_d"])


def _ln_tail(s, z, zkeys, st6, skeys, mv, rs, lng, lnb, out, okey, tag):
    s.op("dve", lambda e: e.bn_aggr(out=mv[:], in_=st6[:].rearrange("p a b -> p (a b)")), reads=skeys, writes=["mv" + tag])
    s.op("act", lambda e: e.activation(out=rs[:], in_=mv[:, 1:2], func=AF.Ln, bias=EPS), reads=["mv" + tag], writes=["rs0" + tag])
    s.op("act", lambda e: e.activation(out=rs[:], in_=rs[:], func=AF.Exp, scale=-0.5), reads=["rs0" + tag], writes=["rs" + tag])
    s.op("dve", lambda e: e.scalar_tensor_tensor(out=z[:], in0=z[:], scalar=mv[:, 0:1], in1=lng[:], op0=ALU.subtract,
                                                 op1=ALU.mult),
         reads=zkeys + ["mv" + tag, "lng"], writes=zkeys)
    s.op("dve", lambda e: e.scalar_tensor_tensor(out=out[:], in0=z[:], scalar=rs[:, 0:1], in1=lnb[:], op0=ALU.mult,
                                                 op1=ALU.add),
         reads=zkeys + ["rs" + tag, "lnb"], writes=[okey])


def phase_experts(nc, s, xs_d, y_d, wgu_d, bgu_d, wd_d, bd_d, identb):
    wgu = [s.sb(f"wgu{i}", [128, 8, 2048], BF16) for i in range(2)]
    wdn = [s.sb(f"wdn{i}", [128, 8, D], BF16) for i in range(2)]
    bgu = [s.sb(f"bgu{i}", [128, 16], F32) for i in range(2)]
    bdb = [s.sb(f"bdb{i}", [128, D], F32) for i in range(2)]
    bgu1 = [s.sb(f"bgu1_{i}", [128, 8], F32) for i in range(2)]
    xs = [s.sb(f"xs{i}", [128, 6, D], BF16) for i in range(2)]
    xTe = s.sb("xTe", [128, 8, CAPT], BF16)
    aT = s.sb("aT", [128, 8, CAPT], BF16)
    NH = CAP // 2
    gt = [s.sb(f"gt{i}", [128, NH], F32) for i in range(2)]
    sg = [s.sb(f"sg{i}", [128, NH], F32) for i in range(2)]
    ut = [s.sb(f"ut{i}", [128, NH], F32) for i in range(2)]
    yst = [s.sb(f"yst{i}", [128, D], F32) for i in range(2)]
    NST = 4
    stg = [s.sb(f"stg{i}", [128, 2048], F32) for i in range(NST)]
    pstr = [s.ps(f"pstr{i}", [128, 1024], BF16) for i in range(2)]
    psg = [s.ps(f"psg{i}") for i in range(2)]
    psu = [s.ps(f"psu{i}") for i in range(2)]
    psy = [s.ps(f"psy{i}") for i in range(2)]

    chunks = []
    for ex in range(NE):
        chunks += [(ex, 0, kc) for kc in range(8)] + [(ex, 1, kc) for kc in range(8)]
    st = {"dma": 0, "cast": 0}

    def emit_dma():
        c = st["dma"]
        if c >= len(chunks):
            return
        st["dma"] = c + 1
        ex, kind, kc = chunks[c]
        t = c % NST
        if kind == 0:
            s.dma("sp", lambda e: e.dma_start(out=stg[t][:, :], in_=wgu_d[ex, kc * 128:(kc + 1) * 128, :]), writes=[("stg", t)])
        else:
            s.dma("sp", lambda e: e.dma_start(out=stg[t][:, 0:D], in_=wd_d[ex, kc * 128:(kc + 1) * 128, :]), writes=[("stg", t)])

    def pump():
        c = st["cast"]
        if c >= len(chunks):
            return
        while st["dma"] < min(c + NST, len(chunks)):
            emit_dma()
        st["cast"] = c + 1
        ex, kind, kc = chunks[c]
        t = c % NST
        b = ex % 2
        if kind == 0:
            s.op("act", lambda e: e.activation(out=wgu[b][:, kc, :], in_=stg[t][:, :], func=AF.Identity), reads=[("stg", t)],
                 writes=[("wgu", b, kc)])
        else:
            s.op("act", lambda e: e.activation(out=wdn[b][:, kc, :], in_=stg[t][:, 0:D], func=AF.Identity), reads=[("stg", t)],
                 writes=[("wdn", b, kc)])

    def small_loads(ex):
        b = ex % 2
        s.dma("sp", lambda e: e.dma_start(out=bgu[b][:], in_=bgu_d[ex]), writes=[("bgu", b)])
        s.op("dve", lambda e: e.tensor_scalar(out=bgu1[b][:], in0=bgu[b][:, 8:16], scalar1=1.0, scalar2=None, op0=ALU.add),
             reads=[("bgu", b)], writes=[("bgu1", b)])
        s.dma("sp", lambda e: e.dma_start(out=bdb[b][:], in_=bd_d[ex:ex + 1, :].broadcast_to([128, D])), writes=[("bdb", b)])
        s.dma("sp", lambda e: e.dma_start(
            out=xs[b][:], in_=xs_d[ex * CAP:ex * CAP + CAPT, :].rearrange("(j p) d -> p j d", p=128)), writes=[("xs", b)])

    s.op("pool", lambda e: e.memset(aT[:], 0.0), writes=[("aT", fc, h) for fc in range(8) for h in range(2)])
    small_loads(0)
    for _ in range(16):
        pump()
    ti_ = 0
    gi = 0
    yi = 0
    for ex in range(NE):
        b = ex % 2
        if ex + 1 < NE:
            small_loads(ex + 1)
        WGU = [("wgu", b, kc) for kc in range(8)]
        WDN = [("wdn", b, kc) for kc in range(8)]
        for j in range(6):
            p = ti_ % 2
            ti_ += 1

            def fn(e, p=p, j=j, b=b):
                ins = None
                for kc in range(8):
                    ins = e.transpose(out=pstr[p][:, kc * 128:(kc + 1) * 128], in_=xs[b][:, j, kc * 128:(kc + 1) * 128],
                                      identity=identb[:])
                return ins
            s.op("pe", fn, reads=[("xs", b), "identb"], writes=[("pstr", p)])
            _copy(s, "act", xTe[:, :, j * 128:(j + 1) * 128], pstr[p][:, :].rearrange("p (a b) -> p a b", a=8),
                  reads=[("pstr", p)], writes=[("xTe", j)])
        for fcp in range(8):
            for nh in range(2):
                p = gi % 2
                gi += 1
                ns = slice(nh * NH, (nh + 1) * NH)
                xk = [("xTe", j) for j in ((0, 1, 2) if nh == 0 else (2, 3, 4, 5))]
                s.mm(psg[p][:, 0:NH], [(wgu[b][:, kc, fcp * 128:(fcp + 1) * 128], xTe[:, kc, ns]) for kc in range(8)],
                     reads=WGU + xk, writes=[("psg", p)])
                s.mm(psu[p][:, 0:NH], [(wgu[b][:, kc, D + fcp * 128:D + (fcp + 1) * 128], xTe[:, kc, ns]) for kc in range(8)],
                     reads=WGU + xk, writes=[("psu", p)])
                s.op("dve", lambda e, p=p, b=b, fcp=fcp: e.tensor_scalar(out=gt[p][:], in0=psg[p][:, 0:NH],
                                                                        scalar1=bgu[b][:, fcp:fcp + 1], scalar2=7.0,
                                                                        op0=ALU.add, op1=ALU.min),
                     reads=[("psg", p), ("bgu", b)], writes=[("gt", p)])
                s.op("act", lambda e, p=p: e.activation(out=sg[p][:], in_=gt[p][:], func=AF.Sigmoid, scale=1.702),
                     reads=[("gt", p)], writes=[("sg", p)])
                pump()
                s.op("dve", lambda e, p=p, b=b, fcp=fcp: e.tensor_scalar(out=ut[p][:], in0=psu[p][:, 0:NH],
                                                                        scalar1=bgu1[b][:, fcp:fcp + 1], scalar2=8.0,
                                                                        op0=ALU.add, op1=ALU.min),
                     reads=[("psu", p), ("bgu1", b)], writes=[("ut", p)])
                s.op("dve", lambda e, p=p: e.scalar_tensor_tensor(out=ut[p][:], in0=ut[p][:], scalar=-6.0, in1=gt[p][:],
                                                                  op0=ALU.max, op1=ALU.mult),
                     reads=[("ut", p), ("gt", p)], writes=[("ut", p)])
                s.op("dve", lambda e, p=p, fcp=fcp, ns=ns: e.tensor_tensor(out=aT[:, fcp, ns], in0=ut[p][:], in1=sg[p][:],
                                                                           op=ALU.mult),
                     reads=[("sg", p), ("ut", p)], writes=[("aT", fcp, nh)])
        for j in range(6):
            yb = yi % 2
            yi += 1
            for dh in range(2):
                s.mm(psy[dh][:, :], [(aT[:, fc, j * 128:(j + 1) * 128], wdn[b][:, fc, dh * 512:(dh + 1) * 512]) for fc in range(8)],
                     reads=WDN + [("aT", fc, h) for fc in range(8) for h in ((0,) if j < 2 else ((0, 1) if j == 2 else (1,)))],
                     writes=[("psy", dh)])
                s.op("dve", lambda e, yb=yb, dh=dh, b=b: e.tensor_tensor(out=yst[yb][:, dh * 512:(dh + 1) * 512], in0=psy[dh][:, :],
                                                                         in1=bdb[b][:, dh * 512:(dh + 1) * 512], op=ALU.add),
                     reads=[("psy", dh), ("bdb", b)], writes=[("yst", yb, dh)])
            r0 = ex * CAP + j * 128
            nr = min(128, CAP - j * 128)
            s.dma("sp", lambda e, yb=yb, r0=r0, nr=nr: e.dma_start(out=y_d[r0:r0 + nr, :], in_=yst[yb][0:nr, :]),
                  reads=[("yst", yb, 0), ("yst", yb, 1)], writes=[("y_d", r0)])


def phase_combine(nc, s, y_d, h1_d, bd_d, ln2g_d, ln2b_d, out_d, identf, idx_all, gk_all, G_all):
    lng = s.sb("lng2", [128, D], F32)
    lnb = s.sb("lnb2", [128, D], F32)
    yk = [[s.sb(f"yk{i}_{k}", [128, D], F32) for k in range(4)] for i in range(2)]
    h1 = [s.sb(f"h1c{i}", [128, D], F32) for i in range(2)]
    z = [s.sb(f"zc{i}", [128, D], F32) for i in range(2)]
    ot = [s.sb(f"ot{i}", [128, D], F32) for i in range(2)]
    st6 = [s.sb(f"st6c{i}", [128, 2, 6], F32) for i in range(2)]
    mv = [s.sb(f"mvc{i}", [128, 2], F32) for i in range(2)]
    rs = [s.sb(f"rsc{i}", [128, 1], F32) for i in range(2)]
    s.dma("sp", lambda e: e.dma_start(out=lng[:], in_=ln2g_d.broadcast_to([128, D])), writes=["lng"])
    s.dma("sp", lambda e: e.dma_start(out=lnb[:], in_=ln2b_d.broadcast_to([128, D])), writes=["lnb"])
    for ti in range(32):
        b = ti % 2
        r0 = ti * 128
        s.dma("sp", lambda e, b=b, r0=r0: e.dma_start(out=h1[b][:], in_=h1_d[r0:r0 + 128, :]), writes=[("h1c", b)])
        for k in range(4):
            s.dma("pool", lambda e, b=b, ti=ti, k=k: e.indirect_dma_start(
                out=yk[b][k][:, :], out_offset=None, in_=y_d,
                in_offset=bass.IndirectOffsetOnAxis(ap=idx_all[:, ti * 4 + k:ti * 4 + k + 1], axis=0),
                bounds_check=_bc(s, e), oob_is_err=False), writes=[("yk", b, k)])
        zk = [("zc", b)]
        s.op("dve", lambda e, b=b, ti=ti: e.tensor_scalar(out=z[b][:], in0=yk[b][0][:], scalar1=gk_all[:, ti, 0:1], scalar2=None,
                                                          op0=ALU.mult),
             reads=[("yk", b, 0)], writes=zk)
        for k in range(1, 4):
            s.op("dve", lambda e, b=b, ti=ti, k=k: e.scalar_tensor_tensor(out=z[b][:], in0=yk[b][k][:], scalar=gk_all[:, ti, k:k + 1],
                                                                           in1=z[b][:], op0=ALU.mult, op1=ALU.add),
                 reads=[("yk", b, k)] + zk, writes=zk)
        s.op("dve", lambda e, b=b: e.scalar_tensor_tensor(out=z[b][:], in0=h1[b][:], scalar=ALPHA, in1=z[b][:], op0=ALU.mult,
                                                          op1=ALU.add),
             reads=[("h1c", b)] + zk, writes=zk)
        for hf in range(2):
            s.op("dve", lambda e, b=b, hf=hf: e.bn_stats(out=st6[b][:, hf, :], in_=z[b][:, hf * 512:(hf + 1) * 512]),
                 reads=zk, writes=[("st6", b, hf)])
        _ln_tail(s, z[b], zk, st6[b], [("st6", b, 0), ("st6", b, 1)], mv[b], rs[b], lng, lnb, ot[b], ("ot", b), f"c{b}")
        s.dma("sp", lambda e, b=b, r0=r0: e.dma_start(out=out_d[r0:r0 + 128, :], in_=ot[b][:]), reads=[("ot", b)],
              writes=[("out_d", ti)])


def _t5_bucket(dist):
    max_exact = 16
    lr = np.log(np.maximum(dist, max_exact).astype(np.float32) / np.float32(max_exact)) / np.float32(math.log(2048 / max_exact))
    large = np.minimum(max_exact + (lr.astype(np.float32) * np.float32(32 - max_exact)).astype(np.int32), 31)
    return np.where(dist < max_exact, dist, large)


def _attn_bias(rel_bias):
    k = np.arange(128)[:, None]
    q = np.arange(128)[None, :]
    out = np.empty((24, 128, 256), np.float32)
    for g, dil in enumerate((1, 4, 16)):
        for kb in range(2):
            dist = q - k + 128 if kb == 0 else q - k
            band = (dist >= 0) & (dist <= 128)
            bk = _t5_bucket(np.maximum(dist, 0) * dil)
            for h in range(8):
                hd = g * 8 + h
                out[hd, :, kb * 128:(kb + 1) * 128] = np.where(band, rel_bias[bk, hd], np.float32(-1e30))
    return out


_NC_CACHE = {}


def prepare_inputs(x, w_in, rel_bias, w_dw, b_dw, conv_ln_g, conv_ln_b, w_o_attn, w_o_conv, w_out,
                   ln1_g, ln1_b, w_router, b_router, w_gate_up, b_gate_up, w_down, b_down, ln2_g, ln2_b):
    f = lambda a: np.ascontiguousarray(np.asarray(a, dtype=np.float32))
    x = f(x)

    def pc(v, n):
        return f(np.asarray(v).reshape(n, 128).T)
    shared = {
        "abias": _attn_bias(f(rel_bias)),
        "w_in": f(w_in[0]),
        "w_dw": f(np.asarray(w_dw)[0, :, 0, :].reshape(31, 6, 128).transpose(2, 1, 0)),
        "b_dw": pc(b_dw[0], 6), "conv_ln_g": pc(conv_ln_g[0], 6), "conv_ln_b": pc(conv_ln_b[0], 6),
        "w_o_attn": f(w_o_attn[0]), "w_o_conv": f(w_o_conv[0]), "w_out": f(w_out[0]),
        "ln1_g": f(ln1_g), "ln1_b": f(ln1_b), "w_router": f(w_router[0]), "b_router": f(b_router),
        "w_gate_up": f(w_gate_up[0]),
        "b_gate_up": f(np.asarray(b_gate_up)[0].reshape(NE, 16, 128).transpose(0, 2, 1)),
        "w_down": f(w_down[0]), "b_down": f(b_down[0]), "ln2_g": f(ln2_g), "ln2_b": f(ln2_b),
    }
    in_maps = []
    for c in range(NCORES):
        bi, hf = c // 2, c % 2
        t0 = hf * OWN
        xh = np.zeros((NP, D), np.float32)
        lo = t0 - HALO
        if lo >= 0:
            xh[:] = x[bi, lo:lo + NP]
        else:
            xh[HALO:] = x[bi, 0:OWN]
        m = dict(shared)
        m["xT"] = np.ascontiguousarray(xh.T)
        m["xown"] = np.ascontiguousarray(x[bi, t0:t0 + OWN])
        m["hbias"] = np.full((128, 1), 0.0 if hf == 1 else -1e30, np.float32)
        in_maps.append(m)
    return in_maps


def kernel(**inputs):
    in_maps = prepare_inputs(**inputs)
    if "nc" not in _NC_CACHE:
        _NC_CACHE["nc"] = build()
    res = run_bass_kernel_spmd(_NC_CACHE["nc"], in_maps, core_ids=list(range(NCORES)))
    out = np.empty((4, 8192, D), np.float32)
    for c in range(NCORES):
        out[c // 2, (c % 2) * OWN:(c % 2 + 1) * OWN] = res.results[c]["out"]
    return out
```

```python
import math
from contextlib import ExitStack
import numpy as np
import concourse.bass as bass
import concourse.mybir as mybir
from concourse.bass_utils import run_bass_kernel_spmd

F32 = mybir.dt.float32
BF16 = mybir.dt.bfloat16
I32 = mybir.dt.int32
AF = mybir.ActivationFunctionType
ALU = mybir.AluOpType

NCORES = 8
D = 1024
OWN = 4096
HALO = 2048
NP = OWN + HALO
CAP = 704
CAPT = 768
NE = 32
ALPHA = 2.0 ** 0.25
EPS = 1e-5
ENGS = ("pe", "act", "dve", "pool", "sp")
DBG = {"iters": 99, "units": True, "final": True, "proj": 3, "pv": True, "slevel": 3, "hb": True, "pvacc": True}


class Sched:
    EPOCH = 8000

    def __init__(self, nc, es):
        self.nc = nc
        self.es = es
        self.loc = es
        self.streams = {e: [] for e in ENGS}
        self.cnt = {e: 0 for e in ENGS}
        self.esems = {e: [] for e in ENGS}
        self.res = {}
        self.waited = {e: {} for e in ENGS}
        self.dmasems = {}
        self.dmarr = {}
        for q, n in {"sp": 10, "act": 30, "pool": 10}.items():
            self.dmasems[q] = [[self.sem(f"dma_{q}{i}"), 0] for i in range(n)]
            self.dmarr[q] = 0

    def sem(self, name):
        return self.es.enter_context(self.nc.semaphore(name))

    def sb(self, name, shape, dt):
        return self.loc.enter_context(self.nc.sbuf_tensor(name, list(shape), dt))

    def ps(self, name, shape=(128, 512), dt=F32):
        return self.loc.enter_context(self.nc.psum_tensor(name, list(shape), dt))

    def _collect(self, eng, reads, writes):
        deps = []
        for r in reads:
            st = self.res.get(r)
            if st and st["w"] is not None:
                deps.append(st["w"])
        for w in writes:
            st = self.res.get(w)
            if st:
                if st["w"] is not None:
                    deps.append(st["w"])
                deps.extend(st["r"])
        know = self.waited[eng]
        out = []
        for (sem, val, peng, vc) in deps:
            if peng == "pe" and eng == "pe":
                continue
            if know.get(id(sem), 0) >= val:
                continue
            out.append((sem, val))
            for k, v in vc.items():
                if know.get(k, 0) < v:
                    know[k] = v
        return out

    def _record(self, tok, reads, writes):
        for r in reads:
            st = self.res.setdefault(r, {"w": None, "r": []})
            st["r"].append(tok)
        for w in writes:
            self.res[w] = {"w": tok, "r": []}

    def op(self, eng, fn, reads=(), writes=(), attach=None):
        waits = self._collect(eng, reads, writes)
        n = self.cnt[eng]
        ep = n // self.EPOCH
        while len(self.esems[eng]) <= ep:
            self.esems[eng].append(self.sem(f"c_{eng}{len(self.esems[eng])}"))
        sem = self.esems[eng][ep]
        val = n - ep * self.EPOCH + 1
        self.cnt[eng] = n + 1
        for w in waits:
            self.streams[eng].append(("wait", w[0], w[1]))
        self.streams[eng].append(("op", fn, sem, 1, (eng != "pe") if attach is None else attach))
        vc = dict(self.waited[eng])
        vc[id(sem)] = val
        for pe_ in range(ep):
            vc[id(self.esems[eng][pe_])] = self.EPOCH
        tok = (sem, val, eng, vc)
        self._record(tok, reads, writes)
        return tok

    def dma(self, q, fn, reads=(), writes=()):
        waits = self._collect(q, reads, writes)
        slot = self.dmasems[q][self.dmarr[q] % len(self.dmasems[q])]
        self.dmarr[q] += 1
        sem, total = slot
        if total > 0 and self.waited[q].get(id(sem), 0) < total:
            self.waited[q][id(sem)] = total
            waits.append((sem, total))
        total += 16
        slot[1] = total
        for w in waits:
            self.streams[q].append(("wait", w[0], w[1]))
        self.streams[q].append(("op", fn, sem, 16, False))
        vc = dict(self.waited[q])
        vc[id(sem)] = total
        tok = (sem, total, "dma", vc)
        self._record(tok, reads, writes)
        return tok

    def barrier(self):
        keys = list(self.res.keys())
        for eng in ENGS:
            for w in self._collect(eng, keys, keys):
                self.streams[eng].append(("wait", w[0], w[1]))
        self.res = {}

    def emit(self):
        streams = self.streams
        self.regcache = {}

        def run(e, items):
            pend = []
            for it in items:
                if it[0] == "wait":
                    pend.append(it)
                    continue
                attach = pend.pop() if (it[4] and pend) else None
                for w in pend:
                    e.wait_ge(w[1], w[2])
                pend = []
                ins = it[1](e)
                first, last = ins if isinstance(ins, tuple) else (ins, ins)
                if attach is not None:
                    first._wait_ge(attach[1], attach[2])
                last.then_inc(it[2], it[3])
            for w in pend:
                e.wait_ge(w[1], w[2])

        with self.nc.Block() as block:
            @block.sync
            def _(e):
                run(e, streams["sp"])

            @block.scalar
            def _(e):
                run(e, streams["act"])

            @block.vector
            def _(e):
                run(e, streams["dve"])

            @block.gpsimd
            def _(e):
                run(e, streams["pool"])

            @block.tensor
            def _(e):
                run(e, streams["pe"])
        self.streams = {e: [] for e in ENGS}

    def mm(self, out, pairs, reads, writes):
        def fn(e):
            n = len(pairs)
            ins = first = None
            for i, (l, r) in enumerate(pairs):
                ins = e.matmul(out, lhsT=l, rhs=r, start=(i == 0), stop=(i == n - 1))
                if first is None:
                    first = ins
            return first, ins
        return self.op("pe", fn, reads, writes, attach=True)


def _bc(s, e):
    if "bc" not in s.regcache:
        s.regcache["bc"] = e.to_reg(NE * CAP - 1)
    return s.regcache["bc"]


def _copy(s, eng, out, in_, reads, writes):
    if eng == "act":
        return s.op("act", lambda e: e.activation(out=out, in_=in_, func=AF.Identity), reads, writes)
    return s.op(eng, lambda e: e.tensor_copy(out=out, in_=in_), reads, writes)


def build(stop_after=99, debug=False):
    nc = bass.Bass("TRN2", target_bir_lowering=False)

    def din(name, shape, dt=F32):
        return nc.dram_tensor(name, list(shape), dt, kind="ExternalInput").ap()

    xT_d = din("xT", [D, NP])
    xown_d = din("xown", [OWN, D])
    hb_d = din("hbias", [128, 1])
    ab_d = din("abias", [24, 128, 256])
    win_d = din("w_in", [D, 8192])
    wdw_d = din("w_dw", [128, 6, 31])
    bdw_d = din("b_dw", [128, 6])
    clg_d = din("conv_ln_g", [128, 6])
    clb_d = din("conv_ln_b", [128, 6])
    woa_d = din("w_o_attn", [512, D])
    woc_d = din("w_o_conv", [768, D])
    wout_d = din("w_out", [D, D])
    ln1g_d = din("ln1_g", [1, D])
    ln1b_d = din("ln1_b", [1, D])
    wr_d = din("w_router", [D, NE])
    br_d = din("b_router", [1, NE])
    wgu_d = din("w_gate_up", [NE, D, 2048]) if stop_after >= 6 else None
    bgu_d = din("b_gate_up", [NE, 128, 16])
    wd_d = din("w_down", [NE, D, D]) if stop_after >= 6 else None
    bd_d = din("b_down", [NE, D])
    ln2g_d = din("ln2_g", [1, D])
    ln2b_d = din("ln2_b", [1, D])
    out_d = nc.dram_tensor("out", [OWN, D], F32, kind="ExternalOutput").ap()
    skind = "ExternalOutput" if debug else "Internal"
    attnT_d = nc.dram_tensor("attnT_s", [512, OWN], BF16, kind=skind).ap()
    convT_d = nc.dram_tensor("convT_s", [768, OWN], BF16, kind=skind).ap()
    mrgT_d = nc.dram_tensor("mrgT_s", [D, OWN], BF16, kind=skind).ap()
    h1_d = nc.dram_tensor("h1_s", [OWN, D], F32, kind=skind).ap()
    xs_d = nc.dram_tensor("xs_s", [NE * CAP + 128, D], BF16, kind="Internal").ap()
    y_d = nc.dram_tensor("y_s", [NE * CAP, D], F32, kind="Internal").ap()
    rt_d = nc.dram_tensor("rt_s", [128, 32, 8], F32, kind=skind).ap()

    win_v = win_d.rearrange("(kc p) n -> p kc n", p=128)

    with ExitStack() as es:
        s = Sched(nc, es)
        identb = s.sb("identb", [128, 128], BF16)
        identf = s.sb("identf", [128, 128], F32)
        idx_all = s.sb("idx_all", [128, 128], I32)
        gk_all = s.sb("gk_all", [128, 32, 4], F32)
        G_all = s.sb("G_all", [128, 32, NE], F32)
        for t, nm in ((identb, "identb"), (identf, "identf")):
            s.op("pool", lambda e, t=t: e.memset(t[:], 1.0), writes=[nm])
            s.op("pool", lambda e, t=t: e.affine_select(out=t[:], in_=t[:], pattern=[[-1, 128]],
                                                         compare_op=ALU.is_equal, fill=0.0, base=0,
                                                         channel_multiplier=1), reads=[nm], writes=[nm])

        with ExitStack() as es_x:
            s.loc = es_x
            xTb = s.sb("xTb", [128, 8, NP], BF16)
            for j in (1, 0, 2):
                for kc in range(8):
                    s.dma("pool", lambda e, kc=kc, j=j: e.dma_start(
                        out=xTb[:, kc, j * 2048:(j + 1) * 2048],
                        in_=xT_d[kc * 128:(kc + 1) * 128, j * 2048:(j + 1) * 2048]),
                        writes=[("x", kc, j)])
            XR = [("x", kc, j) for kc in range(8) for j in range(3)]

            if stop_after >= 2:
                with ExitStack() as es_p:
                    s.loc = es_p
                    phase_attn(nc, s, xTb, XR, win_v, ab_d, hb_d, attnT_d)
                    s.barrier()
                    s.emit()
            if stop_after >= 3:
                with ExitStack() as es_p:
                    s.loc = es_p
                    phase_conv(nc, s, xTb, XR, win_v, wdw_d, bdw_d, clg_d, clb_d, convT_d, identb)
                    s.barrier()
                    s.emit()
            if stop_after < 3:
                s.barrier()
                s.emit()
            if stop_after >= 4:
                with ExitStack() as es_p:
                    s.loc = es_p
                    phase_merge(nc, s, xTb, XR, win_v, woa_d, woc_d, attnT_d, convT_d, mrgT_d, xs_d)
                    s.barrier()
                    s.emit()
        if stop_after >= 5:
            with ExitStack() as es_p:
                s.loc = es_p
                phase_out_router(nc, s, mrgT_d, wout_d, xown_d, ln1g_d, ln1b_d, wr_d, br_d, h1_d, xs_d,
                                 identf, idx_all, gk_all, G_all, rt_d)
                s.barrier()
                s.emit()
        if stop_after >= 6:
            with ExitStack() as es_p:
                s.loc = es_p
                phase_experts(nc, s, xs_d, y_d, wgu_d, bgu_d, wd_d, bd_d, identb)
                s.barrier()
                s.emit()
        if stop_after >= 7:
            with ExitStack() as es_p:
                s.loc = es_p
                phase_combine(nc, s, y_d, h1_d, bd_d, ln2g_d, ln2b_d, out_d, identf, idx_all, gk_all, G_all)
                s.barrier()
                s.emit()
    return nc


def phase_attn(nc, s, xTb, XR, win_v, ab_d, hb_d, attnT_d):
    acc = [s.sb(f"acc{h}", [128, 2048], F32) for h in range(2)]
    qT = [[s.sb(f"qT{b}_{h}", [128, 2048], BF16) for h in range(2)] for b in range(2)]
    kT = [s.sb(f"kT{b}", [128, 4096], BF16) for b in range(2)]
    vB = [s.sb(f"vB{b}", [128, 32, 2, 128], BF16) for b in range(2)]
    wq = s.sb("wq", [128, 8, 128], BF16)
    wk = s.sb("wk", [128, 8, 128], BF16)
    wv = s.sb("wv", [128, 8, 128], BF16)
    ab = s.sb("ab", [128, 3, 2, 256], F32)
    abh = s.sb("abh", [128, 3, 2, 128], F32)
    tmp = [s.sb(f"tmp{i}", [128, 512], F32) for i in range(3)]
    pt = [s.sb(f"pt{i}", [128, 512], BF16) for i in range(3)]
    rec = tmp[0][0:64, :]
    hb = s.sb("hb", [128, 1], F32)
    psA = [s.ps(f"psA{i}") for i in range(2)]
    psV = s.ps("psV")
    psS = [s.ps(f"psS{i}") for i in range(3)]
    psO = [s.ps("psO0"), s.ps("psO1")]

    s.dma("sp", lambda e: e.dma_start(out=hb[:], in_=hb_d), writes=["hb"])
    for b in range(2):
        s.op("pool", lambda e, b=b: e.memset(vB[b][:], 1.0), writes=[("v", b, i) for i in range(8)])
        for h in range(2):
            s.op("pool", lambda e, b=b, h=h: e.memset(qT[b][h][:], 0.0), writes=[("q", b, h, tc) for tc in range(4)])

    def xr(lo, hi):
        return [("x", kc, j) for kc in range(8) for j in range(lo // 2048, (hi - 1) // 2048 + 1)]

    cnt = {"pa": 0, "ev": 0, "si": 0}
    iters = [(hp, half, g) for hp in range(4) for half in range(2) for g in range(3)][:DBG["iters"]]

    def make_proj(idx):
        hp, half, g = iters[idx]
        dil = (1, 4, 16)[g]
        halo = 128 * dil
        p0 = HALO + half * 2048
        b = idx % 2
        nK = halo + 2048
        kb0 = p0 - halo
        bpr = 16 // dil + 1
        nblk = dil * bpr
        steps = []

        def loads():
            for (wt, base, nm) in ((wq, 0, "wq"), (wk, 1536, "wk"), (wv, 3072, "wv")):
                c0 = base + g * 512 + hp * 128
                s.dma("pool", lambda e, wt=wt, c0=c0: e.dma_start(out=wt[:], in_=win_v[:, :, c0:c0 + 128]), writes=[nm])
        steps.append(loads)

        def qstep(tc):
            def f():
                pi = cnt["pa"] % 2
                cnt["pa"] += 1
                ps, pkey = psA[pi], ("psA", pi)
                s.mm(ps[:, 0:512], [(wq[:, kc, :], xTb[:, kc, p0 + tc * 512:p0 + (tc + 1) * 512]) for kc in range(8)],
                     reads=xr(p0 + tc * 512, p0 + (tc + 1) * 512) + ["wq"], writes=[pkey])
                _copy(s, "act", qT[b][0][0:64, tc * 512:(tc + 1) * 512], ps[0:64, 0:512], reads=[pkey], writes=[("q", b, 0, tc)])
                _copy(s, "act", qT[b][1][64:128, tc * 512:(tc + 1) * 512], ps[64:128, 0:512], reads=[pkey],
                      writes=[("q", b, 1, tc)])
            return f
        for tc in range(4):
            steps.append(qstep(tc))

        def kstep(off, n, ci):
            def f():
                pi = cnt["pa"] % 2
                cnt["pa"] += 1
                ps, pkey = psA[pi], ("psA", pi)
                s.mm(ps[:, 0:n], [(wk[:, kc, :], xTb[:, kc, kb0 + off:kb0 + off + n]) for kc in range(8)],
                     reads=xr(kb0 + off, kb0 + off + n) + ["wk"], writes=[pkey])
                _copy(s, ("act", "act", "dve")[cnt["ev"] % 3], kT[b][:, off:off + n], ps[:, 0:n], reads=[pkey], writes=[("k", b, ci)])
                cnt["ev"] += 1
            return f
        off = 0
        ci = 0
        while off < nK:
            n = min(512, nK - off)
            steps.append(kstep(off, n, ci))
            off += n
            ci += 1

        def vstep(blk0):
            def f():
                nb = min(4, nblk - blk0)
                for j in range(nb):
                    blk = blk0 + j
                    r, mi = blk // bpr, blk % bpr
                    st = p0 + r + dil * 128 * (mi - 1)
                    s.mm(psV[:, j * 128:(j + 1) * 128],
                         [(xTb[:, kc, st:st + 127 * dil + 1:dil], wv[:, kc, :]) for kc in range(8)],
                         reads=xr(st, st + 127 * dil + 1) + ["wv"], writes=["psV"])
                _copy(s, ("act", "act", "dve")[cnt["ev"] % 3], vB[b][:, blk0:blk0 + nb, :, 0:64],
                      psV[:, 0:nb * 128].rearrange("p (a b c) -> p a b c", a=nb, b=2),
                      reads=["psV"], writes=[("v", b, blk0 // 4)])
                cnt["ev"] += 1
            return f
        for blk0 in range(0, nblk, 4):
            steps.append(vstep(blk0))
        return steps

    def emit_S2(pair, i, b, g, dil, halo, nK, half):
        hh = pair[0][0]
        pS, tm, pT = psS[i], tmp[i], pt[i]
        mms = []
        rd = []
        flags = []
        for ui, (_, r, m) in enumerate(pair):
            q0 = r + dil * 128 * m
            qap = qT[b][hh][:, q0:q0 + 127 * dil + 1:dil]
            kp = halo + r + dil * 128 * (m - 1)
            kc_ = halo + r + dil * 128 * m
            kprev = kT[b][:, kp:kp + 127 * dil + 1:dil]
            kcur = kT[b][:, kc_:kc_ + 127 * dil + 1:dil]
            rd += [("q", b, hh, c) for c in range(q0 // 512, (q0 + 128 * dil - 1) // 512 + 1)]
            rd += [("k", b, c) for c in range(kp // 512, min((kc_ + 128 * dil - 1) // 512, (nK - 1) // 512) + 1)]
            mms.append((pS[:, ui * 256:ui * 256 + 128], kprev, qap))
            mms.append((pS[:, ui * 256 + 128:ui * 256 + 256], kcur, qap))
            flags.append(half == 0 and m == 0)

        def fn(e):
            ins = first = None
            for (o_, l_, r_) in mms:
                ins = e.matmul(o_, lhsT=l_, rhs=r_, start=True, stop=True)
                first = first or ins
            return first, ins
        s.op("pe", fn, reads=list(dict.fromkeys(rd)), writes=[("psS", i)], attach=True)
        tkeys = [("tmp", i, 0), ("tmp", i, 1)]
        if not any(flags):
            in1 = ab[:, g, hh:hh + 1, :].broadcast_to([128, 2, 256])
            s.op("dve", lambda e: e.scalar_tensor_tensor(out=tm[:].rearrange("p (a b) -> p a b", a=2),
                                                         in0=pS[:, 0:512].rearrange("p (a b) -> p a b", a=2), scalar=0.125,
                                                         in1=in1, op0=ALU.mult, op1=ALU.add),
                 reads=[("psS", i), ("ab", g, hh)], writes=tkeys)
        else:
            for ui in range(2):
                c0 = ui * 256
                if flags[ui]:
                    s.op("dve", lambda e, c0=c0: e.scalar_tensor_tensor(out=tm[:, c0:c0 + 128], in0=pS[:, c0:c0 + 128], scalar=0.125,
                                                                        in1=abh[:, g, hh, :], op0=ALU.mult, op1=ALU.add),
                         reads=[("psS", i), ("abh", g, hh)], writes=[("tmp", i, ui)])
                    s.op("dve", lambda e, c0=c0: e.scalar_tensor_tensor(out=tm[:, c0 + 128:c0 + 256], in0=pS[:, c0 + 128:c0 + 256],
                                                                        scalar=0.125, in1=ab[:, g, hh, 128:256], op0=ALU.mult,
                                                                        op1=ALU.add),
                         reads=[("psS", i), ("ab", g, hh), ("tmp", i, ui)], writes=[("tmp", i, ui)])
                else:
                    s.op("dve", lambda e, c0=c0: e.scalar_tensor_tensor(out=tm[:, c0:c0 + 256], in0=pS[:, c0:c0 + 256], scalar=0.125,
                                                                        in1=ab[:, g, hh, :], op0=ALU.mult, op1=ALU.add),
                         reads=[("psS", i), ("ab", g, hh)], writes=[("tmp", i, ui)])
        s.op("act", lambda e: e.activation(out=pT[:], in_=tm[:], func=AF.Exp), reads=tkeys, writes=[("pt", i)])

    def emit_PV2(pair, i, o, b, g, dil, bpr):
        hh = pair[0][0]
        pT = pt[i]
        mms = []
        rd = [("pt", i)]
        for ui, (_, r, m) in enumerate(pair):
            bp = r * bpr + m
            po = psO[o][:, ui * 128:(ui + 1) * 128]
            mms.append((po, vB[b][:, bp, hh, :], pT[:, ui * 256:ui * 256 + 128], True, False))
            mms.append((po, vB[b][:, bp + 1, hh, :], pT[:, ui * 256 + 128:ui * 256 + 256], False, True))
            rd += [("v", b, bp // 4), ("v", b, (bp + 1) // 4)]

        def fn(e):
            ins = first = None
            for (o_, l_, r_, st_, sp_) in mms:
                ins = e.matmul(o_, lhsT=l_, rhs=r_, start=st_, stop=sp_)
                first = first or ins
            return first, ins
        s.op("pe", fn, reads=list(dict.fromkeys(rd)), writes=[("psO", o)], attach=True)
        av = acc[hh][:].rearrange("p (a j d) -> p a d j", a=16 // dil, j=128, d=dil)
        (_, r0_, m0_), (_, r1_, m1_) = pair
        if m1_ != m0_:
            aap = av[:, m0_:m0_ + 2, r0_, :]
        else:
            aap = av[:, m0_, r0_:r0_ + 2, :]
        pin = psO[o][:, 0:256].rearrange("p (a b) -> p a b", a=2)
        if g == 0:
            s.op("dve", lambda e: e.tensor_copy(out=aap, in_=pin), reads=[("psO", o)], writes=[("acc", hh)])
        else:
            s.op("dve", lambda e: e.tensor_tensor(out=aap, in0=pin, in1=aap, op=ALU.add),
                 reads=[("psO", o), ("acc", hh)], writes=[("acc", hh)])

    def load_ab(hp):
        for g2 in range(3):
            for hh in range(2):
                hd = g2 * 8 + hp * 2 + hh
                s.dma("sp", lambda e, g2=g2, hh=hh, hd=hd: e.dma_start(out=ab[:, g2, hh, :], in_=ab_d[hd]),
                      writes=[("ab", g2, hh)])
                s.op("pool", lambda e, g2=g2, hh=hh: e.tensor_scalar(out=abh[:, g2, hh, :], in0=ab[:, g2, hh, 0:128],
                                                                    scalar1=hb[:, 0:1], scalar2=None, op0=ALU.add),
                     reads=[("ab", g2, hh), "hb"], writes=[("abh", g2, hh)])

    load_ab(0)
    for f in make_proj(0):
        f()
    for idx, (hp, half, g) in enumerate(iters):
        dil = (1, 4, 16)[g]
        halo = 128 * dil
        b = idx % 2
        nK = halo + 2048
        bpr = 16 // dil + 1
        nxt = make_proj(idx + 1) if idx + 1 < len(iters) else []
        units = [(hh, r, m) for hh in range(2) for r in range(dil) for m in range(16 // dil)]
        pairs = [(units[k], units[k + 1]) for k in range(0, len(units), 2)]
        pend = []
        for pi2, pr in enumerate(pairs):
            i = cnt["si"] % 3
            cnt["si"] += 1
            emit_S2(pr, i, b, g, dil, halo, nK, half)
            pend.append((pr, i, pi2 % 2))
            if len(pend) > 2:
                pp, pi_, po_ = pend.pop(0)
                emit_PV2(pp, pi_, po_, b, g, dil, bpr)
            if nxt and pi2 == 0:
                nxt.pop(0)()
            if pi2 >= 5:
                for _ in range(2):
                    if nxt:
                        nxt.pop(0)()
        while pend:
            pp, pi_, po_ = pend.pop(0)
            emit_PV2(pp, pi_, po_, b, g, dil, bpr)
        if idx + 1 < len(iters) and iters[idx + 1][0] != hp:
            load_ab(iters[idx + 1][0])
        while nxt:
            nxt.pop(0)()
        if g == 2:
            for hh in range(2):
                for c in range(4):
                    cs = slice(c * 512, (c + 1) * 512)
                    s.op("dve", lambda e, hh=hh, cs=cs: e.tensor_copy(out=rec, in_=acc[hh][64:128, cs]),
                         reads=[("acc", hh)], writes=[("tmp", 0, 0), ("tmp", 0, 1)])
                    s.op("act", lambda e: e.activation(out=rec, in_=rec, func=AF.Ln),
                         reads=[("tmp", 0, 0), ("tmp", 0, 1)], writes=[("tmp", 0, 0), ("tmp", 0, 1)])
                    s.op("act", lambda e: e.activation(out=rec, in_=rec, func=AF.Exp, scale=-1.0),
                         reads=[("tmp", 0, 0), ("tmp", 0, 1)], writes=[("tmp", 0, 0), ("tmp", 0, 1)])
                    s.op("dve", lambda e, hh=hh, cs=cs: e.tensor_tensor(out=acc[hh][0:64, cs], in0=acc[hh][0:64, cs], in1=rec,
                                                                        op=ALU.mult),
                         reads=[("tmp", 0, 0), ("tmp", 0, 1), ("acc", hh)], writes=[("acc", hh)])
                row = (hp * 2 + hh) * 64
                s.dma("pool", lambda e, hh=hh, row=row, half=half: e.dma_start(
                    out=attnT_d[row:row + 64, half * 2048:(half + 1) * 2048], in_=acc[hh][0:64, :]),
                    reads=[("acc", hh)], writes=[("attnT_d", row, half)])


def phase_conv(nc, s, xTb, XR, win_v, wdw_d, bdw_d, clg_d, clb_d, convT_d, identb):
    dw = s.sb("dw", [128, 6, 2048], F32)
    glu = [s.sb(f"glu{i}", [128, 32 + 2048], BF16) for i in range(2)]
    dg = [s.sb(f"dg{i}", [128, 31, 128], BF16) for i in range(2)]
    wu = [s.sb(f"wu{i}", [128, 8, 128], BF16) for i in range(2)]
    wg = [s.sb(f"wg{i}", [128, 8, 128], BF16) for i in range(2)]
    sgt = [s.sb(f"sgt{i}", [128, 512], F32) for i in range(2)]
    sq = [s.sb(f"sq{i}", [128, 512], F32) for i in range(2)]
    sd = s.sb("sd", [128, 512], F32)
    rstd = s.sb("rstd", [128, 512], F32)
    cst = [s.sb(f"cst{i}", [128, 6, 512], BF16) for i in range(2)]
    wdw = s.sb("wdw", [128, 6, 31], F32)
    bdw = s.sb("bdw", [128, 6], F32)
    clg = s.sb("clg", [128, 6], F32)
    clb = s.sb("clb", [128, 6], F32)
    onesf = s.sb("onesf", [128, 128], F32)
    psU = [s.ps(f"psU{i}") for i in range(2)]
    psG = [s.ps(f"psG{i}") for i in range(2)]
    psM = s.ps("psM")
    psV2 = s.ps("psV2")
    psC = [s.ps("psC0"), s.ps("psC1")]
    for t, d_, nm in ((wdw, wdw_d, "wdw"), (bdw, bdw_d, "bdw"), (clg, clg_d, "clg"), (clb, clb_d, "clb")):
        s.dma("sp", lambda e, t=t, d_=d_: e.dma_start(out=t[:], in_=d_), writes=[nm])
    s.op("pool", lambda e: e.memset(onesf[:], 1.0 / 768.0), writes=["onesf"])
    convT_v = convT_d.rearrange("(cc p) t -> p cc t", p=128)
    it = 0
    pu = 0
    ci = 0
    for half in range(2):
        p0 = HALO + half * 2048
        for cc in range(6):
            b = it % 2
            it += 1
            cv = 4608 + cc * 128
            cg = 4608 + 768 + cc * 128
            s.dma("pool", lambda e, b=b, cv=cv: e.dma_start(out=wu[b][:], in_=win_v[:, :, cv:cv + 128]), writes=[("wu", b)])
            s.dma("pool", lambda e, b=b, cg=cg: e.dma_start(out=wg[b][:], in_=win_v[:, :, cg:cg + 128]), writes=[("wg", b)])
            for (off, n) in [(0, 32)] + [(32 + i * 512, 512) for i in range(4)]:
                pi = pu % 2
                pu += 1
                t0 = p0 - 32 + off
                s.mm(psU[pi][:, 0:n], [(wu[b][:, kc, :], xTb[:, kc, t0:t0 + n]) for kc in range(8)],
                     reads=XR + [("wu", b)], writes=[("psU", pi)])
                s.mm(psG[pi][:, 0:n], [(wg[b][:, kc, :], xTb[:, kc, t0:t0 + n]) for kc in range(8)],
                     reads=XR + [("wg", b)], writes=[("psG", pi)])
                s.op("act", lambda e, pi=pi, n=n: e.activation(out=sgt[pi][:, 0:n], in_=psG[pi][:, 0:n], func=AF.Sigmoid),
                     reads=[("psG", pi)], writes=[("sgt", pi)])
                s.op("dve", lambda e, pi=pi, n=n, off=off, b=b: e.tensor_tensor(out=glu[b][:, off:off + n], in0=psU[pi][:, 0:n],
                                                                                in1=sgt[pi][:, 0:n], op=ALU.mult),
                     reads=[("psU", pi), ("sgt", pi)], writes=[("glu", b)])
            for j in range(31):
                s.op("dve", lambda e, b=b, cc=cc, j=j: e.tensor_scalar(out=dg[b][:, j, :], in0=identb[:], scalar1=wdw[:, cc, j:j + 1],
                                                                       scalar2=None, op0=ALU.mult),
                     reads=["identb", "wdw"], writes=[("dg", b)])
            for tc in range(4):
                pc = (it * 4 + tc) % 2
                s.mm(psC[pc][:, :], [(dg[b][:, j, :], glu[b][:, 2 + j + tc * 512:2 + j + (tc + 1) * 512]) for j in range(31)],
                     reads=[("dg", b), ("glu", b)], writes=[("psC", pc)])
                s.op("dve", lambda e, cc=cc, tc=tc, pc=pc: e.tensor_scalar(out=dw[:, cc, tc * 512:(tc + 1) * 512], in0=psC[pc][:, :],
                                                                          scalar1=bdw[:, cc:cc + 1], scalar2=None, op0=ALU.add),
                     reads=[("psC", pc), "bdw"], writes=[("dw", cc)])
        for tc in range(4):
            ts_ = slice(tc * 512, (tc + 1) * 512)
            s.mm(psM[:, :], [(onesf[:], dw[:, cc, ts_]) for cc in range(6)], reads=[("dw", cc) for cc in range(6)] + ["onesf"],
                 writes=["psM"])
            for cc in range(6):
                s.op("dve", lambda e, cc=cc, ts_=ts_: e.tensor_tensor(out=dw[:, cc, ts_], in0=dw[:, cc, ts_], in1=psM[:, :],
                                                                      op=ALU.subtract),
                     reads=["psM", ("dw", cc)], writes=[("dw", cc)])
            sqt = []
            for cc in range(6):
                qi = cc % 2
                s.op("act", lambda e, cc=cc, qi=qi, ts_=ts_: e.activation(out=sq[qi][:], in_=dw[:, cc, ts_], func=AF.Square),
                     reads=[("dw", cc)], writes=[("sq", qi)])
                def fn(e, cc=cc, qi=qi):
                    return e.matmul(psV2[:, :], lhsT=onesf[:], rhs=sq[qi][:], start=(cc == 0), stop=(cc == 5))
                s.op("pe", fn, reads=[("sq", qi), "onesf"], writes=["psV2"], attach=True)
            s.op("act", lambda e: e.activation(out=sd[:], in_=psV2[:, :], func=AF.Sqrt, bias=EPS), reads=["psV2"], writes=["sd"])
            s.op("dve", lambda e: e.reciprocal(out=rstd[:], in_=sd[:]), reads=["sd"], writes=["rstd"])
            cb = ci % 2
            ci += 1
            for cc in range(6):
                s.op("dve", lambda e, cc=cc, ts_=ts_: e.tensor_tensor(out=dw[:, cc, ts_], in0=dw[:, cc, ts_], in1=rstd[:],
                                                                      op=ALU.mult),
                     reads=["rstd", ("dw", cc)], writes=[("dw", cc)])
                s.op("dve", lambda e, cc=cc, ts_=ts_: e.tensor_scalar(out=dw[:, cc, ts_], in0=dw[:, cc, ts_], scalar1=clg[:, cc:cc + 1],
                                                                      scalar2=clb[:, cc:cc + 1], op0=ALU.mult, op1=ALU.add),
                     reads=[("dw", cc), "clg", "clb"], writes=[("dw", cc)])
                s.op("act", lambda e, cc=cc, ts_=ts_, cb=cb: e.activation(out=cst[cb][:, cc, :], in_=dw[:, cc, ts_], func=AF.Silu),
                     reads=[("dw", cc)], writes=[("cst", cb)])
            t0 = half * 2048 + tc * 512
            s.dma("sp", lambda e, cb=cb, t0=t0: e.dma_start(out=convT_v[:, :, t0:t0 + 512], in_=cst[cb][:]),
                  reads=[("cst", cb)], writes=[("convT_d", t0)])


def phase_merge(nc, s, xTb, XR, win_v, woa_d, woc_d, attnT_d, convT_d, mrgT_d, xs_d):
    woa = s.sb("woa", [128, 4, D], BF16)
    woc = s.sb("woc", [128, 6, D], BF16)
    wga = s.sb("wga", [128, 8, D], BF16)
    wgc = s.sb("wgc", [128, 8, D], BF16)
    at = [s.sb(f"at{i}", [128, 4, 512], BF16) for i in range(2)]
    cv = [s.sb(f"cv{i}", [128, 6, 512], BF16) for i in range(1)]
    mg = [s.sb(f"mg{i}", [128, 8, 512], BF16) for i in range(1)]
    sga = [s.sb(f"sga{i}", [128, 512], F32) for i in range(2)]
    sgc = [s.sb(f"sgc{i}", [128, 512], F32) for i in range(2)]
    psa = [s.ps(f"psa{i}") for i in range(2)]
    psc = [s.ps(f"psc{i}") for i in range(2)]
    psga = [s.ps(f"psga{i}") for i in range(2)]
    psgc = [s.ps(f"psgc{i}") for i in range(2)]
    for kc in range(8):
        s.dma("pool", lambda e, kc=kc: e.dma_start(out=wga[:, kc, :], in_=win_v[:, kc, 6144:7168]), writes=[("wga", kc)])
    for kc in range(8):
        s.dma("pool", lambda e, kc=kc: e.dma_start(out=wgc[:, kc, :], in_=win_v[:, kc, 7168:8192]), writes=[("wgc", kc)])
    s.dma("pool", lambda e: e.dma_start(out=woa[:], in_=woa_d.rearrange("(h p) n -> p h n", p=128)), writes=["woa"])
    s.dma("pool", lambda e: e.dma_start(out=woc[:], in_=woc_d.rearrange("(c p) n -> p c n", p=128)), writes=["woc"])
    zt = s.sb("zt", [128, D], BF16)
    s.op("dve", lambda e: e.memset(zt[:], 0.0), writes=["zt"])
    xs_z = xs_d.rearrange("(p j) d -> p j d", p=128)
    nrow = (NE * CAP + 128) // 128
    zq = list(range(nrow))

    def zero_some(n):
        for _ in range(n):
            if zq:
                c = zq.pop(0)
                s.dma("act", lambda e, c=c: e.dma_start(out=xs_z[:, c, :], in_=zt[:]), reads=["zt"], writes=[("xs_zero", c)])
    WGA = [("wga", kc) for kc in range(8)]
    WGC = [("wgc", kc) for kc in range(8)]
    attn_v = attnT_d.rearrange("(h p) t -> p h t", p=128)
    conv_v = convT_d.rearrange("(c p) t -> p c t", p=128)
    mrg_v = mrgT_d.rearrange("(c p) t -> p c t", p=128)
    pi = 0
    for tc in range(8):
        b = tc % 2
        ts_ = slice(tc * 512, (tc + 1) * 512)
        xs_ = slice(HALO + tc * 512, HALO + (tc + 1) * 512)
        s.dma("sp", lambda e, b=b, ts_=ts_: e.dma_start(out=at[b][:], in_=attn_v[:, :, ts_]), writes=[("at", b)])
        s.dma("sp", lambda e, ts_=ts_: e.dma_start(out=cv[0][:], in_=conv_v[:, :, ts_]), writes=[("cv", 0)])
        for fc in range(8):
            fs = slice(fc * 128, (fc + 1) * 128)
            p = pi % 2
            pi += 1
            s.mm(psga[p][:, :], [(wga[:, kc, fs], xTb[:, kc, xs_]) for kc in range(8)], reads=XR + WGA, writes=[("psga", p)])
            s.mm(psgc[p][:, :], [(wgc[:, kc, fs], xTb[:, kc, xs_]) for kc in range(8)], reads=XR + WGC, writes=[("psgc", p)])
            s.mm(psa[p][:, :], [(woa[:, h, fs], at[b][:, h, :]) for h in range(4)], reads=["woa", ("at", b)],
                 writes=[("psa", p)])
            s.mm(psc[p][:, :], [(woc[:, c, fs], cv[0][:, c, :]) for c in range(6)], reads=["woc", ("cv", 0)],
                 writes=[("psc", p)])
            s.op("act", lambda e, p=p: e.activation(out=sga[p][:], in_=psga[p][:, :], func=AF.Sigmoid),
                 reads=[("psga", p)], writes=[("sga", p)])
            s.op("act", lambda e, p=p: e.activation(out=sgc[p][:], in_=psgc[p][:, :], func=AF.Sigmoid),
                 reads=[("psgc", p)], writes=[("sgc", p)])
            s.op("dve", lambda e, p=p: e.tensor_tensor(out=sga[p][:], in0=psa[p][:, :], in1=sga[p][:], op=ALU.mult),
                 reads=[("psa", p), ("sga", p)], writes=[("sga", p)])
            s.op("dve", lambda e, p=p: e.tensor_tensor(out=sgc[p][:], in0=psc[p][:, :], in1=sgc[p][:], op=ALU.mult),
                 reads=[("psc", p), ("sgc", p)], writes=[("sgc", p)])
            s.op("dve", lambda e, p=p, fc=fc: e.tensor_tensor(out=mg[0][:, fc, :], in0=sga[p][:], in1=sgc[p][:], op=ALU.add),
                 reads=[("sga", p), ("sgc", p)], writes=[("mg", 0)])
            zero_some(3)
        s.dma("sp", lambda e, ts_=ts_: e.dma_start(out=mrg_v[:, :, ts_], in_=mg[0][:]), reads=[("mg", 0)],
              writes=[("mrg_d", tc)])
    zero_some(len(zq))


def phase_out_router(nc, s, mrgT_d, wout_d, xown_d, ln1g_d, ln1b_d, wr_d, br_d, h1_d, xs_d,
                     identf, idx_all, gk_all, G_all, rt_d):
    wout = s.sb("wout", [128, 8, D], BF16)
    lng = s.sb("lng", [128, D], F32)
    lnb = s.sb("lnb", [128, D], F32)
    wr = s.sb("wr", [128, 8, NE], F32)
    brb = s.sb("brb", [128, NE], F32)
    mg = [s.sb(f"mgc{i}", [128, 8, 512], BF16) for i in range(2)]
    xo = [s.sb(f"xo{i}", [128, D], F32) for i in range(2)]
    z = [s.sb(f"z{i}", [128, D], F32) for i in range(2)]
    h1 = [s.sb(f"h1{i}", [128, D], F32) for i in range(2)]
    h1b = [s.sb(f"h1b{i}", [128, D], BF16) for i in range(2)]
    h1T = [s.sb(f"h1T{i}", [128, 8, 128], F32) for i in range(2)]

    def two(name, shape, dt=F32):
        return [s.sb(f"{name}{i}", shape, dt) for i in range(2)]
    st6 = two("st6", [128, 2, 6])
    mv = two("mv", [128, 2])
    rs = two("rs", [128, 1])
    lg = two("lg", [128, NE])
    m8 = two("m8", [128, 8])
    selm = two("selm", [128, NE])
    selb = two("selb", [128, NE], BF16)
    ex = two("ex", [128, NE])
    den = two("den", [128, 1])
    rden = two("rden", [128, 1])
    pos = two("pos", [128, NE])
    key = two("key", [128, NE])
    k8 = two("k8", [128, 8])
    junk = two("junk", [128, NE])
    run = s.sb("run", [128, NE], F32)
    ustr = s.sb("ustr", [128, 128], BF16)
    onesb = s.sb("onesb", [128, 128], BF16)
    rtst = s.sb("rtst", [128, 32, 8], F32)
    pso = [[s.ps(f"pso{i}_{h}") for h in range(2)] for i in range(2)]
    pst = [s.ps(f"pst{i}") for i in range(2)]
    pslp = [s.ps(f"pslp{i}") for i in range(2)]

    for kc in range(8):
        s.dma("pool", lambda e, kc=kc: e.dma_start(out=wout[:, kc, :], in_=wout_d[kc * 128:(kc + 1) * 128, :]),
              writes=[("wout", kc)])
    WO = [("wout", kc) for kc in range(8)]
    s.dma("sp", lambda e: e.dma_start(out=lng[:], in_=ln1g_d.broadcast_to([128, D])), writes=["lng"])
    s.dma("sp", lambda e: e.dma_start(out=lnb[:], in_=ln1b_d.broadcast_to([128, D])), writes=["lnb"])
    s.dma("sp", lambda e: e.dma_start(out=wr[:], in_=wr_d.rearrange("(kc p) n -> p kc n", p=128)), writes=["wr"])
    s.dma("sp", lambda e: e.dma_start(out=brb[:], in_=br_d.broadcast_to([128, NE])), writes=["brb"])
    s.op("pool", lambda e: e.memset(ustr[:], 1.0), writes=["ustr"])
    s.op("pool", lambda e: e.affine_select(out=ustr[:], in_=ustr[:], pattern=[[1, 128]], compare_op=ALU.is_gt, fill=0.0,
                                           base=0, channel_multiplier=-1), reads=["ustr"], writes=["ustr"])
    s.op("pool", lambda e: e.memset(onesb[:], 1.0), writes=["onesb"])
    s.op("pool", lambda e: e.iota(run[:], pattern=[[CAP, NE]], base=1, channel_multiplier=0,
                                  allow_small_or_imprecise_dtypes=True), writes=["run"])
    mrg_v = mrgT_d.rearrange("(c p) t -> p c t", p=128)

    def stage_a(ti):
        tc, tt = ti // 4, ti % 4
        b = tc % 2
        tb = ti % 2
        r0 = ti * 128
        if tt == 0:
            ts_ = slice(tc * 512, (tc + 1) * 512)
            s.dma("sp", lambda e: e.dma_start(out=mg[b][:], in_=mrg_v[:, :, ts_]), writes=[("mgc", b)])
        s.dma("sp", lambda e: e.dma_start(out=xo[tb][:], in_=xown_d[r0:r0 + 128, :]), writes=[("xo", tb)])
        for hf in range(2):
            hs = slice(hf * 512, (hf + 1) * 512)
            s.mm(pso[tb][hf][:, :], [(mg[b][:, kc, tt * 128:(tt + 1) * 128], wout[:, kc, hs]) for kc in range(8)],
                 reads=[("mgc", b)] + WO, writes=[("pso", tb, hf)])
            s.op("dve", lambda e, hf=hf, hs=hs: e.scalar_tensor_tensor(out=z[tb][:, hs], in0=xo[tb][:, hs], scalar=ALPHA,
                                                                       in1=pso[tb][hf][:, :], op0=ALU.mult, op1=ALU.add),
                 reads=[("xo", tb), ("pso", tb, hf)], writes=[("z", tb, hf)])
            s.op("dve", lambda e, hf=hf, hs=hs: e.bn_stats(out=st6[tb][:, hf, :], in_=z[tb][:, hs]),
                 reads=[("z", tb, hf)], writes=[("st6", tb, hf)])
        _ln_tail(s, z[tb], [("z", tb, 0), ("z", tb, 1)], st6[tb], [("st6", tb, 0), ("st6", tb, 1)], mv[tb], rs[tb],
                 lng, lnb, h1[tb], ("h1", tb), f"r{tb}")
        s.dma("sp", lambda e: e.dma_start(out=h1_d[r0:r0 + 128, :], in_=h1[tb][:]), reads=[("h1", tb)], writes=[("h1_d", ti)])
        s.op("act", lambda e: e.activation(out=h1b[tb][:], in_=h1[tb][:], func=AF.Identity), reads=[("h1", tb)],
             writes=[("h1b", tb)])
        for hf in range(2):
            def fn(e, hf=hf):
                ins = None
                for k in range(4):
                    kc = hf * 4 + k
                    ins = e.transpose(out=pst[hf][:, k * 128:(k + 1) * 128], in_=h1[tb][:, kc * 128:(kc + 1) * 128],
                                      identity=identf[:])
                return ins
            s.op("pe", fn, reads=[("h1", tb), "identf"], writes=[("pst", hf)])
            _copy(s, "act", h1T[tb][:, hf * 4:(hf + 1) * 4, :], pst[hf][:, :].rearrange("p (a b) -> p a b", a=4),
                  reads=[("pst", hf)], writes=[("h1T", tb, hf)])

    def stage_b(ti):
        tb = ti % 2
        P = pslp[tb]
        pk = ("pslp", tb)
        lg_, m8_, sel_, selb_, ex_, den_, rden_, pos_, key_, k8_, junk_ = (lg[tb], m8[tb], selm[tb], selb[tb], ex[tb], den[tb],
                                                                          rden[tb], pos[tb], key[tb], k8[tb], junk[tb])
        T = lambda n: (n, tb)
        s.mm(P[:, 0:NE], [(h1T[tb][:, kc, :], wr[:, kc, :]) for kc in range(8)], reads=[("h1T", tb, 0), ("h1T", tb, 1), "wr"],
             writes=[pk])
        s.op("dve", lambda e: e.tensor_tensor(out=lg_[:], in0=P[:, 0:NE], in1=brb[:], op=ALU.add), reads=[pk, "brb"],
             writes=[T("lg")])
        s.op("dve", lambda e: e.max(out=m8_[:], in_=lg_[:]), reads=[T("lg")], writes=[T("m8")])
        s.op("dve", lambda e: e.tensor_scalar(out=sel_[:], in0=lg_[:], scalar1=m8_[:, 3:4], scalar2=None, op0=ALU.is_ge),
             reads=[T("lg"), T("m8")], writes=[T("selm")])
        s.op("dve", lambda e: e.tensor_scalar(out=ex_[:], in0=lg_[:], scalar1=m8_[:, 0:1], scalar2=None, op0=ALU.subtract),
             reads=[T("lg"), T("m8")], writes=[T("ex")])
        s.op("act", lambda e: e.activation(out=ex_[:], in_=ex_[:], func=AF.Exp), reads=[T("ex")], writes=[T("ex")])
        s.op("dve", lambda e: e.scalar_tensor_tensor(out=ex_[:], in0=ex_[:], scalar=1.0, in1=sel_[:], op0=ALU.mult,
                                                     op1=ALU.mult, accum_out=den_[:]),
             reads=[T("ex"), T("selm")], writes=[T("ex"), T("den")])
        s.op("dve", lambda e: e.reciprocal(out=rden_[:], in_=den_[:]), reads=[T("den")], writes=[T("rden")])
        s.op("dve", lambda e: e.tensor_scalar(out=G_all[:, ti, :], in0=ex_[:], scalar1=rden_[:, 0:1], scalar2=None,
                                              op0=ALU.mult),
             reads=[T("ex"), T("rden")], writes=[("G", ti)])
        s.op("act", lambda e: e.activation(out=selb_[:], in_=sel_[:], func=AF.Identity), reads=[T("selm")], writes=[T("selb")])

        def fnp(e):
            e.matmul(P[:, 64:64 + NE], lhsT=ustr[:], rhs=selb_[:], start=True, stop=True)
            return e.matmul(P[:, 128:128 + NE], lhsT=onesb[:], rhs=selb_[:], start=True, stop=True)
        s.op("pe", fnp, reads=["ustr", "onesb", T("selb"), T("lg")], writes=[pk])
        s.op("dve", lambda e: e.tensor_tensor(out=pos_[:], in0=P[:, 64:64 + NE], in1=run[:], op=ALU.add),
             reads=[pk, "run"], writes=[T("pos")])
        s.op("dve", lambda e: e.tensor_tensor(out=run[:], in0=P[:, 128:128 + NE], in1=run[:], op=ALU.add),
             reads=[pk, "run", T("pos")], writes=["run"])
        s.op("dve", lambda e: e.tensor_tensor(out=key_[:], in0=pos_[:], in1=sel_[:], op=ALU.mult),
             reads=[T("pos"), T("selm")], writes=[T("key")])
        s.op("dve", lambda e: e.max(out=k8_[:], in_=key_[:]), reads=[T("key")], writes=[T("k8")])
        s.op("dve", lambda e: e.tensor_scalar(out=idx_all[:, ti * 4:ti * 4 + 4], in0=k8_[:, 0:4], scalar1=-1.0, scalar2=None,
                                              op0=ALU.add),
             reads=[T("k8")], writes=[("idx", ti)])
        for k in range(4):
            s.op("dve", lambda e, k=k: e.scalar_tensor_tensor(out=junk_[:], in0=key_[:], scalar=k8_[:, k:k + 1],
                                                              in1=G_all[:, ti, :], op0=ALU.is_equal, op1=ALU.mult,
                                                              accum_out=gk_all[:, ti, k:k + 1]),
                 reads=[T("key"), T("k8"), ("G", ti), T("junk")], writes=[T("junk"), ("gk", ti, k)])
        if DBG.get("rt"):
            s.op("dve", lambda e: e.tensor_copy(out=rtst[:, ti, 0:4], in_=k8_[:, 0:4]), reads=[T("k8")], writes=[("rt", ti, 0)])
            s.op("dve", lambda e: e.tensor_copy(out=rtst[:, ti, 4:8], in_=gk_all[:, ti, :]),
                 reads=[("gk", ti, k) for k in range(4)], writes=[("rt", ti, 1)])
        for k in range(4):
            s.dma("pool", lambda e, k=k: e.indirect_dma_start(
                out=xs_d, out_offset=bass.IndirectOffsetOnAxis(ap=idx_all[:, ti * 4 + k:ti * 4 + k + 1], axis=0),
                in_=h1b[tb][:, :], in_offset=None, bounds_check=_bc(s, e), oob_is_err=False),
                reads=[("h1b", tb), ("idx", ti)], writes=[("xs_d", ti, k)])

    stage_a(0)
    for ti in range(32):
        if ti + 1 < 32:
            stage_a(ti + 1)
        stage_b(ti)
    if DBG.get("rt"):
        s.dma("sp", lambda e: e.dma_start(out=rt_d, in_=rtst[:]), reads=[("rt", ti, j) for ti in range(32) for j in range(2)],
              writes=["rt_d"])


def _ln_tail(s, z, zkeys, st6, skeys, mv, rs, lng, lnb, out, okey, tag):
    s.op("dve", lambda e: e.bn_aggr(out=mv[:], in_=st6[:].rearrange("p a b -> p (a b)")), reads=skeys, writes=["mv" + tag])
    s.op("act", lambda e: e.activation(out=rs[:], in_=mv[:, 1:2], func=AF.Ln, bias=EPS), reads=["mv" + tag], writes=["rs0" + tag])
    s.op("act", lambda e: e.activation(out=rs[:], in_=rs[:], func=AF.Exp, scale=-0.5), reads=["rs0" + tag], writes=["rs" + tag])
    s.op("dve", lambda e: e.scalar_tensor_tensor(out=z[:], in0=z[:], scalar=mv[:, 0:1], in1=lng[:], op0=ALU.subtract,
                                                 op1=ALU.mult),
         reads=zkeys + ["mv" + tag, "lng"], writes=zkeys)
    s.op("dve", lambda e: e.scalar_tensor_tensor(out=out[:], in0=z[:], scalar=rs[:, 0:1], in1=lnb[:], op0=ALU.mult,
                                                 op1=ALU.add),
         reads=zkeys + ["rs" + tag, "lnb"], writes=[okey])


def phase_experts(nc, s, xs_d, y_d, wgu_d, bgu_d, wd_d, bd_d, identb):
    wgu = [s.sb(f"wgu{i}", [128, 8, 2048], BF16) for i in range(2)]
    wdn = [s.sb(f"wdn{i}", [128, 8, D], BF16) for i in range(2)]
    bgu = [s.sb(f"bgu{i}", [128, 16], F32) for i in range(2)]
    bdb = [s.sb(f"bdb{i}", [128, D], F32) for i in range(2)]
    bgu1 = [s.sb(f"bgu1_{i}", [128, 8], F32) for i in range(2)]
    xs = [s.sb(f"xs{i}", [128, 6, D], BF16) for i in range(2)]
    xTe = s.sb("xTe", [128, 8, CAPT], BF16)
    aT = s.sb("aT", [128, 8, CAPT], BF16)
    NH = CAP // 2
    gt = [s.sb(f"gt{i}", [128, NH], F32) for i in range(2)]
    sg = [s.sb(f"sg{i}", [128, NH], F32) for i in range(2)]
    ut = [s.sb(f"ut{i}", [128, NH], F32) for i in range(2)]
    yst = [s.sb(f"yst{i}", [128, D], F32) for i in range(2)]
    NST = 4
    stg = [s.sb(f"stg{i}", [128, 2048], F32) for i in range(NST)]
    pstr = [s.ps(f"pstr{i}", [128, 1024], BF16) for i in range(2)]
    psg = [s.ps(f"psg{i}") for i in range(2)]
    psu = [s.ps(f"psu{i}") for i in range(2)]
    psy = [s.ps(f"psy{i}") for i in range(2)]

    chunks = []
    for ex in range(NE):
        chunks += [(ex, 0, kc) for kc in range(8)] + [(ex, 1, kc) for kc in range(8)]
    st = {"dma": 0, "cast": 0}

    def emit_dma():
        c = st["dma"]
        if c >= len(chunks):
            return
        st["dma"] = c + 1
        ex, kind, kc = chunks[c]
        t = c % NST
        if kind == 0:
            s.dma("sp", lambda e: e.dma_start(out=stg[t][:, :], in_=wgu_d[ex, kc * 128:(kc + 1) * 128, :]), writes=[("stg", t)])
        else:
            s.dma("sp", lambda e: e.dma_start(out=stg[t][:, 0:D], in_=wd_d[ex, kc * 128:(kc + 1) * 128, :]), writes=[("stg", t)])

    def pump():
        c = st["cast"]
        if c >= len(chunks):
            return
        while st["dma"] < min(c + NST, len(chunks)):
            emit_dma()
        st["cast"] = c + 1
        ex, kind, kc = chunks[c]
        t = c % NST
        b = ex % 2
        if kind == 0:
            s.op("act", lambda e: e.activation(out=wgu[b][:, kc, :], in_=stg[t][:, :], func=AF.Identity), reads=[("stg", t)],
                 writes=[("wgu", b, kc)])
        else:
            s.op("act", lambda e: e.activation(out=wdn[b][:, kc, :], in_=stg[t][:, 0:D], func=AF.Identity), reads=[("stg", t)],
                 writes=[("wdn", b, kc)])

    def small_loads(ex):
        b = ex % 2
        s.dma("sp", lambda e: e.dma_start(out=bgu[b][:], in_=bgu_d[ex]), writes=[("bgu", b)])
        s.op("dve", lambda e: e.tensor_scalar(out=bgu1[b][:], in0=bgu[b][:, 8:16], scalar1=1.0, scalar2=None, op0=ALU.add),
             reads=[("bgu", b)], writes=[("bgu1", b)])
        s.dma("sp", lambda e: e.dma_start(out=bdb[b][:], in_=bd_d[ex:ex + 1, :].broadcast_to([128, D])), writes=[("bdb", b)])
        s.dma("sp", lambda e: e.dma_start(
            out=xs[b][:], in_=xs_d[ex * CAP:ex * CAP + CAPT, :].rearrange("(j p) d -> p j d", p=128)), writes=[("xs", b)])

    s.op("pool", lambda e: e.memset(aT[:], 0.0), writes=[("aT", fc, h) for fc in range(8) for h in range(2)])
    small_loads(0)
    for _ in range(16):
        pump()
    ti_ = 0
    gi = 0
    yi = 0
    for ex in range(NE):
        b = ex % 2
        if ex + 1 < NE:
            small_loads(ex + 1)
        WGU = [("wgu", b, kc) for kc in range(8)]
        WDN = [("wdn", b, kc) for kc in range(8)]
        for j in range(6):
            p = ti_ % 2
            ti_ += 1

            def fn(e, p=p, j=j, b=b):
                ins = None
                for kc in range(8):
                    ins = e.transpose(out=pstr[p][:, kc * 128:(kc + 1) * 128], in_=xs[b][:, j, kc * 128:(kc + 1) * 128],
                                      identity=identb[:])
                return ins
            s.op("pe", fn, reads=[("xs", b), "identb"], writes=[("pstr", p)])
            _copy(s, "act", xTe[:, :, j * 128:(j + 1) * 128], pstr[p][:, :].rearrange("p (a b) -> p a b", a=8),
                  reads=[("pstr", p)], writes=[("xTe", j)])
        for fcp in range(8):
            for nh in range(2):
                p = gi % 2
                gi += 1
                ns = slice(nh * NH, (nh + 1) * NH)
                xk = [("xTe", j) for j in ((0, 1, 2) if nh == 0 else (2, 3, 4, 5))]
                s.mm(psg[p][:, 0:NH], [(wgu[b][:, kc, fcp * 128:(fcp + 1) * 128], xTe[:, kc, ns]) for kc in range(8)],
                     reads=WGU + xk, writes=[("psg", p)])
                s.mm(psu[p][:, 0:NH], [(wgu[b][:, kc, D + fcp * 128:D + (fcp + 1) * 128], xTe[:, kc, ns]) for kc in range(8)],
                     reads=WGU + xk, writes=[("psu", p)])
                s.op("dve", lambda e, p=p, b=b, fcp=fcp: e.tensor_scalar(out=gt[p][:], in0=psg[p][:, 0:NH],
                                                                        scalar1=bgu[b][:, fcp:fcp + 1], scalar2=7.0,
                                                                        op0=ALU.add, op1=ALU.min),
                     reads=[("psg", p), ("bgu", b)], writes=[("gt", p)])
                s.op("act", lambda e, p=p: e.activation(out=sg[p][:], in_=gt[p][:], func=AF.Sigmoid, scale=1.702),
                     reads=[("gt", p)], writes=[("sg", p)])
                pump()
                s.op("dve", lambda e, p=p, b=b, fcp=fcp: e.tensor_scalar(out=ut[p][:], in0=psu[p][:, 0:NH],
                                                                        scalar1=bgu1[b][:, fcp:fcp + 1], scalar2=8.0,
                                                                        op0=ALU.add, op1=ALU.min),
                     reads=[("psu", p), ("bgu1", b)], writes=[("ut", p)])
                s.op("dve", lambda e, p=p: e.scalar_tensor_tensor(out=ut[p][:], in0=ut[p][:], scalar=-6.0, in1=gt[p][:],
                                                                  op0=ALU.max, op1=ALU.mult),
                     reads=[("ut", p), ("gt", p)], writes=[("ut", p)])
                s.op("dve", lambda e, p=p, fcp=fcp, ns=ns: e.tensor_tensor(out=aT[:, fcp, ns], in0=ut[p][:], in1=sg[p][:],
                                                                           op=ALU.mult),
                     reads=[("sg", p), ("ut", p)], writes=[("aT", fcp, nh)])
        for j in range(6):
            yb = yi % 2
            yi += 1
            for dh in range(2):
                s.mm(psy[dh][:, :], [(aT[:, fc, j * 128:(j + 1) * 128], wdn[b][:, fc, dh * 512:(dh + 1) * 512]) for fc in range(8)],
                     reads=WDN + [("aT", fc, h) for fc in range(8) for h in ((0,) if j < 2 else ((0, 1) if j == 2 else (1,)))],
                     writes=[("psy", dh)])
                s.op("dve", lambda e, yb=yb, dh=dh, b=b: e.tensor_tensor(out=yst[yb][:, dh * 512:(dh + 1) * 512], in0=psy[dh][:, :],
                                                                         in1=bdb[b][:, dh * 512:(dh + 1) * 512], op=ALU.add),
                     reads=[("psy", dh), ("bdb", b)], writes=[("yst", yb, dh)])
            r0 = ex * CAP + j * 128
            nr = min(128, CAP - j * 128)
            s.dma("sp", lambda e, yb=yb, r0=r0, nr=nr: e.dma_start(out=y_d[r0:r0 + nr, :], in_=yst[yb][0:nr, :]),
                  reads=[("yst", yb, 0), ("yst", yb, 1)], writes=[("y_d", r0)])


def phase_combine(nc, s, y_d, h1_d, bd_d, ln2g_d, ln2b_d, out_d, identf, idx_all, gk_all, G_all):
    lng = s.sb("lng2", [128, D], F32)
    lnb = s.sb("lnb2", [128, D], F32)
    yk = [[s.sb(f"yk{i}_{k}", [128, D], F32) for k in range(4)] for i in range(3)]
    h1 = [s.sb(f"h1c{i}", [128, D], F32) for i in range(3)]
    z = [s.sb(f"zc{i}", [128, D], F32) for i in range(3)]
    ot = [s.sb(f"ot{i}", [128, D], F32) for i in range(3)]
    st6 = [s.sb(f"st6c{i}", [128, 2, 6], F32) for i in range(3)]
    mv = [s.sb(f"mvc{i}", [128, 2], F32) for i in range(3)]
    rs = [s.sb(f"rsc{i}", [128, 1], F32) for i in range(3)]
    s.dma("sp", lambda e: e.dma_start(out=lng[:], in_=ln2g_d.broadcast_to([128, D])), writes=["lng"])
    s.dma("sp", lambda e: e.dma_start(out=lnb[:], in_=ln2b_d.broadcast_to([128, D])), writes=["lnb"])
    for ti in range(32):
        b = ti % 3
        r0 = ti * 128
        s.dma("sp", lambda e, b=b, r0=r0: e.dma_start(out=h1[b][:], in_=h1_d[r0:r0 + 128, :]), writes=[("h1c", b)])
        for k in range(4):
            s.dma("pool", lambda e, b=b, ti=ti, k=k: e.indirect_dma_start(
                out=yk[b][k][:, :], out_offset=None, in_=y_d,
                in_offset=bass.IndirectOffsetOnAxis(ap=idx_all[:, ti * 4 + k:ti * 4 + k + 1], axis=0),
                bounds_check=_bc(s, e), oob_is_err=False), writes=[("yk", b, k)])
        zk = [("zc", b)]
        s.op("dve", lambda e, b=b, ti=ti: e.tensor_scalar(out=z[b][:], in0=yk[b][0][:], scalar1=gk_all[:, ti, 0:1], scalar2=None,
                                                          op0=ALU.mult),
             reads=[("yk", b, 0)], writes=zk)
        for k in range(1, 4):
            s.op("dve", lambda e, b=b, ti=ti, k=k: e.scalar_tensor_tensor(out=z[b][:], in0=yk[b][k][:], scalar=gk_all[:, ti, k:k + 1],
                                                                           in1=z[b][:], op0=ALU.mult, op1=ALU.add),
                 reads=[("yk", b, k)] + zk, writes=zk)
        s.op("dve", lambda e, b=b: e.scalar_tensor_tensor(out=z[b][:], in0=h1[b][:], scalar=ALPHA, in1=z[b][:], op0=ALU.mult,
                                                          op1=ALU.add),
             reads=[("h1c", b)] + zk, writes=zk)
        for hf in range(2):
            s.op("dve", lambda e, b=b, hf=hf: e.bn_stats(out=st6[b][:, hf, :], in_=z[b][:, hf * 512:(hf + 1) * 512]),
                 reads=zk, writes=[("st6", b, hf)])
        _ln_tail(s, z[b], zk, st6[b], [("st6", b, 0), ("st6", b, 1)], mv[b], rs[b], lng, lnb, ot[b], ("ot", b), f"c{b}")
        s.dma("sp", lambda e, b=b, r0=r0: e.dma_start(out=out_d[r0:r0 + 128, :], in_=ot[b][:]), reads=[("ot", b)],
              writes=[("out_d", ti)])


def _t5_bucket(dist):
    max_exact = 16
    lr = np.log(np.maximum(dist, max_exact).astype(np.float32) / np.float32(max_exact)) / np.float32(math.log(2048 / max_exact))
    large = np.minimum(max_exact + (lr.astype(np.float32) * np.float32(32 - max_exact)).astype(np.int32), 31)
    return np.where(dist < max_exact, dist, large)


def _attn_bias(rel_bias):
    k = np.arange(128)[:, None]
    q = np.arange(128)[None, :]
    out = np.empty((24, 128, 256), np.float32)
    for g, dil in enumerate((1, 4, 16)):
        for kb in range(2):
            dist = q - k + 128 if kb == 0 else q - k
            band = (dist >= 0) & (dist <= 128)
            bk = _t5_bucket(np.maximum(dist, 0) * dil)
            for h in range(8):
                hd = g * 8 + h
                out[hd, :, kb * 128:(kb + 1) * 128] = np.where(band, rel_bias[bk, hd], np.float32(-1e30))
    return out


_NC_CACHE = {}


def prepare_inputs(x, w_in, rel_bias, w_dw, b_dw, conv_ln_g, conv_ln_b, w_o_attn, w_o_conv, w_out,
                   ln1_g, ln1_b, w_router, b_router, w_gate_up, b_gate_up, w_down, b_down, ln2_g, ln2_b):
    f = lambda a: np.ascontiguousarray(np.asarray(a, dtype=np.float32))
    x = f(x)

    def pc(v, n):
        return f(np.asarray(v).reshape(n, 128).T)
    shared = {
        "abias": _attn_bias(f(rel_bias)),
        "w_in": f(w_in[0]),
        "w_dw": f(np.asarray(w_dw)[0, :, 0, :].reshape(31, 6, 128).transpose(2, 1, 0)),
        "b_dw": pc(b_dw[0], 6), "conv_ln_g": pc(conv_ln_g[0], 6), "conv_ln_b": pc(conv_ln_b[0], 6),
        "w_o_attn": f(w_o_attn[0]), "w_o_conv": f(w_o_conv[0]), "w_out": f(w_out[0]),
        "ln1_g": f(ln1_g), "ln1_b": f(ln1_b), "w_router": f(w_router[0]), "b_router": f(b_router),
        "w_gate_up": f(w_gate_up[0]),
        "b_gate_up": f(np.asarray(b_gate_up)[0].reshape(NE, 16, 128).transpose(0, 2, 1)),
        "w_down": f(w_down[0]), "b_down": f(b_down[0]), "ln2_g": f(ln2_g), "ln2_b": f(ln2_b),
    }
    in_maps = []
    for c in range(NCORES):
        bi, hf = c // 2, c % 2
        t0 = hf * OWN
        xh = np.zeros((NP, D), np.float32)
        lo = t0 - HALO
        if lo >= 0:
            xh[:] = x[bi, lo:lo + NP]
        else:
            xh[HALO:] = x[bi, 0:OWN]
        m = dict(shared)
        m["xT"] = np.ascontiguousarray(xh.T)
        m["xown"] = np.ascontiguousarray(x[bi, t0:t0 + OWN])
        m["hbias"] = np.full((128, 1), 0.0 if hf == 1 else -1e30, np.float32)
        in_maps.append(m)
    return in_maps


def kernel(**inputs):
    in_maps = prepare_inputs(**inputs)
    if "nc" not in _NC_CACHE:
        _NC_CACHE["nc"] = build()
    res = run_bass_kernel_spmd(_NC_CACHE["nc"], in_maps, core_ids=list(range(NCORES)))
    out = np.empty((4, 8192, D), np.float32)
    for c in range(NCORES):
        out[c // 2, (c % 2) * OWN:(c % 2 + 1) * OWN] = res.results[c]["out"]
    return out
```

```python
import math
from contextlib import ExitStack
import numpy as np
import concourse.bass as bass
import concourse.mybir as mybir
from concourse.bass_utils import run_bass_kernel_spmd

F32 = mybir.dt.float32
BF16 = mybir.dt.bfloat16
I32 = mybir.dt.int32
AF = mybir.ActivationFunctionType
ALU = mybir.AluOpType

NCORES = 8
D = 1024
OWN = 4096
HALO = 2048
NP = OWN + HALO
CAP = 704
CAPT = 768
NE = 32
ALPHA = 2.0 ** 0.25
EPS = 1e-5
ENGS = ("pe", "act", "dve", "pool", "sp")
DBG = {"iters": 99, "units": True, "final": True, "proj": 3, "pv": True, "slevel": 3, "hb": True, "pvacc": True}


class Sched:
    EPOCH = 8000

    def __init__(self, nc, es):
        self.nc = nc
        self.es = es
        self.loc = es
        self.streams = {e: [] for e in ENGS}
        self.cnt = {e: 0 for e in ENGS}
        self.esems = {e: [] for e in ENGS}
        self.res = {}
        self.waited = {e: {} for e in ENGS}
        self.dmasems = {}
        self.dmarr = {}
        for q, n in {"sp": 10, "act": 30, "pool": 10}.items():
            self.dmasems[q] = [[self.sem(f"dma_{q}{i}"), 0] for i in range(n)]
            self.dmarr[q] = 0

    def sem(self, name):
        return self.es.enter_context(self.nc.semaphore(name))

    def sb(self, name, shape, dt):
        return self.loc.enter_context(self.nc.sbuf_tensor(name, list(shape), dt))

    def ps(self, name, shape=(128, 512), dt=F32):
        return self.loc.enter_context(self.nc.psum_tensor(name, list(shape), dt))

    def _collect(self, eng, reads, writes):
        deps = []
        for r in reads:
            st = self.res.get(r)
            if st and st["w"] is not None:
                deps.append(st["w"])
        for w in writes:
            st = self.res.get(w)
            if st:
                if st["w"] is not None:
                    deps.append(st["w"])
                deps.extend(st["r"])
        know = self.waited[eng]
        out = []
        for (sem, val, peng, vc) in deps:
            if peng == "pe" and eng == "pe":
                continue
            if know.get(id(sem), 0) >= val:
                continue
            out.append((sem, val))
            for k, v in vc.items():
                if know.get(k, 0) < v:
                    know[k] = v
        return out

    def _record(self, tok, reads, writes):
        for r in reads:
            st = self.res.setdefault(r, {"w": None, "r": []})
            st["r"].append(tok)
        for w in writes:
            self.res[w] = {"w": tok, "r": []}

    def op(self, eng, fn, reads=(), writes=(), attach=None):
        waits = self._collect(eng, reads, writes)
        n = self.cnt[eng]
        ep = n // self.EPOCH
        while len(self.esems[eng]) <= ep:
            self.esems[eng].append(self.sem(f"c_{eng}{len(self.esems[eng])}"))
        sem = self.esems[eng][ep]
        val = n - ep * self.EPOCH + 1
        self.cnt[eng] = n + 1
        for w in waits:
            self.streams[eng].append(("wait", w[0], w[1]))
        self.streams[eng].append(("op", fn, sem, 1, (eng != "pe") if attach is None else attach))
        vc = dict(self.waited[eng])
        vc[id(sem)] = val
        for pe_ in range(ep):
            vc[id(self.esems[eng][pe_])] = self.EPOCH
        tok = (sem, val, eng, vc)
        self._record(tok, reads, writes)
        return tok

    def dma(self, q, fn, reads=(), writes=()):
        waits = self._collect(q, reads, writes)
        slot = self.dmasems[q][self.dmarr[q] % len(self.dmasems[q])]
        self.dmarr[q] += 1
        sem, total = slot
        if total > 0 and self.waited[q].get(id(sem), 0) < total:
            self.waited[q][id(sem)] = total
            waits.append((sem, total))
        total += 16
        slot[1] = total
        for w in waits:
            self.streams[q].append(("wait", w[0], w[1]))
        self.streams[q].append(("op", fn, sem, 16, False))
        vc = dict(self.waited[q])
        vc[id(sem)] = total
        tok = (sem, total, "dma", vc)
        self._record(tok, reads, writes)
        return tok

    def barrier(self):
        keys = list(self.res.keys())
        for eng in ENGS:
            for w in self._collect(eng, keys, keys):
                self.streams[eng].append(("wait", w[0], w[1]))
        self.res = {}

    def emit(self):
        streams = self.streams
        self.regcache = {}

        def run(e, items):
            pend = []
            for it in items:
                if it[0] == "wait":
                    pend.append(it)
                    continue
                attach = pend.pop() if (it[4] and pend) else None
                for w in pend:
                    e.wait_ge(w[1], w[2])
                pend = []
                ins = it[1](e)
                first, last = ins if isinstance(ins, tuple) else (ins, ins)
                if attach is not None:
                    first._wait_ge(attach[1], attach[2])
                last.then_inc(it[2], it[3])
            for w in pend:
                e.wait_ge(w[1], w[2])

        with self.nc.Block() as block:
            @block.sync
            def _(e):
                run(e, streams["sp"])

            @block.scalar
            def _(e):
                run(e, streams["act"])

            @block.vector
            def _(e):
                run(e, streams["dve"])

            @block.gpsimd
            def _(e):
                run(e, streams["pool"])

            @block.tensor
            def _(e):
                run(e, streams["pe"])
        self.streams = {e: [] for e in ENGS}

    def mm(self, out, pairs, reads, writes):
        def fn(e):
            n = len(pairs)
            ins = first = None
            for i, (l, r) in enumerate(pairs):
                ins = e.matmul(out, lhsT=l, rhs=r, start=(i == 0), stop=(i == n - 1))
                if first is None:
                    first = ins
            return first, ins
        return self.op("pe", fn, reads, writes, attach=True)


def _bc(s, e):
    if "bc" not in s.regcache:
        s.regcache["bc"] = e.to_reg(NE * CAP - 1)
    return s.regcache["bc"]


def _copy(s, eng, out, in_, reads, writes):
    if eng == "act":
        return s.op("act", lambda e: e.activation(out=out, in_=in_, func=AF.Identity), reads, writes)
    return s.op(eng, lambda e: e.tensor_copy(out=out, in_=in_), reads, writes)


def build(stop_after=99, debug=False):
    nc = bass.Bass("TRN2", target_bir_lowering=False)

    def din(name, shape, dt=F32):
        return nc.dram_tensor(name, list(shape), dt, kind="ExternalInput").ap()

    xT_d = din("xT", [D, NP])
    xown_d = din("xown", [OWN, D])
    hb_d = din("hbias", [128, 1])
    ab_d = din("abias", [24, 128, 256])
    win_d = din("w_in", [D, 8192])
    wdw_d = din("w_dw", [128, 6, 31])
    bdw_d = din("b_dw", [128, 6])
    clg_d = din("conv_ln_g", [128, 6])
    clb_d = din("conv_ln_b", [128, 6])
    woa_d = din("w_o_attn", [512, D])
    woc_d = din("w_o_conv", [768, D])
    wout_d = din("w_out", [D, D])
    ln1g_d = din("ln1_g", [1, D])
    ln1b_d = din("ln1_b", [1, D])
    wr_d = din("w_router", [D, NE])
    br_d = din("b_router", [1, NE])
    wgu_d = din("w_gate_up", [NE, D, 2048]) if stop_after >= 6 else None
    bgu_d = din("b_gate_up", [NE, 128, 16])
    wd_d = din("w_down", [NE, D, D]) if stop_after >= 6 else None
    bd_d = din("b_down", [NE, D])
    ln2g_d = din("ln2_g", [1, D])
    ln2b_d = din("ln2_b", [1, D])
    out_d = nc.dram_tensor("out", [OWN, D], F32, kind="ExternalOutput").ap()
    skind = "ExternalOutput" if debug else "Internal"
    attnT_d = nc.dram_tensor("attnT_s", [512, OWN], BF16, kind=skind).ap()
    convT_d = nc.dram_tensor("convT_s", [768, OWN], BF16, kind=skind).ap()
    mrgT_d = nc.dram_tensor("mrgT_s", [D, OWN], BF16, kind=skind).ap()
    h1_d = nc.dram_tensor("h1_s", [OWN, D], F32, kind=skind).ap()
    xs_d = nc.dram_tensor("xs_s", [NE * CAP + 128, D], BF16, kind="Internal").ap()
    y_d = nc.dram_tensor("y_s", [NE * CAP, D], F32, kind="Internal").ap()
    rt_d = nc.dram_tensor("rt_s", [128, 32, 8], F32, kind=skind).ap()

    win_v = win_d.rearrange("(kc p) n -> p kc n", p=128)

    with ExitStack() as es:
        s = Sched(nc, es)
        identb = s.sb("identb", [128, 128], BF16)
        identf = s.sb("identf", [128, 128], F32)
        idx_all = s.sb("idx_all", [128, 128], I32)
        gk_all = s.sb("gk_all", [128, 32, 4], F32)
        G_all = s.sb("G_all", [128, 32, NE], F32)
        for t, nm in ((identb, "identb"), (identf, "identf")):
            s.op("pool", lambda e, t=t: e.memset(t[:], 1.0), writes=[nm])
            s.op("pool", lambda e, t=t: e.affine_select(out=t[:], in_=t[:], pattern=[[-1, 128]],
                                                         compare_op=ALU.is_equal, fill=0.0, base=0,
                                                         channel_multiplier=1), reads=[nm], writes=[nm])

        with ExitStack() as es_x:
            s.loc = es_x
            xTb = s.sb("xTb", [128, 8, NP], BF16)
            for j in (1, 0, 2):
                for kc in range(8):
                    s.dma("pool", lambda e, kc=kc, j=j: e.dma_start(
                        out=xTb[:, kc, j * 2048:(j + 1) * 2048],
                        in_=xT_d[kc * 128:(kc + 1) * 128, j * 2048:(j + 1) * 2048]),
                        writes=[("x", kc, j)])
            XR = [("x", kc, j) for kc in range(8) for j in range(3)]

            if stop_after >= 2:
                with ExitStack() as es_p:
                    s.loc = es_p
                    phase_attn(nc, s, xTb, XR, win_v, ab_d, hb_d, attnT_d)
                    s.barrier()
                    s.emit()
            if stop_after >= 3:
                with ExitStack() as es_p:
                    s.loc = es_p
                    phase_conv(nc, s, xTb, XR, win_v, wdw_d, bdw_d, clg_d, clb_d, convT_d, identb)
                    s.barrier()
                    s.emit()
            if stop_after < 3:
                s.barrier()
                s.emit()
            if stop_after >= 4:
                with ExitStack() as es_p:
                    s.loc = es_p
                    phase_merge(nc, s, xTb, XR, win_v, woa_d, woc_d, attnT_d, convT_d, mrgT_d, xs_d)
                    s.barrier()
                    s.emit()
        if stop_after >= 5:
            with ExitStack() as es_p:
                s.loc = es_p
                phase_out_router(nc, s, mrgT_d, wout_d, xown_d, ln1g_d, ln1b_d, wr_d, br_d, h1_d, xs_d,
                                 identf, idx_all, gk_all, G_all, rt_d)
                s.barrier()
                s.emit()
        if stop_after >= 6:
            with ExitStack() as es_p:
                s.loc = es_p
                phase_experts(nc, s, xs_d, y_d, wgu_d, bgu_d, wd_d, bd_d, identb)
                s.barrier()
                s.emit()
        if stop_after >= 7:
            with ExitStack() as es_p:
                s.loc = es_p
                phase_combine(nc, s, y_d, h1_d, bd_d, ln2g_d, ln2b_d, out_d, identf, idx_all, gk_all, G_all)
                s.barrier()
                s.emit()
    return nc


def phase_attn(nc, s, xTb, XR, win_v, ab_d, hb_d, attnT_d):
    acc = [s.sb(f"acc{h}", [128, 2048], F32) for h in range(2)]
    qT = [[s.sb(f"qT{b}_{h}", [128, 2048], BF16) for h in range(2)] for b in range(2)]
    kT = [s.sb(f"kT{b}", [128, 4096], BF16) for b in range(2)]
    vB = [s.sb(f"vB{b}", [128, 32, 2, 128], BF16) for b in range(2)]
    wq = s.sb("wq", [128, 8, 128], BF16)
    wk = s.sb("wk", [128, 8, 128], BF16)
    wv = s.sb("wv", [128, 8, 128], BF16)
    ab = s.sb("ab", [128, 3, 2, 256], F32)
    abh = s.sb("abh", [128, 3, 2, 128], F32)
    tmp = [s.sb(f"tmp{i}", [128, 512], F32) for i in range(3)]
    pt = [s.sb(f"pt{i}", [128, 512], BF16) for i in range(3)]
    rec = tmp[0][0:64, :]
    hb = s.sb("hb", [128, 1], F32)
    psA = [s.ps(f"psA{i}") for i in range(2)]
    psV = s.ps("psV")
    psS = [s.ps(f"psS{i}") for i in range(3)]
    psO = [s.ps("psO0"), s.ps("psO1")]

    s.dma("sp", lambda e: e.dma_start(out=hb[:], in_=hb_d), writes=["hb"])
    for b in range(2):
        s.op("pool", lambda e, b=b: e.memset(vB[b][:], 1.0), writes=[("v", b, i) for i in range(8)])
        for h in range(2):
            s.op("pool", lambda e, b=b, h=h: e.memset(qT[b][h][:], 0.0), writes=[("q", b, h, tc) for tc in range(4)])

    def xr(lo, hi):
        return [("x", kc, j) for kc in range(8) for j in range(lo // 2048, (hi - 1) // 2048 + 1)]

    cnt = {"pa": 0, "ev": 0, "si": 0}
    iters = [(hp, half, g) for hp in range(4) for half in range(2) for g in range(3)][:DBG["iters"]]

    def make_proj(idx):
        hp, half, g = iters[idx]
        dil = (1, 4, 16)[g]
        halo = 128 * dil
        p0 = HALO + half * 2048
        b = idx % 2
        nK = halo + 2048
        kb0 = p0 - halo
        bpr = 16 // dil + 1
        nblk = dil * bpr
        steps = []

        def loads():
            for (wt, base, nm) in ((wq, 0, "wq"), (wk, 1536, "wk"), (wv, 3072, "wv")):
                c0 = base + g * 512 + hp * 128
                s.dma("pool", lambda e, wt=wt, c0=c0: e.dma_start(out=wt[:], in_=win_v[:, :, c0:c0 + 128]), writes=[nm])
        steps.append(loads)

        def qstep(tc):
            def f():
                pi = cnt["pa"] % 2
                cnt["pa"] += 1
                ps, pkey = psA[pi], ("psA", pi)
                s.mm(ps[:, 0:512], [(wq[:, kc, :], xTb[:, kc, p0 + tc * 512:p0 + (tc + 1) * 512]) for kc in range(8)],
                     reads=xr(p0 + tc * 512, p0 + (tc + 1) * 512) + ["wq"], writes=[pkey])
                _copy(s, "act", qT[b][0][0:64, tc * 512:(tc + 1) * 512], ps[0:64, 0:512], reads=[pkey], writes=[("q", b, 0, tc)])
                _copy(s, "act", qT[b][1][64:128, tc * 512:(tc + 1) * 512], ps[64:128, 0:512], reads=[pkey],
                      writes=[("q", b, 1, tc)])
            return f
        for tc in range(4):
            steps.append(qstep(tc))

        def kstep(off, n, ci):
            def f():
                pi = cnt["pa"] % 2
                cnt["pa"] += 1
                ps, pkey = psA[pi], ("psA", pi)
                s.mm(ps[:, 0:n], [(wk[:, kc, :], xTb[:, kc, kb0 + off:kb0 + off + n]) for kc in range(8)],
                     reads=xr(kb0 + off, kb0 + off + n) + ["wk"], writes=[pkey])
                _copy(s, ("act", "act", "dve")[cnt["ev"] % 3], kT[b][:, off:off + n], ps[:, 0:n], reads=[pkey], writes=[("k", b, ci)])
                cnt["ev"] += 1
            return f
        off = 0
        ci = 0
        while off < nK:
            n = min(512, nK - off)
            steps.append(kstep(off, n, ci))
            off += n
            ci += 1

        def vstep(blk0):
            def f():
                nb = min(4, nblk - blk0)
                for j in range(nb):
                    blk = blk0 + j
                    r, mi = blk // bpr, blk % bpr
                    st = p0 + r + dil * 128 * (mi - 1)
                    s.mm(psV[:, j * 128:(j + 1) * 128],
                         [(xTb[:, kc, st:st + 127 * dil + 1:dil], wv[:, kc, :]) for kc in range(8)],
                         reads=xr(st, st + 127 * dil + 1) + ["wv"], writes=["psV"])
                _copy(s, ("act", "act", "dve")[cnt["ev"] % 3], vB[b][:, blk0:blk0 + nb, :, 0:64],
                      psV[:, 0:nb * 128].rearrange("p (a b c) -> p a b c", a=nb, b=2),
                      reads=["psV"], writes=[("v", b, blk0 // 4)])
                cnt["ev"] += 1
            return f
        for blk0 in range(0, nblk, 4):
            steps.append(vstep(blk0))
        return steps

    def emit_S2(pair, i, b, g, dil, halo, nK, half):
        hh = pair[0][0]
        pS, tm, pT = psS[i], tmp[i], pt[i]
        mms = []
        rd = []
        flags = []
        for ui, (_, r, m) in enumerate(pair):
            q0 = r + dil * 128 * m
            qap = qT[b][hh][:, q0:q0 + 127 * dil + 1:dil]
            kp = halo + r + dil * 128 * (m - 1)
            kc_ = halo + r + dil * 128 * m
            kprev = kT[b][:, kp:kp + 127 * dil + 1:dil]
            kcur = kT[b][:, kc_:kc_ + 127 * dil + 1:dil]
            rd += [("q", b, hh, c) for c in range(q0 // 512, (q0 + 128 * dil - 1) // 512 + 1)]
            rd += [("k", b, c) for c in range(kp // 512, min((kc_ + 128 * dil - 1) // 512, (nK - 1) // 512) + 1)]
            mms.append((pS[:, ui * 256:ui * 256 + 128], kprev, qap))
            mms.append((pS[:, ui * 256 + 128:ui * 256 + 256], kcur, qap))
            flags.append(half == 0 and m == 0)

        def fn(e):
            ins = first = None
            for (o_, l_, r_) in mms:
                ins = e.matmul(o_, lhsT=l_, rhs=r_, start=True, stop=True)
                first = first or ins
            return first, ins
        s.op("pe", fn, reads=list(dict.fromkeys(rd)), writes=[("psS", i)], attach=True)
        tkeys = [("tmp", i, 0), ("tmp", i, 1)]
        if not any(flags):
            in1 = ab[:, g, hh:hh + 1, :].broadcast_to([128, 2, 256])
            s.op("dve", lambda e: e.scalar_tensor_tensor(out=tm[:].rearrange("p (a b) -> p a b", a=2),
                                                         in0=pS[:, 0:512].rearrange("p (a b) -> p a b", a=2), scalar=0.125,
                                                         in1=in1, op0=ALU.mult, op1=ALU.add),
                 reads=[("psS", i), ("ab", g, hh)], writes=tkeys)
        else:
            for ui in range(2):
                c0 = ui * 256
                if flags[ui]:
                    s.op("dve", lambda e, c0=c0: e.scalar_tensor_tensor(out=tm[:, c0:c0 + 128], in0=pS[:, c0:c0 + 128], scalar=0.125,
                                                                        in1=abh[:, g, hh, :], op0=ALU.mult, op1=ALU.add),
                         reads=[("psS", i), ("abh", g, hh)], writes=[("tmp", i, ui)])
                    s.op("dve", lambda e, c0=c0: e.scalar_tensor_tensor(out=tm[:, c0 + 128:c0 + 256], in0=pS[:, c0 + 128:c0 + 256],
                                                                        scalar=0.125, in1=ab[:, g, hh, 128:256], op0=ALU.mult,
                                                                        op1=ALU.add),
                         reads=[("psS", i), ("ab", g, hh), ("tmp", i, ui)], writes=[("tmp", i, ui)])
                else:
                    s.op("dve", lambda e, c0=c0: e.scalar_tensor_tensor(out=tm[:, c0:c0 + 256], in0=pS[:, c0:c0 + 256], scalar=0.125,
                                                                        in1=ab[:, g, hh, :], op0=ALU.mult, op1=ALU.add),
                         reads=[("psS", i), ("ab", g, hh)], writes=[("tmp", i, ui)])
        s.op("act", lambda e: e.activation(out=pT[:], in_=tm[:], func=AF.Exp), reads=tkeys, writes=[("pt", i)])

    def emit_PV2(pair, i, o, b, g, dil, bpr):
        hh = pair[0][0]
        pT = pt[i]
        mms = []
        rd = [("pt", i)]
        for ui, (_, r, m) in enumerate(pair):
            bp = r * bpr + m
            po = psO[o][:, ui * 128:(ui + 1) * 128]
            mms.append((po, vB[b][:, bp, hh, :], pT[:, ui * 256:ui * 256 + 128], True, False))
            mms.append((po, vB[b][:, bp + 1, hh, :], pT[:, ui * 256 + 128:ui * 256 + 256], False, True))
            rd += [("v", b, bp // 4), ("v", b, (bp + 1) // 4)]

        def fn(e):
            ins = first = None
            for (o_, l_, r_, st_, sp_) in mms:
                ins = e.matmul(o_, lhsT=l_, rhs=r_, start=st_, stop=sp_)
                first = first or ins
            return first, ins
        s.op("pe", fn, reads=list(dict.fromkeys(rd)), writes=[("psO", o)], attach=True)
        av = acc[hh][:].rearrange("p (a j d) -> p a d j", a=16 // dil, j=128, d=dil)
        (_, r0_, m0_), (_, r1_, m1_) = pair
        if m1_ != m0_:
            aap = av[:, m0_:m0_ + 2, r0_, :]
        else:
            aap = av[:, m0_, r0_:r0_ + 2, :]
        pin = psO[o][:, 0:256].rearrange("p (a b) -> p a b", a=2)
        if g == 0:
            s.op("dve", lambda e: e.tensor_copy(out=aap, in_=pin), reads=[("psO", o)], writes=[("acc", hh)])
        else:
            s.op("dve", lambda e: e.tensor_tensor(out=aap, in0=pin, in1=aap, op=ALU.add),
                 reads=[("psO", o), ("acc", hh)], writes=[("acc", hh)])

    def load_ab(hp):
        for g2 in range(3):
            for hh in range(2):
                hd = g2 * 8 + hp * 2 + hh
                s.dma("sp", lambda e, g2=g2, hh=hh, hd=hd: e.dma_start(out=ab[:, g2, hh, :], in_=ab_d[hd]),
                      writes=[("ab", g2, hh)])
                s.op("pool", lambda e, g2=g2, hh=hh: e.tensor_scalar(out=abh[:, g2, hh, :], in0=ab[:, g2, hh, 0:128],
                                                                    scalar1=hb[:, 0:1], scalar2=None, op0=ALU.add),
                     reads=[("ab", g2, hh), "hb"], writes=[("abh", g2, hh)])

    load_ab(0)
    for f in make_proj(0):
        f()
    for idx, (hp, half, g) in enumerate(iters):
        dil = (1, 4, 16)[g]
        halo = 128 * dil
        b = idx % 2
        nK = halo + 2048
        bpr = 16 // dil + 1
        nxt = make_proj(idx + 1) if idx + 1 < len(iters) else []
        units = [(hh, r, m) for hh in range(2) for r in range(dil) for m in range(16 // dil)]
        pairs = [(units[k], units[k + 1]) for k in range(0, len(units), 2)]
        pend = []
        for pi2, pr in enumerate(pairs):
            i = cnt["si"] % 3
            cnt["si"] += 1
            emit_S2(pr, i, b, g, dil, halo, nK, half)
            pend.append((pr, i, pi2 % 2))
            if len(pend) > 2:
                pp, pi_, po_ = pend.pop(0)
                emit_PV2(pp, pi_, po_, b, g, dil, bpr)
            if nxt and pi2 == 0:
                nxt.pop(0)()
            if pi2 >= 5:
                for _ in range(2):
                    if nxt:
                        nxt.pop(0)()
        while pend:
            pp, pi_, po_ = pend.pop(0)
            emit_PV2(pp, pi_, po_, b, g, dil, bpr)
        if idx + 1 < len(iters) and iters[idx + 1][0] != hp:
            load_ab(iters[idx + 1][0])
        while nxt:
            nxt.pop(0)()
        if g == 2:
            for hh in range(2):
                for c in range(4):
                    cs = slice(c * 512, (c + 1) * 512)
                    s.op("dve", lambda e, hh=hh, cs=cs: e.tensor_copy(out=rec, in_=acc[hh][64:128, cs]),
                         reads=[("acc", hh)], writes=[("tmp", 0, 0), ("tmp", 0, 1)])
                    s.op("act", lambda e: e.activation(out=rec, in_=rec, func=AF.Ln),
                         reads=[("tmp", 0, 0), ("tmp", 0, 1)], writes=[("tmp", 0, 0), ("tmp", 0, 1)])
                    s.op("act", lambda e: e.activation(out=rec, in_=rec, func=AF.Exp, scale=-1.0),
                         reads=[("tmp", 0, 0), ("tmp", 0, 1)], writes=[("tmp", 0, 0), ("tmp", 0, 1)])
                    s.op("dve", lambda e, hh=hh, cs=cs: e.tensor_tensor(out=acc[hh][0:64, cs], in0=acc[hh][0:64, cs], in1=rec,
                                                                        op=ALU.mult),
                         reads=[("tmp", 0, 0), ("tmp", 0, 1), ("acc", hh)], writes=[("acc", hh)])
                row = (hp * 2 + hh) * 64
                s.dma("pool", lambda e, hh=hh, row=row, half=half: e.dma_start(
                    out=attnT_d[row:row + 64, half * 2048:(half + 1) * 2048], in_=acc[hh][0:64, :]),
                    reads=[("acc", hh)], writes=[("attnT_d", row, half)])


def phase_conv(nc, s, xTb, XR, win_v, wdw_d, bdw_d, clg_d, clb_d, convT_d, identb):
    dw = s.sb("dw", [128, 6, 2048], F32)
    glu = [s.sb(f"glu{i}", [128, 32 + 2048], BF16) for i in range(2)]
    dg = [s.sb(f"dg{i}", [128, 31, 128], BF16) for i in range(2)]
    wu = [s.sb(f"wu{i}", [128, 8, 128], BF16) for i in range(2)]
    wg = [s.sb(f"wg{i}", [128, 8, 128], BF16) for i in range(2)]
    sgt = [s.sb(f"sgt{i}", [128, 512], F32) for i in range(2)]
    sq = [s.sb(f"sq{i}", [128, 512], F32) for i in range(2)]
    sd = s.sb("sd", [128, 512], F32)
    rstd = s.sb("rstd", [128, 512], F32)
    cst = [s.sb(f"cst{i}", [128, 6, 512], BF16) for i in range(2)]
    wdw = s.sb("wdw", [128, 6, 31], F32)
    bdw = s.sb("bdw", [128, 6], F32)
    clg = s.sb("clg", [128, 6], F32)
    clb = s.sb("clb", [128, 6], F32)
    onesf = s.sb("onesf", [128, 128], F32)
    psU = [s.ps(f"psU{i}") for i in range(2)]
    psG = [s.ps(f"psG{i}") for i in range(2)]
    psM = s.ps("psM")
    psV2 = s.ps("psV2")
    psC = [s.ps("psC0"), s.ps("psC1")]
    for t, d_, nm in ((wdw, wdw_d, "wdw"), (bdw, bdw_d, "bdw"), (clg, clg_d, "clg"), (clb, clb_d, "clb")):
        s.dma("sp", lambda e, t=t, d_=d_: e.dma_start(out=t[:], in_=d_), writes=[nm])
    s.op("pool", lambda e: e.memset(onesf[:], 1.0 / 768.0), writes=["onesf"])
    convT_v = convT_d.rearrange("(cc p) t -> p cc t", p=128)
    it = 0
    pu = 0
    ci = 0
    for half in range(2):
        p0 = HALO + half * 2048
        for cc in range(6):
            b = it % 2
            it += 1
            cv = 4608 + cc * 128
            cg = 4608 + 768 + cc * 128
            s.dma("pool", lambda e, b=b, cv=cv: e.dma_start(out=wu[b][:], in_=win_v[:, :, cv:cv + 128]), writes=[("wu", b)])
            s.dma("pool", lambda e, b=b, cg=cg: e.dma_start(out=wg[b][:], in_=win_v[:, :, cg:cg + 128]), writes=[("wg", b)])
            for (off, n) in [(0, 32)] + [(32 + i * 512, 512) for i in range(4)]:
                pi = pu % 2
                pu += 1
                t0 = p0 - 32 + off
                s.mm(psU[pi][:, 0:n], [(wu[b][:, kc, :], xTb[:, kc, t0:t0 + n]) for kc in range(8)],
                     reads=XR + [("wu", b)], writes=[("psU", pi)])
                s.mm(psG[pi][:, 0:n], [(wg[b][:, kc, :], xTb[:, kc, t0:t0 + n]) for kc in range(8)],
                     reads=XR + [("wg", b)], writes=[("psG", pi)])
                s.op("act", lambda e, pi=pi, n=n: e.activation(out=sgt[pi][:, 0:n], in_=psG[pi][:, 0:n], func=AF.Sigmoid),
                     reads=[("psG", pi)], writes=[("sgt", pi)])
                s.op("dve", lambda e, pi=pi, n=n, off=off, b=b: e.tensor_tensor(out=glu[b][:, off:off + n], in0=psU[pi][:, 0:n],
                                                                                in1=sgt[pi][:, 0:n], op=ALU.mult),
                     reads=[("psU", pi), ("sgt", pi)], writes=[("glu", b)])
            for j in range(31):
                s.op("dve", lambda e, b=b, cc=cc, j=j: e.tensor_scalar(out=dg[b][:, j, :], in0=identb[:], scalar1=wdw[:, cc, j:j + 1],
                                                                       scalar2=None, op0=ALU.mult),
                     reads=["identb", "wdw"], writes=[("dg", b)])
            for tc in range(4):
                pc = (it * 4 + tc) % 2
                s.mm(psC[pc][:, :], [(dg[b][:, j, :], glu[b][:, 2 + j + tc * 512:2 + j + (tc + 1) * 512]) for j in range(31)],
                     reads=[("dg", b), ("glu", b)], writes=[("psC", pc)])
                s.op("dve", lambda e, cc=cc, tc=tc, pc=pc: e.tensor_scalar(out=dw[:, cc, tc * 512:(tc + 1) * 512], in0=psC[pc][:, :],
                                                                          scalar1=bdw[:, cc:cc + 1], scalar2=None, op0=ALU.add),
                     reads=[("psC", pc), "bdw"], writes=[("dw", cc)])
        for tc in range(4):
            ts_ = slice(tc * 512, (tc + 1) * 512)
            s.mm(psM[:, :], [(onesf[:], dw[:, cc, ts_]) for cc in range(6)], reads=[("dw", cc) for cc in range(6)] + ["onesf"],
                 writes=["psM"])
            for cc in range(6):
                s.op("dve", lambda e, cc=cc, ts_=ts_: e.tensor_tensor(out=dw[:, cc, ts_], in0=dw[:, cc, ts_], in1=psM[:, :],
                                                                      op=ALU.subtract),
                     reads=["psM", ("dw", cc)], writes=[("dw", cc)])
            sqt = []
            for cc in range(6):
                qi = cc % 2
                s.op("act", lambda e, cc=cc, qi=qi, ts_=ts_: e.activation(out=sq[qi][:], in_=dw[:, cc, ts_], func=AF.Square),
                     reads=[("dw", cc)], writes=[("sq", qi)])
                def fn(e, cc=cc, qi=qi):
                    return e.matmul(psV2[:, :], lhsT=onesf[:], rhs=sq[qi][:], start=(cc == 0), stop=(cc == 5))
                s.op("pe", fn, reads=[("sq", qi), "onesf"], writes=["psV2"], attach=True)
            s.op("act", lambda e: e.activation(out=sd[:], in_=psV2[:, :], func=AF.Sqrt, bias=EPS), reads=["psV2"], writes=["sd"])
            s.op("dve", lambda e: e.reciprocal(out=rstd[:], in_=sd[:]), reads=["sd"], writes=["rstd"])
            cb = ci % 2
            ci += 1
            for cc in range(6):
                s.op("dve", lambda e, cc=cc, ts_=ts_: e.tensor_tensor(out=dw[:, cc, ts_], in0=dw[:, cc, ts_], in1=rstd[:],
                                                                      op=ALU.mult),
                     reads=["rstd", ("dw", cc)], writes=[("dw", cc)])
                s.op("dve", lambda e, cc=cc, ts_=ts_: e.tensor_scalar(out=dw[:, cc, ts_], in0=dw[:, cc, ts_], scalar1=clg[:, cc:cc + 1],
                                                                      scalar2=clb[:, cc:cc + 1], op0=ALU.mult, op1=ALU.add),
                     reads=[("dw", cc), "clg", "clb"], writes=[("dw", cc)])
                s.op("act", lambda e, cc=cc, ts_=ts_, cb=cb: e.activation(out=cst[cb][:, cc, :], in_=dw[:, cc, ts_], func=AF.Silu),
                     reads=[("dw", cc)], writes=[("cst", cb)])
            t0 = half * 2048 + tc * 512
            s.dma("sp", lambda e, cb=cb, t0=t0: e.dma_start(out=convT_v[:, :, t0:t0 + 512], in_=cst[cb][:]),
                  reads=[("cst", cb)], writes=[("convT_d", t0)])


def phase_merge(nc, s, xTb, XR, win_v, woa_d, woc_d, attnT_d, convT_d, mrgT_d, xs_d):
    woa = s.sb("woa", [128, 4, D], BF16)
    woc = s.sb("woc", [128, 6, D], BF16)
    wga = s.sb("wga", [128, 8, D], BF16)
    wgc = s.sb("wgc", [128, 8, D], BF16)
    at = [s.sb(f"at{i}", [128, 4, 512], BF16) for i in range(2)]
    cv = [s.sb(f"cv{i}", [128, 6, 512], BF16) for i in range(2)]
    mg = [s.sb(f"mg{i}", [128, 8, 512], BF16) for i in range(2)]
    sga = [s.sb(f"sga{i}", [128, 512], F32) for i in range(2)]
    sgc = [s.sb(f"sgc{i}", [128, 512], F32) for i in range(2)]
    psa = [s.ps(f"psa{i}") for i in range(2)]
    psc = [s.ps(f"psc{i}") for i in range(2)]
    psga = [s.ps(f"psga{i}") for i in range(2)]
    psgc = [s.ps(f"psgc{i}") for i in range(2)]
    for kc in range(8):
        s.dma("pool", lambda e, kc=kc: e.dma_start(out=wga[:, kc, :], in_=win_v[:, kc, 6144:7168]), writes=[("wga", kc)])
    for kc in range(8):
        s.dma("pool", lambda e, kc=kc: e.dma_start(out=wgc[:, kc, :], in_=win_v[:, kc, 7168:8192]), writes=[("wgc", kc)])
    s.dma("pool", lambda e: e.dma_start(out=woa[:], in_=woa_d.rearrange("(h p) n -> p h n", p=128)), writes=["woa"])
    s.dma("pool", lambda e: e.dma_start(out=woc[:], in_=woc_d.rearrange("(c p) n -> p c n", p=128)), writes=["woc"])
    zt = s.sb("zt", [128, D], BF16)
    s.op("dve", lambda e: e.memset(zt[:], 0.0), writes=["zt"])
    xs_z = xs_d.rearrange("(p j) d -> p j d", p=128)
    nrow = (NE * CAP + 128) // 128
    zq = list(range(nrow))

    def zero_some(n):
        for _ in range(n):
            if zq:
                c = zq.pop(0)
                s.dma("act", lambda e, c=c: e.dma_start(out=xs_z[:, c, :], in_=zt[:]), reads=["zt"], writes=[("xs_zero", c)])
    WGA = [("wga", kc) for kc in range(8)]
    WGC = [("wgc", kc) for kc in range(8)]
    attn_v = attnT_d.rearrange("(h p) t -> p h t", p=128)
    conv_v = convT_d.rearrange("(c p) t -> p c t", p=128)
    mrg_v = mrgT_d.rearrange("(c p) t -> p c t", p=128)
    pi = 0
    for tc in range(8):
        b = tc % 2
        ts_ = slice(tc * 512, (tc + 1) * 512)
        xs_ = slice(HALO + tc * 512, HALO + (tc + 1) * 512)
        s.dma("sp", lambda e, b=b, ts_=ts_: e.dma_start(out=at[b][:], in_=attn_v[:, :, ts_]), writes=[("at", b)])
        s.dma("sp", lambda e, b=b, ts_=ts_: e.dma_start(out=cv[b][:], in_=conv_v[:, :, ts_]), writes=[("cv", b)])
        for fc in range(8):
            fs = slice(fc * 128, (fc + 1) * 128)
            p = pi % 2
            pi += 1
            s.mm(psga[p][:, :], [(wga[:, kc, fs], xTb[:, kc, xs_]) for kc in range(8)], reads=XR + WGA, writes=[("psga", p)])
            s.mm(psgc[p][:, :], [(wgc[:, kc, fs], xTb[:, kc, xs_]) for kc in range(8)], reads=XR + WGC, writes=[("psgc", p)])
            s.mm(psa[p][:, :], [(woa[:, h, fs], at[b][:, h, :]) for h in range(4)], reads=["woa", ("at", b)],
                 writes=[("psa", p)])
            s.mm(psc[p][:, :], [(woc[:, c, fs], cv[b][:, c, :]) for c in range(6)], reads=["woc", ("cv", b)],
                 writes=[("psc", p)])
            s.op("act", lambda e, p=p: e.activation(out=sga[p][:], in_=psga[p][:, :], func=AF.Sigmoid),
                 reads=[("psga", p)], writes=[("sga", p)])
            s.op("act", lambda e, p=p: e.activation(out=sgc[p][:], in_=psgc[p][:, :], func=AF.Sigmoid),
                 reads=[("psgc", p)], writes=[("sgc", p)])
            s.op("dve", lambda e, p=p: e.tensor_tensor(out=sga[p][:], in0=psa[p][:, :], in1=sga[p][:], op=ALU.mult),
                 reads=[("psa", p), ("sga", p)], writes=[("sga", p)])
            s.op("dve", lambda e, p=p: e.tensor_tensor(out=sgc[p][:], in0=psc[p][:, :], in1=sgc[p][:], op=ALU.mult),
                 reads=[("psc", p), ("sgc", p)], writes=[("sgc", p)])
            s.op("dve", lambda e, p=p, fc=fc, b=b: e.tensor_tensor(out=mg[b][:, fc, :], in0=sga[p][:], in1=sgc[p][:], op=ALU.add),
                 reads=[("sga", p), ("sgc", p)], writes=[("mg", b)])
            zero_some(3)
        s.dma("sp", lambda e, b=b, ts_=ts_: e.dma_start(out=mrg_v[:, :, ts_], in_=mg[b][:]), reads=[("mg", b)],
              writes=[("mrg_d", tc)])
    zero_some(len(zq))


def phase_out_router(nc, s, mrgT_d, wout_d, xown_d, ln1g_d, ln1b_d, wr_d, br_d, h1_d, xs_d,
                     identf, idx_all, gk_all, G_all, rt_d):
    wout = s.sb("wout", [128, 8, D], BF16)
    lng = s.sb("lng", [128, D], F32)
    lnb = s.sb("lnb", [128, D], F32)
    wr = s.sb("wr", [128, 8, NE], F32)
    brb = s.sb("brb", [128, NE], F32)
    mg = [s.sb(f"mgc{i}", [128, 8, 512], BF16) for i in range(2)]
    xo = [s.sb(f"xo{i}", [128, D], F32) for i in range(2)]
    z = [s.sb(f"z{i}", [128, D], F32) for i in range(2)]
    h1 = [s.sb(f"h1{i}", [128, D], F32) for i in range(2)]
    h1b = [s.sb(f"h1b{i}", [128, D], BF16) for i in range(2)]
    h1T = [s.sb(f"h1T{i}", [128, 8, 128], F32) for i in range(2)]

    def two(name, shape, dt=F32):
        return [s.sb(f"{name}{i}", shape, dt) for i in range(2)]
    st6 = two("st6", [128, 2, 6])
    mv = two("mv", [128, 2])
    rs = two("rs", [128, 1])
    lg = two("lg", [128, NE])
    m8 = two("m8", [128, 8])
    selm = two("selm", [128, NE])
    selb = two("selb", [128, NE], BF16)
    ex = two("ex", [128, NE])
    den = two("den", [128, 1])
    rden = two("rden", [128, 1])
    pos = two("pos", [128, NE])
    key = two("key", [128, NE])
    k8 = two("k8", [128, 8])
    junk = two("junk", [128, NE])
    run = s.sb("run", [128, NE], F32)
    ustr = s.sb("ustr", [128, 128], BF16)
    onesb = s.sb("onesb", [128, 128], BF16)
    rtst = s.sb("rtst", [128, 32, 8], F32)
    pso = [[s.ps(f"pso{i}_{h}") for h in range(2)] for i in range(2)]
    pst = [s.ps(f"pst{i}") for i in range(2)]
    pslp = [s.ps(f"pslp{i}") for i in range(2)]

    for kc in range(8):
        s.dma("pool", lambda e, kc=kc: e.dma_start(out=wout[:, kc, :], in_=wout_d[kc * 128:(kc + 1) * 128, :]),
              writes=[("wout", kc)])
    WO = [("wout", kc) for kc in range(8)]
    s.dma("sp", lambda e: e.dma_start(out=lng[:], in_=ln1g_d.broadcast_to([128, D])), writes=["lng"])
    s.dma("sp", lambda e: e.dma_start(out=lnb[:], in_=ln1b_d.broadcast_to([128, D])), writes=["lnb"])
    s.dma("sp", lambda e: e.dma_start(out=wr[:], in_=wr_d.rearrange("(kc p) n -> p kc n", p=128)), writes=["wr"])
    s.dma("sp", lambda e: e.dma_start(out=brb[:], in_=br_d.broadcast_to([128, NE])), writes=["brb"])
    s.op("pool", lambda e: e.memset(ustr[:], 1.0), writes=["ustr"])
    s.op("pool", lambda e: e.affine_select(out=ustr[:], in_=ustr[:], pattern=[[1, 128]], compare_op=ALU.is_gt, fill=0.0,
                                           base=0, channel_multiplier=-1), reads=["ustr"], writes=["ustr"])
    s.op("pool", lambda e: e.memset(onesb[:], 1.0), writes=["onesb"])
    s.op("pool", lambda e: e.iota(run[:], pattern=[[CAP, NE]], base=1, channel_multiplier=0,
                                  allow_small_or_imprecise_dtypes=True), writes=["run"])
    mrg_v = mrgT_d.rearrange("(c p) t -> p c t", p=128)

    def stage_a(ti):
        tc, tt = ti // 4, ti % 4
        b = tc % 2
        tb = ti % 2
        r0 = ti * 128
        if tt == 0:
            ts_ = slice(tc * 512, (tc + 1) * 512)
            s.dma("sp", lambda e: e.dma_start(out=mg[b][:], in_=mrg_v[:, :, ts_]), writes=[("mgc", b)])
        s.dma("sp", lambda e: e.dma_start(out=xo[tb][:], in_=xown_d[r0:r0 + 128, :]), writes=[("xo", tb)])
        for hf in range(2):
            hs = slice(hf * 512, (hf + 1) * 512)
            s.mm(pso[tb][hf][:, :], [(mg[b][:, kc, tt * 128:(tt + 1) * 128], wout[:, kc, hs]) for kc in range(8)],
                 reads=[("mgc", b)] + WO, writes=[("pso", tb, hf)])
            s.op("dve", lambda e, hf=hf, hs=hs: e.scalar_tensor_tensor(out=z[tb][:, hs], in0=xo[tb][:, hs], scalar=ALPHA,
                                                                       in1=pso[tb][hf][:, :], op0=ALU.mult, op1=ALU.add),
                 reads=[("xo", tb), ("pso", tb, hf)], writes=[("z", tb, hf)])
            s.op("dve", lambda e, hf=hf, hs=hs: e.bn_stats(out=st6[tb][:, hf, :], in_=z[tb][:, hs]),
                 reads=[("z", tb, hf)], writes=[("st6", tb, hf)])
        _ln_tail(s, z[tb], [("z", tb, 0), ("z", tb, 1)], st6[tb], [("st6", tb, 0), ("st6", tb, 1)], mv[tb], rs[tb],
                 lng, lnb, h1[tb], ("h1", tb), f"r{tb}")
        s.dma("sp", lambda e: e.dma_start(out=h1_d[r0:r0 + 128, :], in_=h1[tb][:]), reads=[("h1", tb)], writes=[("h1_d", ti)])
        s.op("act", lambda e: e.activation(out=h1b[tb][:], in_=h1[tb][:], func=AF.Identity), reads=[("h1", tb)],
             writes=[("h1b", tb)])
        for hf in range(2):
            def fn(e, hf=hf):
                ins = None
                for k in range(4):
                    kc = hf * 4 + k
                    ins = e.transpose(out=pst[hf][:, k * 128:(k + 1) * 128], in_=h1[tb][:, kc * 128:(kc + 1) * 128],
                                      identity=identf[:])
                return ins
            s.op("pe", fn, reads=[("h1", tb), "identf"], writes=[("pst", hf)])
            _copy(s, "act", h1T[tb][:, hf * 4:(hf + 1) * 4, :], pst[hf][:, :].rearrange("p (a b) -> p a b", a=4),
                  reads=[("pst", hf)], writes=[("h1T", tb, hf)])

    def stage_b(ti):
        tb = ti % 2
        P = pslp[tb]
        pk = ("pslp", tb)
        lg_, m8_, sel_, selb_, ex_, den_, rden_, pos_, key_, k8_, junk_ = (lg[tb], m8[tb], selm[tb], selb[tb], ex[tb], den[tb],
                                                                          rden[tb], pos[tb], key[tb], k8[tb], junk[tb])
        T = lambda n: (n, tb)
        s.mm(P[:, 0:NE], [(h1T[tb][:, kc, :], wr[:, kc, :]) for kc in range(8)], reads=[("h1T", tb, 0), ("h1T", tb, 1), "wr"],
             writes=[pk])
        s.op("dve", lambda e: e.tensor_tensor(out=lg_[:], in0=P[:, 0:NE], in1=brb[:], op=ALU.add), reads=[pk, "brb"],
             writes=[T("lg")])
        s.op("dve", lambda e: e.max(out=m8_[:], in_=lg_[:]), reads=[T("lg")], writes=[T("m8")])
        s.op("dve", lambda e: e.tensor_scalar(out=sel_[:], in0=lg_[:], scalar1=m8_[:, 3:4], scalar2=None, op0=ALU.is_ge),
             reads=[T("lg"), T("m8")], writes=[T("selm")])
        s.op("dve", lambda e: e.tensor_scalar(out=ex_[:], in0=lg_[:], scalar1=m8_[:, 0:1], scalar2=None, op0=ALU.subtract),
             reads=[T("lg"), T("m8")], writes=[T("ex")])
        s.op("act", lambda e: e.activation(out=ex_[:], in_=ex_[:], func=AF.Exp), reads=[T("ex")], writes=[T("ex")])
        s.op("dve", lambda e: e.scalar_tensor_tensor(out=ex_[:], in0=ex_[:], scalar=1.0, in1=sel_[:], op0=ALU.mult,
                                                     op1=ALU.mult, accum_out=den_[:]),
             reads=[T("ex"), T("selm")], writes=[T("ex"), T("den")])
        s.op("dve", lambda e: e.reciprocal(out=rden_[:], in_=den_[:]), reads=[T("den")], writes=[T("rden")])
        s.op("dve", lambda e: e.tensor_scalar(out=G_all[:, ti, :], in0=ex_[:], scalar1=rden_[:, 0:1], scalar2=None,
                                              op0=ALU.mult),
             reads=[T("ex"), T("rden")], writes=[("G", ti)])
        s.op("act", lambda e: e.activation(out=selb_[:], in_=sel_[:], func=AF.Identity), reads=[T("selm")], writes=[T("selb")])

        def fnp(e):
            e.matmul(P[:, 64:64 + NE], lhsT=ustr[:], rhs=selb_[:], start=True, stop=True)
            return e.matmul(P[:, 128:128 + NE], lhsT=onesb[:], rhs=selb_[:], start=True, stop=True)
        s.op("pe", fnp, reads=["ustr", "onesb", T("selb"), T("lg")], writes=[pk])
        s.op("dve", lambda e: e.tensor_tensor(out=pos_[:], in0=P[:, 64:64 + NE], in1=run[:], op=ALU.add),
             reads=[pk, "run"], writes=[T("pos")])
        s.op("dve", lambda e: e.tensor_tensor(out=run[:], in0=P[:, 128:128 + NE], in1=run[:], op=ALU.add),
             reads=[pk, "run", T("pos")], writes=["run"])
        s.op("dve", lambda e: e.tensor_tensor(out=key_[:], in0=pos_[:], in1=sel_[:], op=ALU.mult),
             reads=[T("pos"), T("selm")], writes=[T("key")])
        s.op("dve", lambda e: e.max(out=k8_[:], in_=key_[:]), reads=[T("key")], writes=[T("k8")])
        s.op("dve", lambda e: e.tensor_scalar(out=idx_all[:, ti * 4:ti * 4 + 4], in0=k8_[:, 0:4], scalar1=-1.0, scalar2=None,
                                              op0=ALU.add),
             reads=[T("k8")], writes=[("idx", ti)])
        for k in range(4):
            s.op("dve", lambda e, k=k: e.scalar_tensor_tensor(out=junk_[:], in0=key_[:], scalar=k8_[:, k:k + 1],
                                                              in1=G_all[:, ti, :], op0=ALU.is_equal, op1=ALU.mult,
                                                              accum_out=gk_all[:, ti, k:k + 1]),
                 reads=[T("key"), T("k8"), ("G", ti), T("junk")], writes=[T("junk"), ("gk", ti, k)])
        if DBG.get("rt"):
            s.op("dve", lambda e: e.tensor_copy(out=rtst[:, ti, 0:4], in_=k8_[:, 0:4]), reads=[T("k8")], writes=[("rt", ti, 0)])
            s.op("dve", lambda e: e.tensor_copy(out=rtst[:, ti, 4:8], in_=gk_all[:, ti, :]),
                 reads=[("gk", ti, k) for k in range(4)], writes=[("rt", ti, 1)])
        for k in range(4):
            s.dma("pool", lambda e, k=k: e.indirect_dma_start(
                out=xs_d, out_offset=bass.IndirectOffsetOnAxis(ap=idx_all[:, ti * 4 + k:ti * 4 + k + 1], axis=0),
                in_=h1b[tb][:, :], in_offset=None, bounds_check=_bc(s, e), oob_is_err=False),
                reads=[("h1b", tb), ("idx", ti)], writes=[("xs_d", ti, k)])

    stage_a(0)
    for ti in range(32):
        if ti + 1 < 32:
            stage_a(ti + 1)
        stage_b(ti)
    if DBG.get("rt"):
        s.dma("sp", lambda e: e.dma_start(out=rt_d, in_=rtst[:]), reads=[("rt", ti, j) for ti in range(32) for j in range(2)],
              writes=["rt_d"])


def _ln_tail(s, z, zkeys, st6, skeys, mv, rs, lng, lnb, out, okey, tag):
    s.op("dve", lambda e: e.bn_aggr(out=mv[:], in_=st6[:].rearrange("p a b -> p (a b)")), reads=skeys, writes=["mv" + tag])
    s.op("act", lambda e: e.activation(out=rs[:], in_=mv[:, 1:2], func=AF.Ln, bias=EPS), reads=["mv" + tag], writes=["rs0" + tag])
    s.op("act", lambda e: e.activation(out=rs[:], in_=rs[:], func=AF.Exp, scale=-0.5), reads=["rs0" + tag], writes=["rs" + tag])
    s.op("dve", lambda e: e.scalar_tensor_tensor(out=z[:], in0=z[:], scalar=mv[:, 0:1], in1=lng[:], op0=ALU.subtract,
                                                 op1=ALU.mult),
         reads=zkeys + ["mv" + tag, "lng"], writes=zkeys)
    s.op("dve", lambda e: e.scalar_tensor_tensor(out=out[:], in0=z[:], scalar=rs[:, 0:1], in1=lnb[:], op0=ALU.mult,
                                                 op1=ALU.add),
         reads=zkeys + ["rs" + tag, "lnb"], writes=[okey])


def phase_experts(nc, s, xs_d, y_d, wgu_d, bgu_d, wd_d, bd_d, identb):
    wgu = [s.sb(f"wgu{i}", [128, 8, 2048], BF16) for i in range(2)]
    wdn = [s.sb(f"wdn{i}", [128, 8, D], BF16) for i in range(2)]
    bgu = [s.sb(f"bgu{i}", [128, 16], F32) for i in range(2)]
    bdb = [s.sb(f"bdb{i}", [128, D], F32) for i in range(2)]
    bgu1 = [s.sb(f"bgu1_{i}", [128, 8], F32) for i in range(2)]
    xs = [s.sb(f"xs{i}", [128, 6, D], BF16) for i in range(2)]
    xTe = s.sb("xTe", [128, 8, CAPT], BF16)
    aT = s.sb("aT", [128, 8, CAPT], BF16)
    NH = CAP // 2
    gt = [s.sb(f"gt{i}", [128, NH], F32) for i in range(2)]
    sg = [s.sb(f"sg{i}", [128, NH], F32) for i in range(2)]
    ut = [s.sb(f"ut{i}", [128, NH], F32) for i in range(2)]
    yst = [s.sb(f"yst{i}", [128, D], F32) for i in range(2)]
    NST = 4
    stg = [s.sb(f"stg{i}", [128, 2048], F32) for i in range(NST)]
    pstr = [s.ps(f"pstr{i}", [128, 1024], BF16) for i in range(2)]
    psg = [s.ps(f"psg{i}") for i in range(2)]
    psu = [s.ps(f"psu{i}") for i in range(2)]
    psy = [s.ps(f"psy{i}") for i in range(2)]

    chunks = []
    for ex in range(NE):
        chunks += [(ex, 0, kc) for kc in range(8)] + [(ex, 1, kc) for kc in range(8)]
    st = {"dma": 0, "cast": 0}

    def emit_dma():
        c = st["dma"]
        if c >= len(chunks):
            return
        st["dma"] = c + 1
        ex, kind, kc = chunks[c]
        t = c % NST
        if kind == 0:
            s.dma("sp", lambda e: e.dma_start(out=stg[t][:, :], in_=wgu_d[ex, kc * 128:(kc + 1) * 128, :]), writes=[("stg", t)])
        else:
            s.dma("sp", lambda e: e.dma_start(out=stg[t][:, 0:D], in_=wd_d[ex, kc * 128:(kc + 1) * 128, :]), writes=[("stg", t)])

    def pump():
        c = st["cast"]
        if c >= len(chunks):
            return
        while st["dma"] < min(c + NST, len(chunks)):
            emit_dma()
        st["cast"] = c + 1
        ex, kind, kc = chunks[c]
        t = c % NST
        b = ex % 2
        if kind == 0:
            s.op("act", lambda e: e.activation(out=wgu[b][:, kc, :], in_=stg[t][:, :], func=AF.Identity), reads=[("stg", t)],
                 writes=[("wgu", b, kc)])
        else:
            s.op("act", lambda e: e.activation(out=wdn[b][:, kc, :], in_=stg[t][:, 0:D], func=AF.Identity), reads=[("stg", t)],
                 writes=[("wdn", b, kc)])

    def small_loads(ex):
        b = ex % 2
        s.dma("sp", lambda e: e.dma_start(out=bgu[b][:], in_=bgu_d[ex]), writes=[("bgu", b)])
        s.op("dve", lambda e: e.tensor_scalar(out=bgu1[b][:], in0=bgu[b][:, 8:16], scalar1=1.0, scalar2=None, op0=ALU.add),
             reads=[("bgu", b)], writes=[("bgu1", b)])
        s.dma("sp", lambda e: e.dma_start(out=bdb[b][:], in_=bd_d[ex:ex + 1, :].broadcast_to([128, D])), writes=[("bdb", b)])
        s.dma("sp", lambda e: e.dma_start(
            out=xs[b][:], in_=xs_d[ex * CAP:ex * CAP + CAPT, :].rearrange("(j p) d -> p j d", p=128)), writes=[("xs", b)])

    s.op("pool", lambda e: e.memset(aT[:], 0.0), writes=[("aT", fc, h) for fc in range(8) for h in range(2)])
    small_loads(0)
    for _ in range(16):
        pump()
    ti_ = 0
    gi = 0
    yi = 0
    for ex in range(NE):
        b = ex % 2
        if ex + 1 < NE:
            small_loads(ex + 1)
        WGU = [("wgu", b, kc) for kc in range(8)]
        WDN = [("wdn", b, kc) for kc in range(8)]
        for j in range(6):
            p = ti_ % 2
            ti_ += 1

            def fn(e, p=p, j=j, b=b):
                ins = None
                for kc in range(8):
                    ins = e.transpose(out=pstr[p][:, kc * 128:(kc + 1) * 128], in_=xs[b][:, j, kc * 128:(kc + 1) * 128],
                                      identity=identb[:])
                return ins
            s.op("pe", fn, reads=[("xs", b), "identb"], writes=[("pstr", p)])
            _copy(s, "act", xTe[:, :, j * 128:(j + 1) * 128], pstr[p][:, :].rearrange("p (a b) -> p a b", a=8),
                  reads=[("pstr", p)], writes=[("xTe", j)])
        for fcp in range(8):
            for nh in range(2):
                p = gi % 2
                gi += 1
                ns = slice(nh * NH, (nh + 1) * NH)
                xk = [("xTe", j) for j in ((0, 1, 2) if nh == 0 else (2, 3, 4, 5))]
                s.mm(psg[p][:, 0:NH], [(wgu[b][:, kc, fcp * 128:(fcp + 1) * 128], xTe[:, kc, ns]) for kc in range(8)],
                     reads=WGU + xk, writes=[("psg", p)])
                s.mm(psu[p][:, 0:NH], [(wgu[b][:, kc, D + fcp * 128:D + (fcp + 1) * 128], xTe[:, kc, ns]) for kc in range(8)],
                     reads=WGU + xk, writes=[("psu", p)])
                s.op("dve", lambda e, p=p, b=b, fcp=fcp: e.tensor_scalar(out=gt[p][:], in0=psg[p][:, 0:NH],
                                                                        scalar1=bgu[b][:, fcp:fcp + 1], scalar2=7.0,
                                                                        op0=ALU.add, op1=ALU.min),
                     reads=[("psg", p), ("bgu", b)], writes=[("gt", p)])
                s.op("act", lambda e, p=p: e.activation(out=sg[p][:], in_=gt[p][:], func=AF.Sigmoid, scale=1.702),
                     reads=[("gt", p)], writes=[("sg", p)])
                pump()
                s.op("dve", lambda e, p=p, b=b, fcp=fcp: e.tensor_scalar(out=ut[p][:], in0=psu[p][:, 0:NH],
                                                                        scalar1=bgu1[b][:, fcp:fcp + 1], scalar2=8.0,
                                                                        op0=ALU.add, op1=ALU.min),
                     reads=[("psu", p), ("bgu1", b)], writes=[("ut", p)])
                s.op("dve", lambda e, p=p: e.scalar_tensor_tensor(out=ut[p][:], in0=ut[p][:], scalar=-6.0, in1=gt[p][:],
                                                                  op0=ALU.max, op1=ALU.mult),
                     reads=[("ut", p), ("gt", p)], writes=[("ut", p)])
                s.op("dve", lambda e, p=p, fcp=fcp, ns=ns: e.tensor_tensor(out=aT[:, fcp, ns], in0=ut[p][:], in1=sg[p][:],
                                                                           op=ALU.mult),
                     reads=[("sg", p), ("ut", p)], writes=[("aT", fcp, nh)])
        for j in range(6):
            yb = yi % 2
            yi += 1
            for dh in range(2):
                s.mm(psy[dh][:, :], [(aT[:, fc, j * 128:(j + 1) * 128], wdn[b][:, fc, dh * 512:(dh + 1) * 512]) for fc in range(8)],
                     reads=WDN + [("aT", fc, h) for fc in range(8) for h in ((0,) if j < 2 else ((0, 1) if j == 2 else (1,)))],
                     writes=[("psy", dh)])
                s.op("dve", lambda e, yb=yb, dh=dh, b=b: e.tensor_tensor(out=yst[yb][:, dh * 512:(dh + 1) * 512], in0=psy[dh][:, :],
                                                                         in1=bdb[b][:, dh * 512:(dh + 1) * 512], op=ALU.add),
                     reads=[("psy", dh), ("bdb", b)], writes=[("yst", yb, dh)])
            r0 = ex * CAP + j * 128
            nr = min(128, CAP - j * 128)
            s.dma("sp", lambda e, yb=yb, r0=r0, nr=nr: e.dma_start(out=y_d[r0:r0 + nr, :], in_=yst[yb][0:nr, :]),
                  reads=[("yst", yb, 0), ("yst", yb, 1)], writes=[("y_d", r0)])


def phase_combine(nc, s, y_d, h1_d, bd_d, ln2g_d, ln2b_d, out_d, identf, idx_all, gk_all, G_all):
    lng = s.sb("lng2", [128, D], F32)
    lnb = s.sb("lnb2", [128, D], F32)
    yk = [[s.sb(f"yk{i}_{k}", [128, D], F32) for k in range(4)] for i in range(3)]
    h1 = [s.sb(f"h1c{i}", [128, D], F32) for i in range(3)]
    z = [s.sb(f"zc{i}", [128, D], F32) for i in range(3)]
    ot = [s.sb(f"ot{i}", [128, D], F32) for i in range(3)]
    st6 = [s.sb(f"st6c{i}", [128, 2, 6], F32) for i in range(3)]
    mv = [s.sb(f"mvc{i}", [128, 2], F32) for i in range(3)]
    rs = [s.sb(f"rsc{i}", [128, 1], F32) for i in range(3)]
    s.dma("sp", lambda e: e.dma_start(out=lng[:], in_=ln2g_d.broadcast_to([128, D])), writes=["lng"])
    s.dma("sp", lambda e: e.dma_start(out=lnb[:], in_=ln2b_d.broadcast_to([128, D])), writes=["lnb"])
    for ti in range(32):
        b = ti % 3
        r0 = ti * 128
        s.dma("sp", lambda e, b=b, r0=r0: e.dma_start(out=h1[b][:], in_=h1_d[r0:r0 + 128, :]), writes=[("h1c", b)])
        for k in range(4):
            s.dma("pool", lambda e, b=b, ti=ti, k=k: e.indirect_dma_start(
                out=yk[b][k][:, :], out_offset=None, in_=y_d,
                in_offset=bass.IndirectOffsetOnAxis(ap=idx_all[:, ti * 4 + k:ti * 4 + k + 1], axis=0),
                bounds_check=_bc(s, e), oob_is_err=False), writes=[("yk", b, k)])
        zk = [("zc", b)]
        s.op("dve", lambda e, b=b, ti=ti: e.tensor_scalar(out=z[b][:], in0=yk[b][0][:], scalar1=gk_all[:, ti, 0:1], scalar2=None,
                                                          op0=ALU.mult),
             reads=[("yk", b, 0)], writes=zk)
        for k in range(1, 4):
            s.op("dve", lambda e, b=b, ti=ti, k=k: e.scalar_tensor_tensor(out=z[b][:], in0=yk[b][k][:], scalar=gk_all[:, ti, k:k + 1],
                                                                           in1=z[b][:], op0=ALU.mult, op1=ALU.add),
                 reads=[("yk", b, k)] + zk, writes=zk)
        s.op("dve", lambda e, b=b: e.scalar_tensor_tensor(out=z[b][:], in0=h1[b][:], scalar=ALPHA, in1=z[b][:], op0=ALU.mult,
                                                          op1=ALU.add),
             reads=[("h1c", b)] + zk, writes=zk)
        for hf in range(2):
            s.op("dve", lambda e, b=b, hf=hf: e.bn_stats(out=st6[b][:, hf, :], in_=z[b][:, hf * 512:(hf + 1) * 512]),
                 reads=zk, writes=[("st6", b, hf)])
        _ln_tail(s, z[b], zk, st6[b], [("st6", b, 0), ("st6", b, 1)], mv[b], rs[b], lng, lnb, ot[b], ("ot", b), f"c{b}")
        s.dma("sp", lambda e, b=b, r0=r0: e.dma_start(out=out_d[r0:r0 + 128, :], in_=ot[b][:]), reads=[("ot", b)],
              writes=[("out_d", ti)])


def _t5_bucket(dist):
    max_exact = 16
    lr = np.log(np.maximum(dist, max_exact).astype(np.float32) / np.float32(max_exact)) / np.float32(math.log(2048 / max_exact))
    large = np.minimum(max_exact + (lr.astype(np.float32) * np.float32(32 - max_exact)).astype(np.int32), 31)
    return np.where(dist < max_exact, dist, large)


def _attn_bias(rel_bias):
    k = np.arange(128)[:, None]
    q = np.arange(128)[None, :]
    out = np.empty((24, 128, 256), np.float32)
    for g, dil in enumerate((1, 4, 16)):
        for kb in range(2):
            dist = q - k + 128 if kb == 0 else q - k
            band = (dist >= 0) & (dist <= 128)
            bk = _t5_bucket(np.maximum(dist, 0) * dil)
            for h in range(8):
                hd = g * 8 + h
                out[hd, :, kb * 128:(kb + 1) * 128] = np.where(band, rel_bias[bk, hd], np.float32(-1e30))
    return out


_NC_CACHE = {}


def prepare_inputs(x, w_in, rel_bias, w_dw, b_dw, conv_ln_g, conv_ln_b, w_o_attn, w_o_conv, w_out,
                   ln1_g, ln1_b, w_router, b_router, w_gate_up, b_gate_up, w_down, b_down, ln2_g, ln2_b):
    f = lambda a: np.ascontiguousarray(np.asarray(a, dtype=np.float32))
    x = f(x)

    def pc(v, n):
        return f(np.asarray(v).reshape(n, 128).T)
    shared = {
        "abias": _attn_bias(f(rel_bias)),
        "w_in": f(w_in[0]),
        "w_dw": f(np.asarray(w_dw)[0, :, 0, :].reshape(31, 6, 128).transpose(2, 1, 0)),
        "b_dw": pc(b_dw[0], 6), "conv_ln_g": pc(conv_ln_g[0], 6), "conv_ln_b": pc(conv_ln_b[0], 6),
        "w_o_attn": f(w_o_attn[0]), "w_o_conv": f(w_o_conv[0]), "w_out": f(w_out[0]),
        "ln1_g": f(ln1_g), "ln1_b": f(ln1_b), "w_router": f(w_router[0]), "b_router": f(b_router),
        "w_gate_up": f(w_gate_up[0]),
        "b_gate_up": f(np.asarray(b_gate_up)[0].reshape(NE, 16, 128).transpose(0, 2, 1)),
        "w_down": f(w_down[0]), "b_down": f(b_down[0]), "ln2_g": f(ln2_g), "ln2_b": f(ln2_b),
    }
    in_maps = []
    for c in range(NCORES):
        bi, hf = c // 2, c % 2
        t0 = hf * OWN
        xh = np.zeros((NP, D), np.float32)
        lo = t0 - HALO
        if lo >= 0:
            xh[:] = x[bi, lo:lo + NP]
        else:
            xh[HALO:] = x[bi, 0:OWN]
        m = dict(shared)
        m["xT"] = np.ascontiguousarray(xh.T)
        m["xown"] = np.ascontiguousarray(x[bi, t0:t0 + OWN])
        m["hbias"] = np.full((128, 1), 0.0 if hf == 1 else -1e30, np.float32)
        in_maps.append(m)
    return in_maps


def kernel(**inputs):
    in_maps = prepare_inputs(**inputs)
    if "nc" not in _NC_CACHE:
        _NC_CACHE["nc"] = build()
    res = run_bass_kernel_spmd(_NC_CACHE["nc"], in_maps, core_ids=list(range(NCORES)))
    out = np.empty((4, 8192, D), np.float32)
    for c in range(NCORES):
        out[c // 2, (c % 2) * OWN:(c % 2 + 1) * OWN] = res.results[c]["out"]
    return out
```

```python
import math
from contextlib import ExitStack
import numpy as np
import concourse.bass as bass
import concourse.mybir as mybir
from concourse.bass_utils import run_bass_kernel_spmd

F32 = mybir.dt.float32
BF16 = mybir.dt.bfloat16
I32 = mybir.dt.int32
AF = mybir.ActivationFunctionType
ALU = mybir.AluOpType

NCORES = 8
D = 1024
OWN = 4096
HALO = 2048
NP = OWN + HALO
CAP = 704
CAPT = 768
NE = 32
ALPHA = 2.0 ** 0.25
EPS = 1e-5
ENGS = ("pe", "act", "dve", "pool", "sp")
DBG = {"iters": 99, "units": True, "final": True, "proj": 3, "pv": True, "slevel": 3, "hb": True, "pvacc": True}


class Sched:
    EPOCH = 8000

    def __init__(self, nc, es):
        self.nc = nc
        self.es = es
        self.loc = es
        self.streams = {e: [] for e in ENGS}
        self.cnt = {e: 0 for e in ENGS}
        self.esems = {e: [] for e in ENGS}
        self.res = {}
        self.waited = {e: {} for e in ENGS}
        self.dmasems = {}
        self.dmarr = {}
        for q, n in {"sp": 10, "act": 30, "pool": 16}.items():
            self.dmasems[q] = [[self.sem(f"dma_{q}{i}"), 0] for i in range(n)]
            self.dmarr[q] = 0

    def sem(self, name):
        return self.es.enter_context(self.nc.semaphore(name))

    def sb(self, name, shape, dt):
        return self.loc.enter_context(self.nc.sbuf_tensor(name, list(shape), dt))

    def ps(self, name, shape=(128, 512), dt=F32):
        return self.loc.enter_context(self.nc.psum_tensor(name, list(shape), dt))

    def _collect(self, eng, reads, writes):
        deps = []
        for r in reads:
            st = self.res.get(r)
            if st and st["w"] is not None:
                deps.append(st["w"])
        for w in writes:
            st = self.res.get(w)
            if st:
                if st["w"] is not None:
                    deps.append(st["w"])
                deps.extend(st["r"])
        know = self.waited[eng]
        out = []
        for (sem, val, peng, vc) in deps:
            if peng == "pe" and eng == "pe":
                continue
            if know.get(id(sem), 0) >= val:
                continue
            out.append((sem, val))
            for k, v in vc.items():
                if know.get(k, 0) < v:
                    know[k] = v
        return out

    def _record(self, tok, reads, writes):
        for r in reads:
            st = self.res.setdefault(r, {"w": None, "r": []})
            st["r"].append(tok)
        for w in writes:
            self.res[w] = {"w": tok, "r": []}

    def op(self, eng, fn, reads=(), writes=(), attach=None):
        waits = self._collect(eng, reads, writes)
        n = self.cnt[eng]
        ep = n // self.EPOCH
        while len(self.esems[eng]) <= ep:
            self.esems[eng].append(self.sem(f"c_{eng}{len(self.esems[eng])}"))
        sem = self.esems[eng][ep]
        val = n - ep * self.EPOCH + 1
        self.cnt[eng] = n + 1
        for w in waits:
            self.streams[eng].append(("wait", w[0], w[1]))
        self.streams[eng].append(("op", fn, sem, 1, (eng != "pe") if attach is None else attach))
        vc = dict(self.waited[eng])
        vc[id(sem)] = val
        for pe_ in range(ep):
            vc[id(self.esems[eng][pe_])] = self.EPOCH
        tok = (sem, val, eng, vc)
        self._record(tok, reads, writes)
        return tok

    def dma(self, q, fn, reads=(), writes=()):
        waits = self._collect(q, reads, writes)
        slot = self.dmasems[q][self.dmarr[q] % len(self.dmasems[q])]
        self.dmarr[q] += 1
        sem, total = slot
        if total > 0 and self.waited[q].get(id(sem), 0) < total:
            self.waited[q][id(sem)] = total
            waits.append((sem, total))
        total += 16
        slot[1] = total
        for w in waits:
            self.streams[q].append(("wait", w[0], w[1]))
        self.streams[q].append(("op", fn, sem, 16, False))
        vc = dict(self.waited[q])
        vc[id(sem)] = total
        tok = (sem, total, "dma", vc)
        self._record(tok, reads, writes)
        return tok

    def barrier(self):
        keys = list(self.res.keys())
        for eng in ENGS:
            for w in self._collect(eng, keys, keys):
                self.streams[eng].append(("wait", w[0], w[1]))
        self.res = {}

    def emit(self):
        streams = self.streams
        self.regcache = {}

        def run(e, items):
            pend = []
            for it in items:
                if it[0] == "wait":
                    pend.append(it)
                    continue
                attach = pend.pop() if (it[4] and pend) else None
                for w in pend:
                    e.wait_ge(w[1], w[2])
                pend = []
                ins = it[1](e)
                first, last = ins if isinstance(ins, tuple) else (ins, ins)
                if attach is not None:
                    first._wait_ge(attach[1], attach[2])
                last.then_inc(it[2], it[3])
            for w in pend:
                e.wait_ge(w[1], w[2])

        with self.nc.Block() as block:
            @block.sync
            def _(e):
                run(e, streams["sp"])

            @block.scalar
            def _(e):
                run(e, streams["act"])

            @block.vector
            def _(e):
                run(e, streams["dve"])

            @block.gpsimd
            def _(e):
                run(e, streams["pool"])

            @block.tensor
            def _(e):
                run(e, streams["pe"])
        self.streams = {e: [] for e in ENGS}

    def mm(self, out, pairs, reads, writes):
        def fn(e):
            n = len(pairs)
            ins = first = None
            for i, (l, r) in enumerate(pairs):
                ins = e.matmul(out, lhsT=l, rhs=r, start=(i == 0), stop=(i == n - 1))
                if first is None:
                    first = ins
            return first, ins
        return self.op("pe", fn, reads, writes, attach=True)


def _bc(s, e):
    if "bc" not in s.regcache:
        s.regcache["bc"] = e.to_reg(NE * CAP - 1)
    return s.regcache["bc"]


def _copy(s, eng, out, in_, reads, writes):
    if eng == "act":
        return s.op("act", lambda e: e.activation(out=out, in_=in_, func=AF.Identity), reads, writes)
    return s.op(eng, lambda e: e.tensor_copy(out=out, in_=in_), reads, writes)


def build(stop_after=99, debug=False):
    nc = bass.Bass("TRN2", target_bir_lowering=False)

    def din(name, shape, dt=F32):
        return nc.dram_tensor(name, list(shape), dt, kind="ExternalInput").ap()

    xT_d = din("xT", [D, NP])
    xown_d = din("xown", [OWN, D])
    hb_d = din("hbias", [128, 1])
    ab_d = din("abias", [24, 128, 256])
    win_d = din("w_in", [D, 8192])
    wdw_d = din("w_dw", [128, 6, 31])
    bdw_d = din("b_dw", [128, 6])
    clg_d = din("conv_ln_g", [128, 6])
    clb_d = din("conv_ln_b", [128, 6])
    woa_d = din("w_o_attn", [512, D])
    woc_d = din("w_o_conv", [768, D])
    wout_d = din("w_out", [D, D])
    ln1g_d = din("ln1_g", [1, D])
    ln1b_d = din("ln1_b", [1, D])
    wr_d = din("w_router", [D, NE])
    br_d = din("b_router", [1, NE])
    wgu_d = din("w_gate_up", [NE, D, 2048]) if stop_after >= 6 else None
    bgu_d = din("b_gate_up", [NE, 128, 16])
    wd_d = din("w_down", [NE, D, D]) if stop_after >= 6 else None
    bd_d = din("b_down", [NE, D])
    ln2g_d = din("ln2_g", [1, D])
    ln2b_d = din("ln2_b", [1, D])
    out_d = nc.dram_tensor("out", [OWN, D], F32, kind="ExternalOutput").ap()
    skind = "ExternalOutput" if debug else "Internal"
    attnT_d = nc.dram_tensor("attnT_s", [512, OWN], BF16, kind=skind).ap()
    convT_d = nc.dram_tensor("convT_s", [768, OWN], BF16, kind=skind).ap()
    mrgT_d = nc.dram_tensor("mrgT_s", [D, OWN], BF16, kind=skind).ap()
    h1_d = nc.dram_tensor("h1_s", [OWN, D], F32, kind=skind).ap()
    xs_d = nc.dram_tensor("xs_s", [NE * CAP + 128, D], BF16, kind="Internal").ap()
    y_d = nc.dram_tensor("y_s", [NE * CAP, D], F32, kind="Internal").ap()
    rt_d = nc.dram_tensor("rt_s", [128, 32, 8], F32, kind=skind).ap()

    win_v = win_d.rearrange("(kc p) n -> p kc n", p=128)

    with ExitStack() as es:
        s = Sched(nc, es)
        identb = s.sb("identb", [128, 128], BF16)
        identf = s.sb("identf", [128, 128], F32)
        idx_all = s.sb("idx_all", [128, 128], I32)
        gk_all = s.sb("gk_all", [128, 32, 4], F32)
        G_all = s.sb("G_all", [128, 32, NE], F32)
        for t, nm in ((identb, "identb"), (identf, "identf")):
            s.op("pool", lambda e, t=t: e.memset(t[:], 1.0), writes=[nm])
            s.op("pool", lambda e, t=t: e.affine_select(out=t[:], in_=t[:], pattern=[[-1, 128]],
                                                         compare_op=ALU.is_equal, fill=0.0, base=0,
                                                         channel_multiplier=1), reads=[nm], writes=[nm])

        with ExitStack() as es_x:
            s.loc = es_x
            xTb = s.sb("xTb", [128, 8, NP], BF16)
            for j in (1, 0, 2):
                for kc in range(8):
                    s.dma("pool", lambda e, kc=kc, j=j: e.dma_start(
                        out=xTb[:, kc, j * 2048:(j + 1) * 2048],
                        in_=xT_d[kc * 128:(kc + 1) * 128, j * 2048:(j + 1) * 2048]),
                        writes=[("x", kc, j)])
            XR = [("x", kc, j) for kc in range(8) for j in range(3)]

            if stop_after >= 2:
                with ExitStack() as es_p:
                    s.loc = es_p
                    phase_attn(nc, s, xTb, XR, win_v, ab_d, hb_d, attnT_d)
                    s.barrier()
                    s.emit()
            if stop_after >= 3:
                with ExitStack() as es_p:
                    s.loc = es_p
                    phase_conv(nc, s, xTb, XR, win_v, wdw_d, bdw_d, clg_d, clb_d, convT_d, identb)
                    s.barrier()
                    s.emit()
            if stop_after < 3:
                s.barrier()
                s.emit()
            if stop_after >= 4:
                with ExitStack() as es_p:
                    s.loc = es_p
                    phase_merge(nc, s, xTb, XR, win_v, woa_d, woc_d, attnT_d, convT_d, mrgT_d, xs_d)
                    s.barrier()
                    s.emit()
        if stop_after >= 5:
            with ExitStack() as es_p:
                s.loc = es_p
                phase_out_router(nc, s, mrgT_d, wout_d, xown_d, ln1g_d, ln1b_d, wr_d, br_d, h1_d, xs_d,
                                 identf, idx_all, gk_all, G_all, rt_d)
                s.barrier()
                s.emit()
        if stop_after >= 6:
            with ExitStack() as es_p:
                s.loc = es_p
                phase_experts(nc, s, xs_d, y_d, wgu_d, bgu_d, wd_d, bd_d, identb)
                s.barrier()
                s.emit()
        if stop_after >= 7:
            with ExitStack() as es_p:
                s.loc = es_p
                phase_combine(nc, s, y_d, h1_d, bd_d, ln2g_d, ln2b_d, out_d, identf, idx_all, gk_all, G_all)
                s.barrier()
                s.emit()
    return nc


def phase_attn(nc, s, xTb, XR, win_v, ab_d, hb_d, attnT_d):
    acc = [s.sb(f"acc{h}", [128, 2048], F32) for h in range(2)]
    qT = [[s.sb(f"qT{b}_{h}", [128, 2048], BF16) for h in range(2)] for b in range(2)]
    kT = [s.sb(f"kT{b}", [128, 4096], BF16) for b in range(2)]
    vB = [s.sb(f"vB{b}", [128, 32, 2, 128], BF16) for b in range(2)]
    wq = s.sb("wq", [128, 8, 128], BF16)
    wk = s.sb("wk", [128, 8, 128], BF16)
    wv = s.sb("wv", [128, 8, 128], BF16)
    ab = s.sb("ab", [128, 3, 2, 256], F32)
    abh = s.sb("abh", [128, 3, 2, 128], F32)
    tmp = [s.sb(f"tmp{i}", [128, 512], F32) for i in range(3)]
    pt = [s.sb(f"pt{i}", [128, 512], BF16) for i in range(3)]
    rec = tmp[0][0:64, :]
    hb = s.sb("hb", [128, 1], F32)
    psA = [s.ps(f"psA{i}") for i in range(2)]
    psV = s.ps("psV")
    psS = [s.ps(f"psS{i}") for i in range(3)]
    psO = [s.ps("psO0"), s.ps("psO1")]

    s.dma("sp", lambda e: e.dma_start(out=hb[:], in_=hb_d), writes=["hb"])
    for b in range(2):
        s.op("pool", lambda e, b=b: e.memset(vB[b][:], 1.0), writes=[("v", b, i) for i in range(8)])
        for h in range(2):
            s.op("pool", lambda e, b=b, h=h: e.memset(qT[b][h][:], 0.0), writes=[("q", b, h, tc) for tc in range(4)])

    def xr(lo, hi):
        return [("x", kc, j) for kc in range(8) for j in range(lo // 2048, (hi - 1) // 2048 + 1)]

    cnt = {"pa": 0, "ev": 0, "si": 0}
    iters = [(hp, half, g) for hp in range(4) for half in range(2) for g in range(3)][:DBG["iters"]]

    def make_proj(idx):
        hp, half, g = iters[idx]
        dil = (1, 4, 16)[g]
        halo = 128 * dil
        p0 = HALO + half * 2048
        b = idx % 2
        nK = halo + 2048
        kb0 = p0 - halo
        bpr = 16 // dil + 1
        nblk = dil * bpr
        steps = []

        def loads():
            for (wt, base, nm) in ((wq, 0, "wq"), (wk, 1536, "wk"), (wv, 3072, "wv")):
                c0 = base + g * 512 + hp * 128
                s.dma("pool", lambda e, wt=wt, c0=c0: e.dma_start(out=wt[:], in_=win_v[:, :, c0:c0 + 128]), writes=[nm])
        steps.append(loads)

        def qstep(tc):
            def f():
                pi = cnt["pa"] % 2
                cnt["pa"] += 1
                ps, pkey = psA[pi], ("psA", pi)
                s.mm(ps[:, 0:512], [(wq[:, kc, :], xTb[:, kc, p0 + tc * 512:p0 + (tc + 1) * 512]) for kc in range(8)],
                     reads=xr(p0 + tc * 512, p0 + (tc + 1) * 512) + ["wq"], writes=[pkey])
                _copy(s, "act", qT[b][0][0:64, tc * 512:(tc + 1) * 512], ps[0:64, 0:512], reads=[pkey], writes=[("q", b, 0, tc)])
                _copy(s, "act", qT[b][1][64:128, tc * 512:(tc + 1) * 512], ps[64:128, 0:512], reads=[pkey],
                      writes=[("q", b, 1, tc)])
            return f
        for tc in range(4):
            steps.append(qstep(tc))

        def kstep(off, n, ci):
            def f():
                pi = cnt["pa"] % 2
                cnt["pa"] += 1
                ps, pkey = psA[pi], ("psA", pi)
                s.mm(ps[:, 0:n], [(wk[:, kc, :], xTb[:, kc, kb0 + off:kb0 + off + n]) for kc in range(8)],
                     reads=xr(kb0 + off, kb0 + off + n) + ["wk"], writes=[pkey])
                _copy(s, ("act", "act", "dve")[cnt["ev"] % 3], kT[b][:, off:off + n], ps[:, 0:n], reads=[pkey], writes=[("k", b, ci)])
                cnt["ev"] += 1
            return f
        off = 0
        ci = 0
        while off < nK:
            n = min(512, nK - off)
            steps.append(kstep(off, n, ci))
            off += n
            ci += 1

        def vstep(blk0):
            def f():
                nb = min(4, nblk - blk0)
                for j in range(nb):
                    blk = blk0 + j
                    r, mi = blk // bpr, blk % bpr
                    st = p0 + r + dil * 128 * (mi - 1)
                    s.mm(psV[:, j * 128:(j + 1) * 128],
                         [(xTb[:, kc, st:st + 127 * dil + 1:dil], wv[:, kc, :]) for kc in range(8)],
                         reads=xr(st, st + 127 * dil + 1) + ["wv"], writes=["psV"])
                _copy(s, ("act", "act", "dve")[cnt["ev"] % 3], vB[b][:, blk0:blk0 + nb, :, 0:64],
                      psV[:, 0:nb * 128].rearrange("p (a b c) -> p a b c", a=nb, b=2),
                      reads=["psV"], writes=[("v", b, blk0 // 4)])
                cnt["ev"] += 1
            return f
        for blk0 in range(0, nblk, 4):
            steps.append(vstep(blk0))
        return steps

    def emit_S2(pair, i, b, g, dil, halo, nK, half):
        hh = pair[0][0]
        pS, tm, pT = psS[i], tmp[i], pt[i]
        mms = []
        rd = []
        flags = []
        for ui, (_, r, m) in enumerate(pair):
            q0 = r + dil * 128 * m
            qap = qT[b][hh][:, q0:q0 + 127 * dil + 1:dil]
            kp = halo + r + dil * 128 * (m - 1)
            kc_ = halo + r + dil * 128 * m
            kprev = kT[b][:, kp:kp + 127 * dil + 1:dil]
            kcur = kT[b][:, kc_:kc_ + 127 * dil + 1:dil]
            rd += [("q", b, hh, c) for c in range(q0 // 512, (q0 + 128 * dil - 1) // 512 + 1)]
            rd += [("k", b, c) for c in range(kp // 512, min((kc_ + 128 * dil - 1) // 512, (nK - 1) // 512) + 1)]
            mms.append((pS[:, ui * 256:ui * 256 + 128], kprev, qap))
            mms.append((pS[:, ui * 256 + 128:ui * 256 + 256], kcur, qap))
            flags.append(half == 0 and m == 0)

        def fn(e):
            ins = first = None
            for (o_, l_, r_) in mms:
                ins = e.matmul(o_, lhsT=l_, rhs=r_, start=True, stop=True)
                first = first or ins
            return first, ins
        s.op("pe", fn, reads=list(dict.fromkeys(rd)), writes=[("psS", i)], attach=True)
        tkeys = [("tmp", i, 0), ("tmp", i, 1)]
        if not any(flags):
            in1 = ab[:, g, hh:hh + 1, :].broadcast_to([128, 2, 256])
            s.op("dve", lambda e: e.scalar_tensor_tensor(out=tm[:].rearrange("p (a b) -> p a b", a=2),
                                                         in0=pS[:, 0:512].rearrange("p (a b) -> p a b", a=2), scalar=0.125,
                                                         in1=in1, op0=ALU.mult, op1=ALU.add),
                 reads=[("psS", i), ("ab", g, hh)], writes=tkeys)
        else:
            for ui in range(2):
                c0 = ui * 256
                if flags[ui]:
                    s.op("dve", lambda e, c0=c0: e.scalar_tensor_tensor(out=tm[:, c0:c0 + 128], in0=pS[:, c0:c0 + 128], scalar=0.125,
                                                                        in1=abh[:, g, hh, :], op0=ALU.mult, op1=ALU.add),
                         reads=[("psS", i), ("abh", g, hh)], writes=[("tmp", i, ui)])
                    s.op("dve", lambda e, c0=c0: e.scalar_tensor_tensor(out=tm[:, c0 + 128:c0 + 256], in0=pS[:, c0 + 128:c0 + 256],
                                                                        scalar=0.125, in1=ab[:, g, hh, 128:256], op0=ALU.mult,
                                                                        op1=ALU.add),
                         reads=[("psS", i), ("ab", g, hh), ("tmp", i, ui)], writes=[("tmp", i, ui)])
                else:
                    s.op("dve", lambda e, c0=c0: e.scalar_tensor_tensor(out=tm[:, c0:c0 + 256], in0=pS[:, c0:c0 + 256], scalar=0.125,
                                                                        in1=ab[:, g, hh, :], op0=ALU.mult, op1=ALU.add),
                         reads=[("psS", i), ("ab", g, hh)], writes=[("tmp", i, ui)])
        s.op("act", lambda e: e.activation(out=pT[:], in_=tm[:], func=AF.Exp), reads=tkeys, writes=[("pt", i)])

    def emit_PV2(pair, i, o, b, g, dil, bpr):
        hh = pair[0][0]
        pT = pt[i]
        mms = []
        rd = [("pt", i)]
        for ui, (_, r, m) in enumerate(pair):
            bp = r * bpr + m
            po = psO[o][:, ui * 128:(ui + 1) * 128]
            mms.append((po, vB[b][:, bp, hh, :], pT[:, ui * 256:ui * 256 + 128], True, False))
            mms.append((po, vB[b][:, bp + 1, hh, :], pT[:, ui * 256 + 128:ui * 256 + 256], False, True))
            rd += [("v", b, bp // 4), ("v", b, (bp + 1) // 4)]

        def fn(e):
            ins = first = None
            for (o_, l_, r_, st_, sp_) in mms:
                ins = e.matmul(o_, lhsT=l_, rhs=r_, start=st_, stop=sp_)
                first = first or ins
            return first, ins
        s.op("pe", fn, reads=list(dict.fromkeys(rd)), writes=[("psO", o)], attach=True)
        av = acc[hh][:].rearrange("p (a j d) -> p a d j", a=16 // dil, j=128, d=dil)
        (_, r0_, m0_), (_, r1_, m1_) = pair
        if m1_ != m0_:
            aap = av[:, m0_:m0_ + 2, r0_, :]
        else:
            aap = av[:, m0_, r0_:r0_ + 2, :]
        pin = psO[o][:, 0:256].rearrange("p (a b) -> p a b", a=2)
        if g == 0:
            s.op("dve", lambda e: e.tensor_copy(out=aap, in_=pin), reads=[("psO", o)], writes=[("acc", hh)])
        else:
            s.op("dve", lambda e: e.tensor_tensor(out=aap, in0=pin, in1=aap, op=ALU.add),
                 reads=[("psO", o), ("acc", hh)], writes=[("acc", hh)])

    def load_ab(hp):
        for g2 in range(3):
            for hh in range(2):
                hd = g2 * 8 + hp * 2 + hh
                s.dma("sp", lambda e, g2=g2, hh=hh, hd=hd: e.dma_start(out=ab[:, g2, hh, :], in_=ab_d[hd]),
                      writes=[("ab", g2, hh)])
                s.op("pool", lambda e, g2=g2, hh=hh: e.tensor_scalar(out=abh[:, g2, hh, :], in0=ab[:, g2, hh, 0:128],
                                                                    scalar1=hb[:, 0:1], scalar2=None, op0=ALU.add),
                     reads=[("ab", g2, hh), "hb"], writes=[("abh", g2, hh)])

    load_ab(0)
    for f in make_proj(0):
        f()
    for idx, (hp, half, g) in enumerate(iters):
        dil = (1, 4, 16)[g]
        halo = 128 * dil
        b = idx % 2
        nK = halo + 2048
        bpr = 16 // dil + 1
        nxt = make_proj(idx + 1) if idx + 1 < len(iters) else []
        units = [(hh, r, m) for hh in range(2) for r in range(dil) for m in range(16 // dil)]
        pairs = [(units[k], units[k + 1]) for k in range(0, len(units), 2)]
        pend = []
        for pi2, pr in enumerate(pairs):
            i = cnt["si"] % 3
            cnt["si"] += 1
            emit_S2(pr, i, b, g, dil, halo, nK, half)
            pend.append((pr, i, pi2 % 2))
            if len(pend) > 2:
                pp, pi_, po_ = pend.pop(0)
                emit_PV2(pp, pi_, po_, b, g, dil, bpr)
            if nxt and pi2 == 0:
                nxt.pop(0)()
            if pi2 >= 5:
                for _ in range(2):
                    if nxt:
                        nxt.pop(0)()
        while pend:
            pp, pi_, po_ = pend.pop(0)
            emit_PV2(pp, pi_, po_, b, g, dil, bpr)
        if idx + 1 < len(iters) and iters[idx + 1][0] != hp:
            load_ab(iters[idx + 1][0])
        while nxt:
            nxt.pop(0)()
        if g == 2:
            for hh in range(2):
                for c in range(4):
                    cs = slice(c * 512, (c + 1) * 512)
                    s.op("dve", lambda e, hh=hh, cs=cs: e.tensor_copy(out=rec, in_=acc[hh][64:128, cs]),
                         reads=[("acc", hh)], writes=[("tmp", 0, 0), ("tmp", 0, 1)])
                    s.op("act", lambda e: e.activation(out=rec, in_=rec, func=AF.Ln),
                         reads=[("tmp", 0, 0), ("tmp", 0, 1)], writes=[("tmp", 0, 0), ("tmp", 0, 1)])
                    s.op("act", lambda e: e.activation(out=rec, in_=rec, func=AF.Exp, scale=-1.0),
                         reads=[("tmp", 0, 0), ("tmp", 0, 1)], writes=[("tmp", 0, 0), ("tmp", 0, 1)])
                    s.op("dve", lambda e, hh=hh, cs=cs: e.tensor_tensor(out=acc[hh][0:64, cs], in0=acc[hh][0:64, cs], in1=rec,
                                                                        op=ALU.mult),
                         reads=[("tmp", 0, 0), ("tmp", 0, 1), ("acc", hh)], writes=[("acc", hh)])
                row = (hp * 2 + hh) * 64
                s.dma("pool", lambda e, hh=hh, row=row, half=half: e.dma_start(
                    out=attnT_d[row:row + 64, half * 2048:(half + 1) * 2048], in_=acc[hh][0:64, :]),
                    reads=[("acc", hh)], writes=[("attnT_d", row, half)])


def phase_conv(nc, s, xTb, XR, win_v, wdw_d, bdw_d, clg_d, clb_d, convT_d, identb):
    dw = s.sb("dw", [128, 6, 2048], F32)
    glu = [s.sb(f"glu{i}", [128, 32 + 2048], BF16) for i in range(2)]
    dg = [s.sb(f"dg{i}", [128, 31, 128], BF16) for i in range(2)]
    wu = [s.sb(f"wu{i}", [128, 8, 128], BF16) for i in range(2)]
    wg = [s.sb(f"wg{i}", [128, 8, 128], BF16) for i in range(2)]
    sgt = [s.sb(f"sgt{i}", [128, 512], F32) for i in range(2)]
    sq = [s.sb(f"sq{i}", [128, 512], F32) for i in range(2)]
    sd = s.sb("sd", [128, 512], F32)
    rstd = s.sb("rstd", [128, 512], F32)
    cst = [s.sb(f"cst{i}", [128, 6, 512], BF16) for i in range(2)]
    wdw = s.sb("wdw", [128, 6, 31], F32)
    bdw = s.sb("bdw", [128, 6], F32)
    clg = s.sb("clg", [128, 6], F32)
    clb = s.sb("clb", [128, 6], F32)
    onesf = s.sb("onesf", [128, 128], F32)
    psU = [s.ps(f"psU{i}") for i in range(2)]
    psG = [s.ps(f"psG{i}") for i in range(2)]
    psM = s.ps("psM")
    psV2 = s.ps("psV2")
    psC = [s.ps("psC0"), s.ps("psC1")]
    for t, d_, nm in ((wdw, wdw_d, "wdw"), (bdw, bdw_d, "bdw"), (clg, clg_d, "clg"), (clb, clb_d, "clb")):
        s.dma("sp", lambda e, t=t, d_=d_: e.dma_start(out=t[:], in_=d_), writes=[nm])
    s.op("pool", lambda e: e.memset(onesf[:], 1.0 / 768.0), writes=["onesf"])
    convT_v = convT_d.rearrange("(cc p) t -> p cc t", p=128)
    it = 0
    pu = 0
    ci = 0
    for half in range(2):
        p0 = HALO + half * 2048
        for cc in range(6):
            b = it % 2
            it += 1
            cv = 4608 + cc * 128
            cg = 4608 + 768 + cc * 128
            s.dma("pool", lambda e, b=b, cv=cv: e.dma_start(out=wu[b][:], in_=win_v[:, :, cv:cv + 128]), writes=[("wu", b)])
            s.dma("pool", lambda e, b=b, cg=cg: e.dma_start(out=wg[b][:], in_=win_v[:, :, cg:cg + 128]), writes=[("wg", b)])
            for (off, n) in [(0, 32)] + [(32 + i * 512, 512) for i in range(4)]:
                pi = pu % 2
                pu += 1
                t0 = p0 - 32 + off
                s.mm(psU[pi][:, 0:n], [(wu[b][:, kc, :], xTb[:, kc, t0:t0 + n]) for kc in range(8)],
                     reads=XR + [("wu", b)], writes=[("psU", pi)])
                s.mm(psG[pi][:, 0:n], [(wg[b][:, kc, :], xTb[:, kc, t0:t0 + n]) for kc in range(8)],
                     reads=XR + [("wg", b)], writes=[("psG", pi)])
                s.op("act", lambda e, pi=pi, n=n: e.activation(out=sgt[pi][:, 0:n], in_=psG[pi][:, 0:n], func=AF.Sigmoid),
                     reads=[("psG", pi)], writes=[("sgt", pi)])
                s.op("dve", lambda e, pi=pi, n=n, off=off, b=b: e.tensor_tensor(out=glu[b][:, off:off + n], in0=psU[pi][:, 0:n],
                                                                                in1=sgt[pi][:, 0:n], op=ALU.mult),
                     reads=[("psU", pi), ("sgt", pi)], writes=[("glu", b)])
            for j in range(31):
                s.op("dve", lambda e, b=b, cc=cc, j=j: e.tensor_scalar(out=dg[b][:, j, :], in0=identb[:], scalar1=wdw[:, cc, j:j + 1],
                                                                       scalar2=None, op0=ALU.mult),
                     reads=["identb", "wdw"], writes=[("dg", b)])
            for tc in range(4):
                pc = (it * 4 + tc) % 2
                s.mm(psC[pc][:, :], [(dg[b][:, j, :], glu[b][:, 2 + j + tc * 512:2 + j + (tc + 1) * 512]) for j in range(31)],
                     reads=[("dg", b), ("glu", b)], writes=[("psC", pc)])
                s.op("dve", lambda e, cc=cc, tc=tc, pc=pc: e.tensor_scalar(out=dw[:, cc, tc * 512:(tc + 1) * 512], in0=psC[pc][:, :],
                                                                          scalar1=bdw[:, cc:cc + 1], scalar2=None, op0=ALU.add),
                     reads=[("psC", pc), "bdw"], writes=[("dw", cc)])
        for tc in range(4):
            ts_ = slice(tc * 512, (tc + 1) * 512)
            s.mm(psM[:, :], [(onesf[:], dw[:, cc, ts_]) for cc in range(6)], reads=[("dw", cc) for cc in range(6)] + ["onesf"],
                 writes=["psM"])
            for cc in range(6):
                s.op("dve", lambda e, cc=cc, ts_=ts_: e.tensor_tensor(out=dw[:, cc, ts_], in0=dw[:, cc, ts_], in1=psM[:, :],
                                                                      op=ALU.subtract),
                     reads=["psM", ("dw", cc)], writes=[("dw", cc)])
            sqt = []
            for cc in range(6):
                qi = cc % 2
                s.op("act", lambda e, cc=cc, qi=qi, ts_=ts_: e.activation(out=sq[qi][:], in_=dw[:, cc, ts_], func=AF.Square),
                     reads=[("dw", cc)], writes=[("sq", qi)])
                def fn(e, cc=cc, qi=qi):
                    return e.matmul(psV2[:, :], lhsT=onesf[:], rhs=sq[qi][:], start=(cc == 0), stop=(cc == 5))
                s.op("pe", fn, reads=[("sq", qi), "onesf"], writes=["psV2"], attach=True)
            s.op("act", lambda e: e.activation(out=sd[:], in_=psV2[:, :], func=AF.Sqrt, bias=EPS), reads=["psV2"], writes=["sd"])
            s.op("dve", lambda e: e.reciprocal(out=rstd[:], in_=sd[:]), reads=["sd"], writes=["rstd"])
            cb = ci % 2
            ci += 1
            for cc in range(6):
                s.op("dve", lambda e, cc=cc, ts_=ts_: e.tensor_tensor(out=dw[:, cc, ts_], in0=dw[:, cc, ts_], in1=rstd[:],
                                                                      op=ALU.mult),
                     reads=["rstd", ("dw", cc)], writes=[("dw", cc)])
                s.op("dve", lambda e, cc=cc, ts_=ts_: e.tensor_scalar(out=dw[:, cc, ts_], in0=dw[:, cc, ts_], scalar1=clg[:, cc:cc + 1],
                                                                      scalar2=clb[:, cc:cc + 1], op0=ALU.mult, op1=ALU.add),
                     reads=[("dw", cc), "clg", "clb"], writes=[("dw", cc)])
                s.op("act", lambda e, cc=cc, ts_=ts_, cb=cb: e.activation(out=cst[cb][:, cc, :], in_=dw[:, cc, ts_], func=AF.Silu),
                     reads=[("dw", cc)], writes=[("cst", cb)])
            t0 = half * 2048 + tc * 512
            s.dma("sp", lambda e, cb=cb, t0=t0: e.dma_start(out=convT_v[:, :, t0:t0 + 512], in_=cst[cb][:]),
                  reads=[("cst", cb)], writes=[("convT_d", t0)])


def phase_merge(nc, s, xTb, XR, win_v, woa_d, woc_d, attnT_d, convT_d, mrgT_d, xs_d):
    woa = s.sb("woa", [128, 4, D], BF16)
    woc = s.sb("woc", [128, 6, D], BF16)
    wga = s.sb("wga", [128, 8, D], BF16)
    wgc = s.sb("wgc", [128, 8, D], BF16)
    at = [s.sb(f"at{i}", [128, 4, 512], BF16) for i in range(2)]
    cv = [s.sb(f"cv{i}", [128, 6, 512], BF16) for i in range(1)]
    mg = [s.sb(f"mg{i}", [128, 8, 512], BF16) for i in range(1)]
    sga = [s.sb(f"sga{i}", [128, 512], F32) for i in range(2)]
    sgc = [s.sb(f"sgc{i}", [128, 512], F32) for i in range(2)]
    psa = [s.ps(f"psa{i}") for i in range(2)]
    psc = [s.ps(f"psc{i}") for i in range(2)]
    psga = [s.ps(f"psga{i}") for i in range(2)]
    psgc = [s.ps(f"psgc{i}") for i in range(2)]
    for kc in range(8):
        s.dma("pool", lambda e, kc=kc: e.dma_start(out=wga[:, kc, :], in_=win_v[:, kc, 6144:7168]), writes=[("wga", kc)])
    for kc in range(8):
        s.dma("pool", lambda e, kc=kc: e.dma_start(out=wgc[:, kc, :], in_=win_v[:, kc, 7168:8192]), writes=[("wgc", kc)])
    s.dma("pool", lambda e: e.dma_start(out=woa[:], in_=woa_d.rearrange("(h p) n -> p h n", p=128)), writes=["woa"])
    s.dma("pool", lambda e: e.dma_start(out=woc[:], in_=woc_d.rearrange("(c p) n -> p c n", p=128)), writes=["woc"])
    zt = s.sb("zt", [128, D], BF16)
    s.op("dve", lambda e: e.memset(zt[:], 0.0), writes=["zt"])
    xs_z = xs_d.rearrange("(p j) d -> p j d", p=128)
    nrow = (NE * CAP + 128) // 128
    zq = list(range(nrow))

    def zero_some(n):
        for _ in range(n):
            if zq:
                c = zq.pop(0)
                s.dma("act", lambda e, c=c: e.dma_start(out=xs_z[:, c, :], in_=zt[:]), reads=["zt"], writes=[("xs_zero", c)])
    WGA = [("wga", kc) for kc in range(8)]
    WGC = [("wgc", kc) for kc in range(8)]
    attn_v = attnT_d.rearrange("(h p) t -> p h t", p=128)
    conv_v = convT_d.rearrange("(c p) t -> p c t", p=128)
    mrg_v = mrgT_d.rearrange("(c p) t -> p c t", p=128)
    pi = 0
    for tc in range(8):
        b = tc % 2
        ts_ = slice(tc * 512, (tc + 1) * 512)
        xs_ = slice(HALO + tc * 512, HALO + (tc + 1) * 512)
        s.dma("sp", lambda e, b=b, ts_=ts_: e.dma_start(out=at[b][:], in_=attn_v[:, :, ts_]), writes=[("at", b)])
        s.dma("sp", lambda e, ts_=ts_: e.dma_start(out=cv[0][:], in_=conv_v[:, :, ts_]), writes=[("cv", 0)])
        for fc in range(8):
            fs = slice(fc * 128, (fc + 1) * 128)
            p = pi % 2
            pi += 1
            s.mm(psga[p][:, :], [(wga[:, kc, fs], xTb[:, kc, xs_]) for kc in range(8)], reads=XR + WGA, writes=[("psga", p)])
            s.mm(psgc[p][:, :], [(wgc[:, kc, fs], xTb[:, kc, xs_]) for kc in range(8)], reads=XR + WGC, writes=[("psgc", p)])
            s.mm(psa[p][:, :], [(woa[:, h, fs], at[b][:, h, :]) for h in range(4)], reads=["woa", ("at", b)],
                 writes=[("psa", p)])
            s.mm(psc[p][:, :], [(woc[:, c, fs], cv[0][:, c, :]) for c in range(6)], reads=["woc", ("cv", 0)],
                 writes=[("psc", p)])
            s.op("act", lambda e, p=p: e.activation(out=sga[p][:], in_=psga[p][:, :], func=AF.Sigmoid),
                 reads=[("psga", p)], writes=[("sga", p)])
            s.op("act", lambda e, p=p: e.activation(out=sgc[p][:], in_=psgc[p][:, :], func=AF.Sigmoid),
                 reads=[("psgc", p)], writes=[("sgc", p)])
            s.op("dve", lambda e, p=p: e.tensor_tensor(out=sga[p][:], in0=psa[p][:, :], in1=sga[p][:], op=ALU.mult),
                 reads=[("psa", p), ("sga", p)], writes=[("sga", p)])
            s.op("dve", lambda e, p=p: e.tensor_tensor(out=sgc[p][:], in0=psc[p][:, :], in1=sgc[p][:], op=ALU.mult),
                 reads=[("psc", p), ("sgc", p)], writes=[("sgc", p)])
            s.op("dve", lambda e, p=p, fc=fc: e.tensor_tensor(out=mg[0][:, fc, :], in0=sga[p][:], in1=sgc[p][:], op=ALU.add),
                 reads=[("sga", p), ("sgc", p)], writes=[("mg", 0)])
            zero_some(3)
        s.dma("sp", lambda e, ts_=ts_: e.dma_start(out=mrg_v[:, :, ts_], in_=mg[0][:]), reads=[("mg", 0)],
              writes=[("mrg_d", tc)])
    zero_some(len(zq))


def phase_out_router(nc, s, mrgT_d, wout_d, xown_d, ln1g_d, ln1b_d, wr_d, br_d, h1_d, xs_d,
                     identf, idx_all, gk_all, G_all, rt_d):
    wout = s.sb("wout", [128, 8, D], BF16)
    lng = s.sb("lng", [128, D], F32)
    lnb = s.sb("lnb", [128, D], F32)
    wr = s.sb("wr", [128, 8, NE], F32)
    brb = s.sb("brb", [128, NE], F32)
    mg = [s.sb(f"mgc{i}", [128, 8, 512], BF16) for i in range(2)]
    xo = [s.sb(f"xo{i}", [128, D], F32) for i in range(2)]
    z = [s.sb(f"z{i}", [128, D], F32) for i in range(2)]
    h1 = [s.sb(f"h1{i}", [128, D], F32) for i in range(2)]
    h1b = [s.sb(f"h1b{i}", [128, D], BF16) for i in range(2)]
    h1T = [s.sb(f"h1T{i}", [128, 8, 128], F32) for i in range(2)]

    def two(name, shape, dt=F32):
        return [s.sb(f"{name}{i}", shape, dt) for i in range(2)]
    st6 = two("st6", [128, 2, 6])
    mv = two("mv", [128, 2])
    rs = two("rs", [128, 1])
    lg = two("lg", [128, NE])
    m8 = two("m8", [128, 8])
    selm = two("selm", [128, NE])
    selb = two("selb", [128, NE], BF16)
    ex = two("ex", [128, NE])
    den = two("den", [128, 1])
    rden = two("rden", [128, 1])
    pos = two("pos", [128, NE])
    key = two("key", [128, NE])
    k8 = two("k8", [128, 8])
    junk = two("junk", [128, NE])
    run = s.sb("run", [128, NE], F32)
    ustr = s.sb("ustr", [128, 128], BF16)
    onesb = s.sb("onesb", [128, 128], BF16)
    rtst = s.sb("rtst", [128, 32, 8], F32)
    pso = [[s.ps(f"pso{i}_{h}") for h in range(2)] for i in range(2)]
    pst = [s.ps(f"pst{i}") for i in range(2)]
    pslp = [s.ps(f"pslp{i}") for i in range(2)]

    for kc in range(8):
        s.dma("pool", lambda e, kc=kc: e.dma_start(out=wout[:, kc, :], in_=wout_d[kc * 128:(kc + 1) * 128, :]),
              writes=[("wout", kc)])
    WO = [("wout", kc) for kc in range(8)]
    s.dma("sp", lambda e: e.dma_start(out=lng[:], in_=ln1g_d.broadcast_to([128, D])), writes=["lng"])
    s.dma("sp", lambda e: e.dma_start(out=lnb[:], in_=ln1b_d.broadcast_to([128, D])), writes=["lnb"])
    s.dma("sp", lambda e: e.dma_start(out=wr[:], in_=wr_d.rearrange("(kc p) n -> p kc n", p=128)), writes=["wr"])
    s.dma("sp", lambda e: e.dma_start(out=brb[:], in_=br_d.broadcast_to([128, NE])), writes=["brb"])
    s.op("pool", lambda e: e.memset(ustr[:], 1.0), writes=["ustr"])
    s.op("pool", lambda e: e.affine_select(out=ustr[:], in_=ustr[:], pattern=[[1, 128]], compare_op=ALU.is_gt, fill=0.0,
                                           base=0, channel_multiplier=-1), reads=["ustr"], writes=["ustr"])
    s.op("pool", lambda e: e.memset(onesb[:], 1.0), writes=["onesb"])
    s.op("pool", lambda e: e.iota(run[:], pattern=[[CAP, NE]], base=1, channel_multiplier=0,
                                  allow_small_or_imprecise_dtypes=True), writes=["run"])
    mrg_v = mrgT_d.rearrange("(c p) t -> p c t", p=128)

    def stage_a(ti):
        tc, tt = ti // 4, ti % 4
        b = tc % 2
        tb = ti % 2
        r0 = ti * 128
        if tt == 0:
            ts_ = slice(tc * 512, (tc + 1) * 512)
            s.dma("sp", lambda e: e.dma_start(out=mg[b][:], in_=mrg_v[:, :, ts_]), writes=[("mgc", b)])
        s.dma("sp", lambda e: e.dma_start(out=xo[tb][:], in_=xown_d[r0:r0 + 128, :]), writes=[("xo", tb)])
        for hf in range(2):
            hs = slice(hf * 512, (hf + 1) * 512)
            s.mm(pso[tb][hf][:, :], [(mg[b][:, kc, tt * 128:(tt + 1) * 128], wout[:, kc, hs]) for kc in range(8)],
                 reads=[("mgc", b)] + WO, writes=[("pso", tb, hf)])
            s.op("dve", lambda e, hf=hf, hs=hs: e.scalar_tensor_tensor(out=z[tb][:, hs], in0=xo[tb][:, hs], scalar=ALPHA,
                                                                       in1=pso[tb][hf][:, :], op0=ALU.mult, op1=ALU.add),
                 reads=[("xo", tb), ("pso", tb, hf)], writes=[("z", tb, hf)])
            s.op("dve", lambda e, hf=hf, hs=hs: e.bn_stats(out=st6[tb][:, hf, :], in_=z[tb][:, hs]),
                 reads=[("z", tb, hf)], writes=[("st6", tb, hf)])
        _ln_tail(s, z[tb], [("z", tb, 0), ("z", tb, 1)], st6[tb], [("st6", tb, 0), ("st6", tb, 1)], mv[tb], rs[tb],
                 lng, lnb, h1[tb], ("h1", tb), f"r{tb}")
        s.dma("sp", lambda e: e.dma_start(out=h1_d[r0:r0 + 128, :], in_=h1[tb][:]), reads=[("h1", tb)], writes=[("h1_d", ti)])
        s.op("act", lambda e: e.activation(out=h1b[tb][:], in_=h1[tb][:], func=AF.Identity), reads=[("h1", tb)],
             writes=[("h1b", tb)])
        for hf in range(2):
            def fn(e, hf=hf):
                ins = None
                for k in range(4):
                    kc = hf * 4 + k
                    ins = e.transpose(out=pst[hf][:, k * 128:(k + 1) * 128], in_=h1[tb][:, kc * 128:(kc + 1) * 128],
                                      identity=identf[:])
                return ins
            s.op("pe", fn, reads=[("h1", tb), "identf"], writes=[("pst", hf)])
            _copy(s, "act", h1T[tb][:, hf * 4:(hf + 1) * 4, :], pst[hf][:, :].rearrange("p (a b) -> p a b", a=4),
                  reads=[("pst", hf)], writes=[("h1T", tb, hf)])

    def stage_b(ti):
        tb = ti % 2
        P = pslp[tb]
        pk = ("pslp", tb)
        lg_, m8_, sel_, selb_, ex_, den_, rden_, pos_, key_, k8_, junk_ = (lg[tb], m8[tb], selm[tb], selb[tb], ex[tb], den[tb],
                                                                          rden[tb], pos[tb], key[tb], k8[tb], junk[tb])
        T = lambda n: (n, tb)
        s.mm(P[:, 0:NE], [(h1T[tb][:, kc, :], wr[:, kc, :]) for kc in range(8)], reads=[("h1T", tb, 0), ("h1T", tb, 1), "wr"],
             writes=[pk])
        s.op("dve", lambda e: e.tensor_tensor(out=lg_[:], in0=P[:, 0:NE], in1=brb[:], op=ALU.add), reads=[pk, "brb"],
             writes=[T("lg")])
        s.op("dve", lambda e: e.max(out=m8_[:], in_=lg_[:]), reads=[T("lg")], writes=[T("m8")])
        s.op("dve", lambda e: e.tensor_scalar(out=sel_[:], in0=lg_[:], scalar1=m8_[:, 3:4], scalar2=None, op0=ALU.is_ge),
             reads=[T("lg"), T("m8")], writes=[T("selm")])
        s.op("dve", lambda e: e.tensor_scalar(out=ex_[:], in0=lg_[:], scalar1=m8_[:, 0:1], scalar2=None, op0=ALU.subtract),
             reads=[T("lg"), T("m8")], writes=[T("ex")])
        s.op("act", lambda e: e.activation(out=ex_[:], in_=ex_[:], func=AF.Exp), reads=[T("ex")], writes=[T("ex")])
        s.op("dve", lambda e: e.scalar_tensor_tensor(out=ex_[:], in0=ex_[:], scalar=1.0, in1=sel_[:], op0=ALU.mult,
                                                     op1=ALU.mult, accum_out=den_[:]),
             reads=[T("ex"), T("selm")], writes=[T("ex"), T("den")])
        s.op("dve", lambda e: e.reciprocal(out=rden_[:], in_=den_[:]), reads=[T("den")], writes=[T("rden")])
        s.op("dve", lambda e: e.tensor_scalar(out=G_all[:, ti, :], in0=ex_[:], scalar1=rden_[:, 0:1], scalar2=None,
                                              op0=ALU.mult),
             reads=[T("ex"), T("rden")], writes=[("G", ti)])
        s.op("act", lambda e: e.activation(out=selb_[:], in_=sel_[:], func=AF.Identity), reads=[T("selm")], writes=[T("selb")])

        def fnp(e):
            e.matmul(P[:, 64:64 + NE], lhsT=ustr[:], rhs=selb_[:], start=True, stop=True)
            return e.matmul(P[:, 128:128 + NE], lhsT=onesb[:], rhs=selb_[:], start=True, stop=True)
        s.op("pe", fnp, reads=["ustr", "onesb", T("selb"), T("lg")], writes=[pk])
        s.op("dve", lambda e: e.tensor_tensor(out=pos_[:], in0=P[:, 64:64 + NE], in1=run[:], op=ALU.add),
             reads=[pk, "run"], writes=[T("pos")])
        s.op("dve", lambda e: e.tensor_tensor(out=run[:], in0=P[:, 128:128 + NE], in1=run[:], op=ALU.add),
             reads=[pk, "run", T("pos")], writes=["run"])
        s.op("dve", lambda e: e.tensor_tensor(out=key_[:], in0=pos_[:], in1=sel_[:], op=ALU.mult),
             reads=[T("pos"), T("selm")], writes=[T("key")])
        s.op("dve", lambda e: e.max(out=k8_[:], in_=key_[:]), reads=[T("key")], writes=[T("k8")])
        s.op("dve", lambda e: e.tensor_scalar(out=idx_all[:, ti * 4:ti * 4 + 4], in0=k8_[:, 0:4], scalar1=-1.0, scalar2=None,
                                              op0=ALU.add),
             reads=[T("k8")], writes=[("idx", ti)])
        for k in range(4):
            s.op("dve", lambda e, k=k: e.scalar_tensor_tensor(out=junk_[:], in0=key_[:], scalar=k8_[:, k:k + 1],
                                                              in1=G_all[:, ti, :], op0=ALU.is_equal, op1=ALU.mult,
                                                              accum_out=gk_all[:, ti, k:k + 1]),
                 reads=[T("key"), T("k8"), ("G", ti), T("junk")], writes=[T("junk"), ("gk", ti, k)])
        if DBG.get("rt"):
            s.op("dve", lambda e: e.tensor_copy(out=rtst[:, ti, 0:4], in_=k8_[:, 0:4]), reads=[T("k8")], writes=[("rt", ti, 0)])
            s.op("dve", lambda e: e.tensor_copy(out=rtst[:, ti, 4:8], in_=gk_all[:, ti, :]),
                 reads=[("gk", ti, k) for k in range(4)], writes=[("rt", ti, 1)])
        for k in range(4):
            s.dma("pool", lambda e, k=k: e.indirect_dma_start(
                out=xs_d, out_offset=bass.IndirectOffsetOnAxis(ap=idx_all[:, ti * 4 + k:ti * 4 + k + 1], axis=0),
                in_=h1b[tb][:, :], in_offset=None, bounds_check=_bc(s, e), oob_is_err=False),
                reads=[("h1b", tb), ("idx", ti)], writes=[("xs_d", ti, k)])

    stage_a(0)
    for ti in range(32):
        if ti + 1 < 32:
            stage_a(ti + 1)
        stage_b(ti)
    if DBG.get("rt"):
        s.dma("sp", lambda e: e.dma_start(out=rt_d, in_=rtst[:]), reads=[("rt", ti, j) for ti in range(32) for j in range(2)],
              writes=["rt_d"])


def _ln_tail(s, z, zkeys, st6, skeys, mv, rs, lng, lnb, out, okey, tag):
    s.op("dve", lambda e: e.bn_aggr(out=mv[:], in_=st6[:].rearrange("p a b -> p (a b)")), reads=skeys, writes=["mv" + tag])
    s.op("act", lambda e: e.activation(out=rs[:], in_=mv[:, 1:2], func=AF.Ln, bias=EPS), reads=["mv" + tag], writes=["rs0" + tag])
    s.op("act", lambda e: e.activation(out=rs[:], in_=rs[:], func=AF.Exp, scale=-0.5), reads=["rs0" + tag], writes=["rs" + tag])
    s.op("dve", lambda e: e.scalar_tensor_tensor(out=z[:], in0=z[:], scalar=mv[:, 0:1], in1=lng[:], op0=ALU.subtract,
                                                 op1=ALU.mult),
         reads=zkeys + ["mv" + tag, "lng"], writes=zkeys)
    s.op("dve", lambda e: e.scalar_tensor_tensor(out=out[:], in0=z[:], scalar=rs[:, 0:1], in1=lnb[:], op0=ALU.mult,
                                                 op1=ALU.add),
         reads=zkeys + ["rs" + tag, "lnb"], writes=[okey])


def phase_experts(nc, s, xs_d, y_d, wgu_d, bgu_d, wd_d, bd_d, identb):
    wgu = [s.sb(f"wgu{i}", [128, 8, 2048], BF16) for i in range(2)]
    wdn = [s.sb(f"wdn{i}", [128, 8, D], BF16) for i in range(2)]
    bgu = [s.sb(f"bgu{i}", [128, 16], F32) for i in range(2)]
    bdb = [s.sb(f"bdb{i}", [128, D], F32) for i in range(2)]
    bgu1 = [s.sb(f"bgu1_{i}", [128, 8], F32) for i in range(2)]
    xs = [s.sb(f"xs{i}", [128, 6, D], BF16) for i in range(2)]
    xTe = s.sb("xTe", [128, 8, CAPT], BF16)
    aT = s.sb("aT", [128, 8, CAPT], BF16)
    NH = CAP // 2
    gt = [s.sb(f"gt{i}", [128, NH], F32) for i in range(2)]
    sg = [s.sb(f"sg{i}", [128, NH], F32) for i in range(2)]
    ut = [s.sb(f"ut{i}", [128, NH], F32) for i in range(2)]
    yst = [s.sb(f"yst{i}", [128, D], F32) for i in range(2)]
    NST = 4
    stg = [s.sb(f"stg{i}", [128, 2048], F32) for i in range(NST)]
    pstr = [s.ps(f"pstr{i}", [128, 1024], BF16) for i in range(2)]
    psg = [s.ps(f"psg{i}") for i in range(2)]
    psu = [s.ps(f"psu{i}") for i in range(2)]
    psy = [s.ps(f"psy{i}") for i in range(2)]

    chunks = []
    for ex in range(NE):
        chunks += [(ex, 0, kc) for kc in range(8)] + [(ex, 1, kc) for kc in range(8)]
    st = {"dma": 0, "cast": 0}

    def emit_dma():
        c = st["dma"]
        if c >= len(chunks):
            return
        st["dma"] = c + 1
        ex, kind, kc = chunks[c]
        t = c % NST
        if kind == 0:
            s.dma("sp", lambda e: e.dma_start(out=stg[t][:, :], in_=wgu_d[ex, kc * 128:(kc + 1) * 128, :]), writes=[("stg", t)])
        else:
            s.dma("sp", lambda e: e.dma_start(out=stg[t][:, 0:D], in_=wd_d[ex, kc * 128:(kc + 1) * 128, :]), writes=[("stg", t)])

    def pump():
        c = st["cast"]
        if c >= len(chunks):
            return
        while st["dma"] < min(c + NST, len(chunks)):
            emit_dma()
        st["cast"] = c + 1
        ex, kind, kc = chunks[c]
        t = c % NST
        b = ex % 2
        if kind == 0:
            s.op("act", lambda e: e.activation(out=wgu[b][:, kc, :], in_=stg[t][:, :], func=AF.Identity), reads=[("stg", t)],
                 writes=[("wgu", b, kc)])
        else:
            s.op("act", lambda e: e.activation(out=wdn[b][:, kc, :], in_=stg[t][:, 0:D], func=AF.Identity), reads=[("stg", t)],
                 writes=[("wdn", b, kc)])

    def small_loads(ex):
        b = ex % 2
        s.dma("sp", lambda e: e.dma_start(out=bgu[b][:], in_=bgu_d[ex]), writes=[("bgu", b)])
        s.op("dve", lambda e: e.tensor_scalar(out=bgu1[b][:], in0=bgu[b][:, 8:16], scalar1=1.0, scalar2=None, op0=ALU.add),
             reads=[("bgu", b)], writes=[("bgu1", b)])
        s.dma("sp", lambda e: e.dma_start(out=bdb[b][:], in_=bd_d[ex:ex + 1, :].broadcast_to([128, D])), writes=[("bdb", b)])
        s.dma("sp", lambda e: e.dma_start(
            out=xs[b][:], in_=xs_d[ex * CAP:ex * CAP + CAPT, :].rearrange("(j p) d -> p j d", p=128)), writes=[("xs", b)])

    s.op("pool", lambda e: e.memset(aT[:], 0.0), writes=[("aT", fc, h) for fc in range(8) for h in range(2)])
    small_loads(0)
    for _ in range(16):
        pump()
    ti_ = 0
    gi = 0
    yi = 0
    for ex in range(NE):
        b = ex % 2
        if ex + 1 < NE:
            small_loads(ex + 1)
        WGU = [("wgu", b, kc) for kc in range(8)]
        WDN = [("wdn", b, kc) for kc in range(8)]
        for j in range(6):
            p = ti_ % 2
            ti_ += 1

            def fn(e, p=p, j=j, b=b):
                ins = None
                for kc in range(8):
                    ins = e.transpose(out=pstr[p][:, kc * 128:(kc + 1) * 128], in_=xs[b][:, j, kc * 128:(kc + 1) * 128],
                                      identity=identb[:])
                return ins
            s.op("pe", fn, reads=[("xs", b), "identb"], writes=[("pstr", p)])
            _copy(s, "act", xTe[:, :, j * 128:(j + 1) * 128], pstr[p][:, :].rearrange("p (a b) -> p a b", a=8),
                  reads=[("pstr", p)], writes=[("xTe", j)])
        for fcp in range(8):
            for nh in range(2):
                p = gi % 2
                gi += 1
                ns = slice(nh * NH, (nh + 1) * NH)
                xk = [("xTe", j) for j in ((0, 1, 2) if nh == 0 else (2, 3, 4, 5))]
                s.mm(psg[p][:, 0:NH], [(wgu[b][:, kc, fcp * 128:(fcp + 1) * 128], xTe[:, kc, ns]) for kc in range(8)],
                     reads=WGU + xk, writes=[("psg", p)])
                s.mm(psu[p][:, 0:NH], [(wgu[b][:, kc, D + fcp * 128:D + (fcp + 1) * 128], xTe[:, kc, ns]) for kc in range(8)],
                     reads=WGU + xk, writes=[("psu", p)])
                s.op("dve", lambda e, p=p, b=b, fcp=fcp: e.tensor_scalar(out=gt[p][:], in0=psg[p][:, 0:NH],
                                                                        scalar1=bgu[b][:, fcp:fcp + 1], scalar2=7.0,
                                                                        op0=ALU.add, op1=ALU.min),
                     reads=[("psg", p), ("bgu", b)], writes=[("gt", p)])
                s.op("act", lambda e, p=p: e.activation(out=sg[p][:], in_=gt[p][:], func=AF.Sigmoid, scale=1.702),
                     reads=[("gt", p)], writes=[("sg", p)])
                pump()
                s.op("dve", lambda e, p=p, b=b, fcp=fcp: e.tensor_scalar(out=ut[p][:], in0=psu[p][:, 0:NH],
                                                                        scalar1=bgu1[b][:, fcp:fcp + 1], scalar2=8.0,
                                                                        op0=ALU.add, op1=ALU.min),
                     reads=[("psu", p), ("bgu1", b)], writes=[("ut", p)])
                s.op("dve", lambda e, p=p: e.scalar_tensor_tensor(out=ut[p][:], in0=ut[p][:], scalar=-6.0, in1=gt[p][:],
                                                                  op0=ALU.max, op1=ALU.mult),
                     reads=[("ut", p), ("gt", p)], writes=[("ut", p)])
                s.op("dve", lambda e, p=p, fcp=fcp, ns=ns: e.tensor_tensor(out=aT[:, fcp, ns], in0=ut[p][:], in1=sg[p][:],
                                                                           op=ALU.mult),
                     reads=[("sg", p), ("ut", p)], writes=[("aT", fcp, nh)])
        for j in range(6):
            yb = yi % 2
            yi += 1
            for dh in range(2):
                s.mm(psy[dh][:, :], [(aT[:, fc, j * 128:(j + 1) * 128], wdn[b][:, fc, dh * 512:(dh + 1) * 512]) for fc in range(8)],
                     reads=WDN + [("aT", fc, h) for fc in range(8) for h in ((0,) if j < 2 else ((0, 1) if j == 2 else (1,)))],
                     writes=[("psy", dh)])
                s.op("dve", lambda e, yb=yb, dh=dh, b=b: e.tensor_tensor(out=yst[yb][:, dh * 512:(dh + 1) * 512], in0=psy[dh][:, :],
                                                                         in1=bdb[b][:, dh * 512:(dh + 1) * 512], op=ALU.add),
                     reads=[("psy", dh), ("bdb", b)], writes=[("yst", yb, dh)])
            r0 = ex * CAP + j * 128
            nr = min(128, CAP - j * 128)
            s.dma("sp", lambda e, yb=yb, r0=r0, nr=nr: e.dma_start(out=y_d[r0:r0 + nr, :], in_=yst[yb][0:nr, :]),
                  reads=[("yst", yb, 0), ("yst", yb, 1)], writes=[("y_d", r0)])


def phase_combine(nc, s, y_d, h1_d, bd_d, ln2g_d, ln2b_d, out_d, identf, idx_all, gk_all, G_all):
    lng = s.sb("lng2", [128, D], F32)
    lnb = s.sb("lnb2", [128, D], F32)
    yk = [[s.sb(f"yk{i}_{k}", [128, D], F32) for k in range(4)] for i in range(3)]
    h1 = [s.sb(f"h1c{i}", [128, D], F32) for i in range(3)]
    z = [s.sb(f"zc{i}", [128, D], F32) for i in range(3)]
    ot = [s.sb(f"ot{i}", [128, D], F32) for i in range(3)]
    st6 = [s.sb(f"st6c{i}", [128, 2, 6], F32) for i in range(3)]
    mv = [s.sb(f"mvc{i}", [128, 2], F32) for i in range(3)]
    rs = [s.sb(f"rsc{i}", [128, 1], F32) for i in range(3)]
    s.dma("sp", lambda e: e.dma_start(out=lng[:], in_=ln2g_d.broadcast_to([128, D])), writes=["lng"])
    s.dma("sp", lambda e: e.dma_start(out=lnb[:], in_=ln2b_d.broadcast_to([128, D])), writes=["lnb"])
    for ti in range(32):
        b = ti % 3
        r0 = ti * 128
        s.dma("sp", lambda e, b=b, r0=r0: e.dma_start(out=h1[b][:], in_=h1_d[r0:r0 + 128, :]), writes=[("h1c", b)])
        for k in range(4):
            s.dma("pool", lambda e, b=b, ti=ti, k=k: e.indirect_dma_start(
                out=yk[b][k][:, :], out_offset=None, in_=y_d,
                in_offset=bass.IndirectOffsetOnAxis(ap=idx_all[:, ti * 4 + k:ti * 4 + k + 1], axis=0),
                bounds_check=_bc(s, e), oob_is_err=False), writes=[("yk", b, k)])
        zk = [("zc", b)]
        s.op("dve", lambda e, b=b, ti=ti: e.tensor_scalar(out=z[b][:], in0=yk[b][0][:], scalar1=gk_all[:, ti, 0:1], scalar2=None,
                                                          op0=ALU.mult),
             reads=[("yk", b, 0)], writes=zk)
        for k in range(1, 4):
            s.op("dve", lambda e, b=b, ti=ti, k=k: e.scalar_tensor_tensor(out=z[b][:], in0=yk[b][k][:], scalar=gk_all[:, ti, k:k + 1],
                                                                           in1=z[b][:], op0=ALU.mult, op1=ALU.add),
                 reads=[("yk", b, k)] + zk, writes=zk)
        s.op("dve", lambda e, b=b: e.scalar_tensor_tensor(out=z[b][:], in0=h1[b][:], scalar=ALPHA, in1=z[b][:], op0=ALU.mult,
                                                          op1=ALU.add),
             reads=[("h1c", b)] + zk, writes=zk)
        for hf in range(2):
            s.op("dve", lambda e, b=b, hf=hf: e.bn_stats(out=st6[b][:, hf, :], in_=z[b][:, hf * 512:(hf + 1) * 512]),
                 reads=zk, writes=[("st6", b, hf)])
        _ln_tail(s, z[b], zk, st6[b], [("st6", b, 0), ("st6", b, 1)], mv[b], rs[b], lng, lnb, ot[b], ("ot", b), f"c{b}")
        s.dma("sp", lambda e, b=b, r0=r0: e.dma_start(out=out_d[r0:r0 + 128, :], in_=ot[b][:]), reads=[("ot", b)],
              writes=[("out_d", ti)])


def _t5_bucket(dist):
    max_exact = 16
    lr = np.log(np.maximum(dist, max_exact).astype(np.float32) / np.float32(max_exact)) / np.float32(math.log(2048 / max_exact))
    large = np.minimum(max_exact + (lr.astype(np.float32) * np.float32(32 - max_exact)).astype(np.int32), 31)
    return np.where(dist < max_exact, dist, large)


def _attn_bias(rel_bias):
    k = np.arange(128)[:, None]
    q = np.arange(128)[None, :]
    out = np.empty((24, 128, 256), np.float32)
    for g, dil in enumerate((1, 4, 16)):
        for kb in range(2):
            dist = q - k + 128 if kb == 0 else q - k
            band = (dist >= 0) & (dist <= 128)
            bk = _t5_bucket(np.maximum(dist, 0) * dil)
            for h in range(8):
                hd = g * 8 + h
                out[hd, :, kb * 128:(kb + 1) * 128] = np.where(band, rel_bias[bk, hd], np.float32(-1e30))
    return out


_NC_CACHE = {}


def prepare_inputs(x, w_in, rel_bias, w_dw, b_dw, conv_ln_g, conv_ln_b, w_o_attn, w_o_conv, w_out,
                   ln1_g, ln1_b, w_router, b_router, w_gate_up, b_gate_up, w_down, b_down, ln2_g, ln2_b):
    f = lambda a: np.ascontiguousarray(np.asarray(a, dtype=np.float32))
    x = f(x)

    def pc(v, n):
        return f(np.asarray(v).reshape(n, 128).T)
    shared = {
        "abias": _attn_bias(f(rel_bias)),
        "w_in": f(w_in[0]),
        "w_dw": f(np.asarray(w_dw)[0, :, 0, :].reshape(31, 6, 128).transpose(2, 1, 0)),
        "b_dw": pc(b_dw[0], 6), "conv_ln_g": pc(conv_ln_g[0], 6), "conv_ln_b": pc(conv_ln_b[0], 6),
        "w_o_attn": f(w_o_attn[0]), "w_o_conv": f(w_o_conv[0]), "w_out": f(w_out[0]),
        "ln1_g": f(ln1_g), "ln1_b": f(ln1_b), "w_router": f(w_router[0]), "b_router": f(b_router),
        "w_gate_up": f(w_gate_up[0]),
        "b_gate_up": f(np.asarray(b_gate_up)[0].reshape(NE, 16, 128).transpose(0, 2, 1)),
        "w_down": f(w_down[0]), "b_down": f(b_down[0]), "ln2_g": f(ln2_g), "ln2_b": f(ln2_b),
    }
    in_maps = []
    for c in range(NCORES):
        bi, hf = c // 2, c % 2
        t0 = hf * OWN
        xh = np.zeros((NP, D), np.float32)
        lo = t0 - HALO
        if lo >= 0:
            xh[:] = x[bi, lo:lo + NP]
        else:
            xh[HALO:] = x[bi, 0:OWN]
        m = dict(shared)
        m["xT"] = np.ascontiguousarray(xh.T)
        m["xown"] = np.ascontiguousarray(x[bi, t0:t0 + OWN])
        m["hbias"] = np.full((128, 1), 0.0 if hf == 1 else -1e30, np.float32)
        in_maps.append(m)
    return in_maps


def kernel(**inputs):
    in_maps = prepare_inputs(**inputs)
    if "nc" not in _NC_CACHE:
        _NC_CACHE["nc"] = build()
    res = run_bass_kernel_spmd(_NC_CACHE["nc"], in_maps, core_ids=list(range(NCORES)))
    out = np.empty((4, 8192, D), np.float32)
    for c in range(NCORES):
        out[c // 2, (c % 2) * OWN:(c % 2 + 1) * OWN] = res.results[c]["out"]
    return out
```
